# Optimizing a Trainium2 kernel written in Bass

```python
import math
import jax, jax.numpy as jnp
from jax import lax
import numpy as np

D_MODEL = 1024
BATCH = 2
SEQ = 8192
DEPTH = 2

CHUNK = 64
N_MIXERS = 2
N_A_LAYERS = (DEPTH + 1) // 2
N_B_LAYERS = DEPTH // 2
EPS = 1e-6

RET_HEADS = 4
RET_QK_DIM = D_MODEL // RET_HEADS
RET_V_DIM = 2 * RET_QK_DIM
RET_V_WIDTH = RET_HEADS * RET_V_DIM
RET_IN_WIDTH = 2 * D_MODEL + 2 * RET_V_WIDTH
ROPE_BASE = 10000.0

SGU_BLOCK = 128
SGU_GROUPS = 8
SGU_FFN = 6 * D_MODEL
SGU_HALF = SGU_FFN // 2
SGU_GROUP_DIM = SGU_HALF // SGU_GROUPS

PEER_HEADS = 8
PEER_NKEYS = 128
PEER_EXPERTS = PEER_NKEYS * PEER_NKEYS
PEER_KEY_DIM = 256
PEER_HALF = PEER_KEY_DIM // 2
PEER_TOPK = 16
PEER_TOKEN_BLOCK = 128

kernel_name = "hybrid_retention_sgu_peer_adaln"


def rmsnorm(x, gain):
    x32 = x.astype(jnp.float32)
    y = x32 * lax.rsqrt(jnp.mean(x32 * x32, axis=-1, keepdims=True) + EPS)
    return (y * gain.astype(jnp.float32)).astype(x.dtype)


def layernorm(x, gain, bias):
    x32 = x.astype(jnp.float32)
    mu = jnp.mean(x32, axis=-1, keepdims=True)
    xc = x32 - mu
    y = xc * lax.rsqrt(jnp.mean(xc * xc, axis=-1, keepdims=True) + EPS)
    return (y * gain.astype(jnp.float32) + bias.astype(jnp.float32)).astype(x.dtype)


def modulate(h, shift, scale):
    return h * (1 + scale[:, None, :]) + shift[:, None, :]


def rotary(x, positions):
    half = x.shape[-1] // 2
    inv_freq = 1.0 / (ROPE_BASE ** (jnp.arange(half, dtype=jnp.float32) / half))
    ang = positions.astype(jnp.float32)[..., None] * inv_freq
    cos = jnp.cos(ang)[:, :, None, :]
    sin = jnp.sin(ang)[:, :, None, :]
    x32 = x.astype(jnp.float32)
    x1, x2 = x32[..., :half], x32[..., half:]
    return jnp.concatenate([x1 * cos - x2 * sin, x1 * sin + x2 * cos], axis=-1)


def retention_mixer(h, positions, w_in, w_out):
    B, S, _ = h.shape
    nc = S // CHUNK
    H, Dk, Dv = RET_HEADS, RET_QK_DIM, RET_V_DIM
    proj = h @ w_in
    q, k, v, g = jnp.split(proj, [D_MODEL, 2 * D_MODEL, 2 * D_MODEL + RET_V_WIDTH], axis=-1)
    q = rotary(q.reshape(B, S, H, Dk), positions)
    k = rotary(k.reshape(B, S, H, Dk), positions) * (Dk ** -0.5)
    v = v.reshape(B, S, H, Dv).astype(jnp.float32)
    log_gamma = jnp.log(1.0 - 2.0 ** (-5.0 - jnp.arange(H, dtype=jnp.float32)))
    idx = jnp.arange(CHUNK, dtype=jnp.float32)
    d_intra = jnp.exp(log_gamma[:, None, None] * jnp.abs(idx[:, None] - idx[None, :]))
    xi = jnp.exp(log_gamma[None, :] * (idx[:, None] + 1.0))
    zeta = jnp.exp(log_gamma[None, :] * (CHUNK - 1.0 - idx[:, None]))
    gamma_chunk = jnp.exp(log_gamma * CHUNK)

    def to_chunks(t):
        return jnp.moveaxis(t.reshape(B, nc, CHUNK, H, t.shape[-1]), 1, 0)

    def step(state, inp):
        qi, ki, vi = inp
        scores = jnp.einsum('bihd,bjhd->bhij', qi, ki) * d_intra[None]
        intra = jnp.einsum('bhij,bjhe->bihe', scores, vi)
        cross = jnp.einsum('bihd,bhde->bihe', qi * xi[None, :, :, None], state)
        state = state * gamma_chunk[None, :, None, None] + jnp.einsum(
            'bjhd,bjhe->bhde', ki * zeta[None, :, :, None], vi)
        return state, intra + cross

    state0 = jnp.zeros((B, H, Dk, Dv), jnp.float32)
    _, y = lax.scan(step, state0, (to_chunks(q), to_chunks(k), to_chunks(v)))
    y = jnp.moveaxis(y, 0, 1).reshape(B, S, H, Dv)
    y = y * lax.rsqrt(jnp.mean(y * y, axis=-1, keepdims=True) + EPS)
    y = y.reshape(B, S, RET_V_WIDTH).astype(h.dtype)
    return (jax.nn.silu(g) * y) @ w_out


def sgu_mixer(h, w_in, b_in, ln_g, ln_b, w_s, b_s, w_out):
    B, S, _ = h.shape
    nb = S // SGU_BLOCK
    z = jax.nn.gelu(h @ w_in + b_in)
    u, v = jnp.split(z, 2, axis=-1)
    v = layernorm(v, ln_g, ln_b)
    pos_chunk = jnp.arange(SGU_BLOCK) // CHUNK
    mask = (pos_chunk[None, :] <= pos_chunk[:, None]).astype(w_s.dtype)
    vb = v.reshape(B, nb, SGU_BLOCK, SGU_GROUPS, SGU_GROUP_DIM)
    s = jnp.einsum('gij,bnjgc->bnigc', w_s * mask[None], vb) + b_s.T[None, None, :, :, None]
    s = s.reshape(B, S, SGU_HALF)
    return (u * s) @ w_out


def peer_ffn(h, w_query, sub_keys, expert_u, expert_v):
    B, S, D = h.shape
    T = B * S
    Hh, K = PEER_HEADS, PEER_TOPK
    ht = h.reshape(T, D)
    q = (ht @ w_query).reshape(T, Hh, 2, PEER_HALF).astype(jnp.float32)
    scores = jnp.einsum('thpd,hpkd->thpk', q, sub_keys.astype(jnp.float32))
    s_top, i_top = lax.top_k(scores, K)
    cand = (s_top[:, :, 0, :, None] + s_top[:, :, 1, None, :]).reshape(T, Hh, K * K)
    cand_idx = (i_top[:, :, 0, :, None] * PEER_NKEYS + i_top[:, :, 1, None, :]).reshape(T, Hh, K * K)
    best, pos = lax.top_k(cand, K)
    expert_idx = jnp.take_along_axis(cand_idx, pos, axis=-1)
    gates = jax.nn.softmax(best, axis=-1).astype(h.dtype)
    nb = T // PEER_TOKEN_BLOCK

    def block(args):
        xb, idx, g = args
        u = expert_u[idx]
        a = jax.nn.gelu(jnp.einsum('tkd,td->tk', u, xb)) * g
        return jnp.einsum('tk,tkd->td', a, expert_v[idx])

    out = lax.map(block, (ht.reshape(nb, PEER_TOKEN_BLOCK, D),
                          expert_idx.reshape(nb, PEER_TOKEN_BLOCK, Hh * K),
                          gates.reshape(nb, PEER_TOKEN_BLOCK, Hh * K)))
    return out.reshape(B, S, D)


def setup_inputs(seed: int = 0) -> dict:
    key = jax.random.key(seed)
    ks = jax.random.split(key, 24)
    f32 = jnp.float32
    D = D_MODEL

    def nrm(k, shape, scale):
        return jax.random.normal(k, shape, f32) * scale

    return {
        "x": nrm(ks[0], (BATCH, SEQ, D), 1.0),
        "c": nrm(ks[1], (BATCH, D), 1.0),
        "positions": jnp.broadcast_to(jnp.arange(SEQ, dtype=jnp.int32), (BATCH, SEQ)),
        "norm_mix": 1.0 + nrm(ks[2], (DEPTH, D), 0.05),
        "norm_ffn": 1.0 + nrm(ks[3], (DEPTH, D), 0.05),
        "ada_w": nrm(ks[4], (DEPTH, D, 6 * D), 0.5 * D ** -0.5),
        "ada_b": nrm(ks[5], (DEPTH, 6 * D), 0.02),
        "ret_w_in": nrm(ks[6], (N_A_LAYERS, D, RET_IN_WIDTH), D ** -0.5),
        "ret_w_out": nrm(ks[7], (N_A_LAYERS, RET_V_WIDTH, D), RET_V_WIDTH ** -0.5),
        "sgu_w_in": nrm(ks[8], (N_B_LAYERS, D, SGU_FFN), D ** -0.5),
        "sgu_b_in": nrm(ks[9], (N_B_LAYERS, SGU_FFN), 0.02),
        "sgu_ln_g": 1.0 + nrm(ks[10], (N_B_LAYERS, SGU_HALF), 0.05),
        "sgu_ln_b": nrm(ks[11], (N_B_LAYERS, SGU_HALF), 0.02),
        "sgu_w_s": nrm(ks[12], (N_B_LAYERS, SGU_GROUPS, SGU_BLOCK, SGU_BLOCK), SGU_BLOCK ** -0.5),
        "sgu_b_s": 1.0 + nrm(ks[13], (N_B_LAYERS, SGU_GROUPS, SGU_BLOCK), 0.1),
        "sgu_w_out": nrm(ks[14], (N_B_LAYERS, SGU_HALF, D), SGU_HALF ** -0.5),
        "peer_w_query": nrm(ks[15], (DEPTH, D, PEER_HEADS * PEER_KEY_DIM), D ** -0.5),
        "peer_sub_keys": nrm(ks[16], (DEPTH, PEER_HEADS, 2, PEER_NKEYS, PEER_HALF), PEER_HALF ** -0.5),
        "peer_u": nrm(ks[17], (DEPTH, PEER_EXPERTS, D), D ** -0.5),
        "peer_v": nrm(ks[18], (DEPTH, PEER_EXPERTS, D), 0.5),
        "norm_final": 1.0 + nrm(ks[19], (D,), 0.05),
    }


def reference(x, c, positions, norm_mix, norm_ffn, ada_w, ada_b, ret_w_in, ret_w_out,
              sgu_w_in, sgu_b_in, sgu_ln_g, sgu_ln_b, sgu_w_s, sgu_b_s, sgu_w_out,
              peer_w_query, peer_sub_keys, peer_u, peer_v, norm_final):
    c_act = jax.nn.silu(c)
    for layer in range(DEPTH):
        mod = c_act @ ada_w[layer] + ada_b[layer]
        sh1, sc1, g1, sh2, sc2, g2 = jnp.split(mod, 6, axis=-1)
        h = modulate(rmsnorm(x, norm_mix[layer]), sh1, sc1)
        j = layer // N_MIXERS
        if layer % N_MIXERS == 0:
            y = retention_mixer(h, positions, ret_w_in[j], ret_w_out[j])
        else:
            y = sgu_mixer(h, sgu_w_in[j], sgu_b_in[j], sgu_ln_g[j], sgu_ln_b[j],
                          sgu_w_s[j], sgu_b_s[j], sgu_w_out[j])
        x = x + g1[:, None, :] * y
        h = modulate(rmsnorm(x, norm_ffn[layer]), sh2, sc2)
        x = x + g2[:, None, :] * peer_ffn(h, peer_w_query[layer], peer_sub_keys[layer],
                                          peer_u[layer], peer_v[layer])
    return rmsnorm(x, norm_final)
```

```python
import math
from contextlib import ExitStack

import numpy as np
import concourse.bass as bass
import concourse.mybir as mybir
from concourse.bass_utils import run_bass_kernel_spmd

F32 = mybir.dt.float32
BF16 = mybir.dt.bfloat16
I32 = mybir.dt.int32
U32 = mybir.dt.uint32
ALU = mybir.AluOpType
AF = mybir.ActivationFunctionType
AX = mybir.AxisListType

D = 1024
DC = 8
TL = 2048
NPREV = 6144
EPS = 1e-6
ENGS = ("pe", "act", "dve", "pool", "sp")
NDMA_SEM = 6
SAME_ENGINE_SYNC = True
NEG = -1.0e30


class T:
    __slots__ = ("name", "lw", "rd")

    def __init__(self, name=""):
        self.name = name
        self.lw = None
        self.rd = {}


class Sched:
    def __init__(self, nc, stack):
        self.nc = nc
        self._stack = stack
        self.sem = {e: stack.enter_context(nc.semaphore("s_" + e)) for e in ENGS}
        self.dsem = {}
        for q in ("sp", "act", "pool"):
            for k in range(NDMA_SEM):
                self.dsem[(q, k)] = stack.enter_context(nc.semaphore(f"d_{q}{k}"))
        self.cnt = {e: 0 for e in ENGS}
        self.dcnt = {q: 0 for q in ("sp", "act", "pool")}
        self.waited = {e: {} for e in ENGS}
        self.lists = {e: [] for e in ENGS}

    def _deps(self, eng, reads, writes):
        deps = {}

        def add(sv):
            if sv is None:
                return
            s, v = sv
            if deps.get(s, 0) < v:
                deps[s] = v
        for r in reads:
            add(r.lw)
        for w in writes:
            add(w.lw)
            for s, v in w.rd.items():
                add((s, v))
        out = []
        for s, v in deps.items():
            if s == eng and (eng == "pe" or not SAME_ENGINE_SYNC):
                continue
            if self.waited[eng].get(s, 0) >= v:
                continue
            self.waited[eng][s] = v
            out.append((s, v))
        return out

    def _semof(self, s):
        return self.sem[s] if isinstance(s, str) else self.dsem[s]

    def op(self, eng, fn, reads=(), writes=()):
        waits = self._deps(eng, reads, writes)
        self.cnt[eng] += 1
        v = self.cnt[eng]
        self.lists[eng].append((waits, fn, (self.sem[eng], 1)))
        for r in reads:
            r.rd[eng] = v
        for w in writes:
            w.lw = (eng, v)
            w.rd = {}

    def dma(self, q, out, in_, reads=(), writes=(), **kw):
        j = self.dcnt[q]
        self.dcnt[q] += 1
        src = (q, j % NDMA_SEM)
        val = 16 * (j // NDMA_SEM + 1)
        waits = self._deps(q, reads, writes)
        if j >= NDMA_SEM and self.waited[q].get(src, 0) < val - 16:
            self.waited[q][src] = val - 16
            waits.append((src, val - 16))

        def fn(e, out=out, in_=in_, kw=kw):
            return e.dma_start(out=out, in_=in_, **kw)
        self.lists[q].append((waits, fn, (self.dsem[src], 16)))
        for r in reads:
            r.rd[src] = val
        for w in writes:
            w.lw = (src, val)
            w.rd = {}

    def cc(self, fn, reads=(), writes=()):
        if ("cc", 0) not in self.dsem:
            self.dsem[("cc", 0)] = self._stack.enter_context(self.nc.semaphore("cc"))
            self.ccn = 0
        waits = self._deps("pool", reads, writes)
        for k in range(NDMA_SEM):
            n = (self.dcnt["pool"] - k + NDMA_SEM - 1) // NDMA_SEM
            if n > 0 and 16 * n > self.waited["pool"].get(("pool", k), 0):
                self.waited["pool"][("pool", k)] = 16 * n
                waits.append((("pool", k), 16 * n))
        self.ccn += 1
        src, val = ("cc", 0), self.ccn
        self.lists["pool"].append((waits, fn, (self.dsem[src], 1)))
        self.lists["pool"].append(([(src, val)], None, None))
        self.waited["pool"][src] = val
        for r in reads:
            r.rd[src] = val
        for w in writes:
            w.lw = (src, val)
            w.rd = {}

    def barrier(self):
        for e in ENGS:
            waits = []
            for s in ENGS:
                v = self.cnt[s]
                if s != e and v > self.waited[e].get(s, 0):
                    self.waited[e][s] = v
                    waits.append((s, v))
            for q in ("sp", "act", "pool"):
                for k in range(NDMA_SEM):
                    n = (self.dcnt[q] - k + NDMA_SEM - 1) // NDMA_SEM
                    v = 16 * n
                    if n > 0 and v > self.waited[e].get((q, k), 0):
                        self.waited[e][(q, k)] = v
                        waits.append(((q, k), v))
            if waits:
                self.lists[e].append((waits, None, None))

    def emit(self):
        nc = self.nc
        handles = {"pe": "tensor", "act": "scalar", "dve": "vector", "pool": "gpsimd", "sp": "sync"}
        with nc.Block() as block:
            for e in ENGS:
                def body(eng, lst=self.lists[e]):
                    for waits, fn, inc in lst:
                        for s, v in waits:
                            eng.wait_ge(self._semof(s), v)
                        if fn is not None:
                            fn(eng).then_inc(inc[0], inc[1])
                getattr(block, handles[e])(body)


class Arena:
    def __init__(self, ap, nwords):
        self.ap = ap
        self.n = nwords
        self.off = 0

    def mark(self):
        return self.off

    def reset(self, m):
        self.off = m

    def alloc(self, shape, dtype):
        nel = int(np.prod(shape))
        bpe = 2 if dtype == BF16 else 4
        nw = (nel * bpe + 3) // 4
        nw = (nw + 7) // 8 * 8
        assert self.off + nw <= self.n, f"SBUF arena overflow {self.off}+{nw}>{self.n}"
        v = self.ap[:, self.off:self.off + nw]
        self.off += nw
        if dtype != F32:
            v = v.bitcast(dtype)
        v = v[:, 0:nel]
        if len(shape) == 2:
            v = v.rearrange("p (a b) -> p a b", b=shape[1])
        elif len(shape) == 3:
            v = v.rearrange("p (a b c) -> p a b c", b=shape[1], c=shape[2])
        return v


class Prog:
    def __init__(self, phases, final_norm=True):
        self.phases = phases
        self.final_norm = final_norm
        self.nc = bass.Bass("TRN2", target_bir_lowering=False)
        self.din = {}

    def dram(self, name, shape, dtype=F32):
        if name in self.din:
            return self.din[name]
        t = self.nc.dram_tensor(name, list(shape), dtype, kind="ExternalInput").ap()
        self.din[name] = t
        return t

    def mm(self, out, lhsT, rhs, start, stop, reads, writes):
        self.S.op("pe", lambda e: e.matmul(out, lhsT, rhs, start=start, stop=stop,
                                           skip_group_check=True), reads, writes)

    def tr(self, out, in_, reads, writes):
        idn = self.ident
        self.S.op("pe", lambda e: e.transpose(out, in_, idn), list(reads) + [self.Tc], writes)

    def tt(self, eng, out, in0, in1, op, reads, writes):
        self.S.op(eng, lambda e: e.tensor_tensor(out, in0, in1, op), reads, writes)

    def ts(self, eng, out, in0, s1, s2, op0, op1, reads, writes):
        if s2 is None:
            self.S.op(eng, lambda e: e.tensor_scalar(out, in0, s1, None, op0), reads, writes)
        else:
            self.S.op(eng, lambda e: e.tensor_scalar(out, in0, s1, s2, op0, op1), reads, writes)

    def stt(self, out, in0, sc, in1, op0, op1, reads, writes):
        self.S.op("dve", lambda e: e.scalar_tensor_tensor(out, in0, sc, in1, op0, op1), reads, writes)

    def act(self, out, in_, func, reads, writes, bias=None, scale=None, accum_out=None):
        kw = {}
        if bias is not None:
            kw["bias"] = bias
        if scale is not None:
            kw["scale"] = scale
        if accum_out is not None:
            kw["accum_out"] = accum_out
        self.S.op("act", lambda e: e.activation(out, in_, func, **kw), reads, writes)

    def cp(self, eng, out, in_, reads, writes):
        if eng == "act":
            self.S.op("act", lambda e: e.copy(out, in_), reads, writes)
        else:
            self.S.op(eng, lambda e: e.tensor_copy(out, in_), reads, writes)

    def build(self):
        nc = self.nc
        with ExitStack() as st:
            self.st = st
            self.S = S = Sched(nc, st)
            NW = 52992
            arena_t = st.enter_context(nc.sbuf_tensor("arena", [128, NW], F32))
            self.A = A = Arena(arena_t, NW)
            self.ps = [st.enter_context(nc.psum_tensor(f"ps{k}", [128, 512], F32)) for k in range(8)]
            self.TP = [T(f"ps{k}") for k in range(8)]
            self.Tc = T("consts")

            xloc = self.dram("xloc", [TL, D])
            self.y = nc.dram_tensor("y", [TL, D], F32, kind="ExternalOutput").ap()
            ident_d = self.dram("ident", [128, 128])
            cT_d = self.dram("cT", [128, 8])
            nm_d = self.dram("nmT", [128, 2, 8])
            nf_d = self.dram("nfT", [128, 2, 8])
            nfin_d = self.dram("nfinT", [128, 8])
            adaw_d = self.dram("adaw", [2, 48, 128, 8, 128])
            adab_d = self.dram("adab", [128, 2, 48])

            self.ident = A.alloc([128], F32)
            self.ones = A.alloc([128], F32)
            self.onesb = A.alloc([128], BF16)
            self.epsb = A.alloc([1], F32)
            self.xT = A.alloc([8, TL], F32)
            self.Tx = [T(f"x{g}") for g in range(TL // 128)]
            self.modT = A.alloc([2, 48], F32)
            self.nm = A.alloc([2, 8], F32)
            self.nf = A.alloc([2, 8], F32)
            self.nfin = A.alloc([8], F32)
            self.coefA = A.alloc([2, 2, 8], F32)
            cTt = A.alloc([8], F32)
            adab = A.alloc([2, 48], F32)

            S.dma("sp", self.ident, ident_d, writes=[self.Tc])
            S.op("dve", lambda e: e.memset(self.ones, 1.0), writes=[self.Tc])
            S.op("dve", lambda e: e.memset(self.onesb, 1.0), writes=[self.Tc])
            S.op("dve", lambda e: e.memset(self.epsb, EPS), writes=[self.Tc])
            Tm = T("mod")
            S.dma("sp", cTt, cT_d, writes=[Tm])
            S.dma("sp", self.nm, nm_d, writes=[Tm])
            S.dma("sp", self.nf, nf_d, writes=[Tm])
            S.dma("sp", self.nfin, nfin_d, writes=[Tm])
            S.dma("sp", adab, adab_d, writes=[Tm])
            self.act(cTt, cTt, AF.Silu, [Tm], [Tm])

            m0 = A.mark()
            wbuf = [A.alloc([8, 128], F32) for _ in range(3)]
            Tw = [T(f"adaw{i}") for i in range(3)]
            qs = ("sp", "act")
            for l in range(2):
                for fc in range(48):
                    i = (l * 48 + fc) % 3
                    S.dma(qs[fc % 2], wbuf[i], adaw_d[l, fc], writes=[Tw[i]])
                    for dc in range(8):
                        self.mm(self.ps[0][:, fc:fc + 1], wbuf[i][:, dc, :], cTt[:, dc:dc + 1],
                                dc == 0, dc == 7, [Tw[i], Tm], [self.TP[0]])
                self.tt("dve", self.modT[:, l, :], self.ps[0][:, 0:48], adab[:, l, :], ALU.add,
                        [self.TP[0], Tm], [Tm])
            for l in range(2):
                for sub, gn in ((0, self.nm), (1, self.nf)):
                    sc = self.modT[:, l, (3 * sub + 1) * 8:(3 * sub + 2) * 8]
                    self.stt(self.coefA[:, l, sub, :], sc, 1.0, gn[:, l, :], ALU.add, ALU.mult, [Tm], [Tm])
            self.Tm = Tm
            S.barrier()
            A.reset(m0)

            m0 = A.mark()
            xin = [A.alloc([D], F32) for _ in range(2)]
            Txin = [T("xin0"), T("xin1")]
            for tl in range(TL // 128):
                b = tl % 2
                S.dma(qs[b], xin[b], xloc[tl * 128:(tl + 1) * 128, :], writes=[Txin[b]])
                for half in range(2):
                    pk = 2 * b + half
                    for q4 in range(4):
                        dc = half * 4 + q4
                        self.tr(self.ps[pk][:, q4 * 128:(q4 + 1) * 128], xin[b][:, dc * 128:(dc + 1) * 128],
                                [Txin[b]], [self.TP[pk]])
                    self.cp("act" if half else "dve",
                            self.xT[:, half * 4:(half + 1) * 4, tl * 128:(tl + 1) * 128],
                            self.ps[pk][:, :].rearrange("p (a b) -> p a b", b=128),
                            [self.TP[pk]], [self.Tx[tl]])
            S.barrier()
            A.reset(m0)

            for ph in self.phases:
                m0 = A.mark()
                if ph == "ret":
                    self.phase_ret()
                elif ph == "peer0":
                    self.phase_peer(0)
                elif ph == "sgu":
                    self.phase_sgu()
                elif ph == "peer1":
                    self.phase_peer(1)
                S.barrier()
                A.reset(m0)

            self.phase_final()
            S.emit()
        return nc

    def norm_mod(self, src, Tsrc, n, coef, shift, dst, Tdst, scr):
        S = self.S
        sq, Tsq, rstd, Trs, tmp, Ttmp, pk = scr
        for dc in range(8):
            i = dc % 2
            self.act(sq[i][:, :n], src[:, dc, :], AF.Square, Tsrc, [Tsq[i]])
            self.mm(self.ps[pk][:, :n], self.ones, sq[i][:, :n], dc == 0, dc == 7,
                    [Tsq[i], self.Tc], [self.TP[pk]])
        self.act(rstd[:, :n], self.ps[pk][:, :n], AF.Sqrt, [self.TP[pk], self.Tc], [Trs],
                 bias=self.epsb[:, 0:1], scale=1.0 / D)
        S.op("dve", lambda e: e.reciprocal(rstd[:, :n], rstd[:, :n]), [Trs], [Trs])
        for dc in range(8):
            i = dc % 2
            self.tt("dve", tmp[i][:, :n], src[:, dc, :], rstd[:, :n], ALU.mult, list(Tsrc) + [Trs], [Ttmp[i]])
            if shift is None:
                self.act(dst[:, dc, :], tmp[i][:, :n], AF.Copy, [Ttmp[i], self.Tm], Tdst,
                         scale=coef[:, dc:dc + 1])
            else:
                self.act(dst[:, dc, :], tmp[i][:, :n], AF.Identity, [Ttmp[i], self.Tm], Tdst,
                         bias=shift[:, dc:dc + 1], scale=coef[:, dc:dc + 1])

    def norm_scratch(self, pk, n=512):
        A = self.A
        sq = [A.alloc([n], F32) for _ in range(2)]
        rstd = A.alloc([n], F32)
        tmp = [A.alloc([n], F32) for _ in range(2)]
        return (sq, [T("sq0"), T("sq1")], rstd, T("rstd"), tmp, [T("tmp0"), T("tmp1")], pk)

    def phase_final(self):
        S, A = self.S, self.A
        m0 = A.mark()
        scr = self.norm_scratch(0)
        xn = [A.alloc([8, 512], F32) for _ in range(1)]
        Txn = T("xn")
        ob = [A.alloc([D], F32) for _ in range(2)]
        Tob = [T("ob0"), T("ob1")]
        Tout = T("yout")
        for g in range(TL // 512):
            src = self.xT[:, :, g * 512:(g + 1) * 512]
            Tsrc = self.Tx[g * 4:(g + 1) * 4]
            if self.final_norm:
                self.norm_mod(src, Tsrc, 512, self.nfin, None, xn[0], [Txn], scr)
                s2, Ts2 = xn[0], [Txn]
            else:
                s2, Ts2 = src, Tsrc
            for tt in range(4):
                b = tt % 2
                for half in range(2):
                    pk = 2 + 2 * b + half
                    for q4 in range(4):
                        dc = half * 4 + q4
                        self.tr(self.ps[pk][:, q4 * 128:(q4 + 1) * 128], s2[:, dc, tt * 128:(tt + 1) * 128],
                                Ts2, [self.TP[pk]])
                    self.cp("act" if half else "dve", ob[b][:, half * 512:(half + 1) * 512],
                            self.ps[pk][:, :], [self.TP[pk]], [Tob[b]])
                r0 = g * 512 + tt * 128
                S.dma("sp" if b else "act", self.y[r0:r0 + 128, :], ob[b], reads=[Tob[b]], writes=[Tout])
        waits = S._deps("sp", [Tout], ())
        S.lists["sp"].append((waits, None, None))
        A.reset(m0)


def _consts():
    c = {}
    c["ident"] = np.eye(128, dtype=np.float32)
    half = 128
    c["invf"] = (1.0 / (np.float32(10000.0) ** (np.arange(half, dtype=np.float32) / np.float32(half)))
                 ).astype(np.float32).reshape(128, 1)
    H = 4
    lg = np.log(1.0 - 2.0 ** (-5.0 - np.arange(H, dtype=np.float64)))
    idx = np.arange(128, dtype=np.float64)
    ch = (np.arange(128) // 64)
    dect = np.zeros((128, H, 128), np.float64)
    for h in range(H):
        i = idx[None, :]
        j = idx[:, None]
        same = (ch[None, :] == ch[:, None])
        earlier = (ch[:, None] < ch[None, :])
        w = np.where(same, np.exp(lg[h] * np.abs(i - j)), np.where(earlier, np.exp(lg[h] * (i - j)), 0.0))
        dect[:, h, :] = w / 16.0
    c["dect"] = dect.astype(np.float32)
    xi = np.exp(lg[:, None] * (idx[None, :] + 1.0))
    c["xit"] = np.broadcast_to(xi[None], (128, H, 128)).astype(np.float32).copy()
    ze = np.exp(lg[None, :] * (127.0 - idx[:, None])) / 16.0
    c["zet"] = ze.astype(np.float32)
    c["gam128"] = [float(np.exp(lg[h] * 128.0)) for h in range(H)]
    c["iota"] = np.broadcast_to(np.arange(128, dtype=np.float32)[None], (128, 128)).copy()
    pc = np.arange(128) // 64
    c["sgumask"] = (pc[None, :] <= pc[:, None]).astype(np.float32)
    return c


_CONSTS = _consts()


def _host_layout(inp, names):
    f = np.float32
    x = np.asarray(inp["x"], f)
    pos = np.asarray(inp["positions"], np.int32)
    shared = {}

    def need(n):
        return n in names
    for k in ("ident", "invf", "dect", "xit", "zet", "iota", "sgumask"):
        if need(k):
            shared[k] = _CONSTS[k]
    if need("nmT"):
        shared["nmT"] = np.ascontiguousarray(np.asarray(inp["norm_mix"], f).reshape(2, 8, 128).transpose(2, 0, 1))
        shared["nfT"] = np.ascontiguousarray(np.asarray(inp["norm_ffn"], f).reshape(2, 8, 128).transpose(2, 0, 1))
        shared["nfinT"] = np.ascontiguousarray(np.asarray(inp["norm_final"], f).reshape(8, 128).T)
        aw = np.asarray(inp["ada_w"], f).reshape(2, 8, 128, 48, 128)
        shared["adaw"] = np.ascontiguousarray(aw.transpose(0, 3, 2, 1, 4))
        shared["adab"] = np.ascontiguousarray(np.asarray(inp["ada_b"], f).reshape(2, 48, 128).transpose(2, 0, 1))
    if need("retwin"):
        w = np.asarray(inp["ret_w_in"], f)[0].reshape(8, 128, 6144)
        parts = []
        for h in range(4):
            cols = np.concatenate([np.arange(h * 256, (h + 1) * 256), 1024 + np.arange(h * 256, (h + 1) * 256),
                                   2048 + np.arange(h * 512, (h + 1) * 512), 4096 + np.arange(h * 512, (h + 1) * 512)])
            parts.append(w[:, :, cols].transpose(1, 0, 2))
        shared["retwin"] = np.ascontiguousarray(np.stack(parts))
        wo = np.asarray(inp["ret_w_out"], f)[0].reshape(4, 4, 128, 1024)
        shared["retwo"] = np.ascontiguousarray(wo.transpose(0, 2, 1, 3))
    if need("sguwu"):
        w = np.asarray(inp["sgu_w_in"], f)[0].reshape(8, 128, 6144)
        shared["sguwu"] = np.ascontiguousarray(w[:, :, :3072].reshape(8, 128, 24, 128).transpose(2, 1, 0, 3))
        shared["sguwv"] = np.ascontiguousarray(w[:, :, 3072:].reshape(8, 128, 6, 512).transpose(2, 1, 0, 3))
        b = np.asarray(inp["sgu_b_in"], f)[0]
        shared["sgubu"] = np.ascontiguousarray(b[:3072].reshape(24, 128).T)
        shared["sgubv"] = np.ascontiguousarray(b[3072:].reshape(1, 3072))
        shared["sgulng"] = np.ascontiguousarray(np.asarray(inp["sgu_ln_g"], f)[0].reshape(24, 128).T)
        lb = np.asarray(inp["sgu_ln_b"], f)[0]
        shared["sgulnb2"] = np.ascontiguousarray(np.stack([lb, np.ones_like(lb)]))
        shared["sguws"] = np.ascontiguousarray(np.asarray(inp["sgu_w_s"], f)[0].transpose(1, 0, 2))
        shared["sgubs"] = np.ascontiguousarray(np.asarray(inp["sgu_b_s"], f)[0].reshape(1, 8, 128))
        shared["sguwo"] = np.ascontiguousarray(np.asarray(inp["sgu_w_out"], f)[0].reshape(24, 128, 1024))
    if need("peerq"):
        wq = np.asarray(inp["peer_w_query"], f).reshape(2, 8, 128, 16, 128)
        shared["peerq"] = np.ascontiguousarray(wq.transpose(0, 3, 2, 1, 4))
        sk = np.asarray(inp["peer_sub_keys"], f).reshape(2, 16, 128, 128)
        shared["peerk"] = np.ascontiguousarray(sk.transpose(0, 3, 1, 2))
        u = np.asarray(inp["peer_u"], f).reshape(2, 128, 128, 8, 128)
        shared["peeru"] = np.ascontiguousarray(u.transpose(0, 1, 4, 3, 2))
        shared["peerv"] = np.ascontiguousarray(np.asarray(inp["peer_v"], f).reshape(2, 128, 128, 1024))
    maps = []
    for c in range(8):
        b, s = c // 4, c % 4
        m = dict(shared)
        m["xloc"] = np.ascontiguousarray(x[b, s * TL:(s + 1) * TL])
        m["cT"] = np.ascontiguousarray(np.asarray(inp["c"], f)[b].reshape(8, 128).T)
        if need("wst"):
            lg = np.log(1.0 - 2.0 ** (-5.0 - np.arange(4, dtype=np.float64)))
            w = np.zeros((128, 32), f)
            for c2 in range(8):
                b2, s2 = c2 // 4, c2 % 4
                if b2 == b and s2 < s:
                    for h in range(4):
                        w[:, c2 * 4 + h] = np.exp(lg[h] * TL * (s - s2 - 1))
            m["wst"] = w
            m["posloc"] = np.ascontiguousarray(pos[b, s * TL:(s + 1) * TL].reshape(1, TL))
        if need("xprev"):
            xp = np.zeros((NPREV, D), f)
            pp = np.zeros((1, NPREV), np.int32)
            n = s * TL
            if n:
                xp[NPREV - n:] = x[b, :n]
                pp[0, NPREV - n:] = pos[b, :n]
            m["xprev"] = xp
            m["posprev"] = pp
            m["posloc"] = np.ascontiguousarray(pos[b, s * TL:(s + 1) * TL].reshape(1, TL))
            val = np.zeros((128, NPREV // 128), f)
            val[:, (NPREV - n) // 128:] = 1.0
            m["valid"] = val
        maps.append(m)
    return maps


_PROG_CACHE = {}


def _run(inp, phases, final_norm=True, xoverride=None):
    key = (tuple(phases), final_norm)
    if key not in _PROG_CACHE:
        p = Prog(phases, final_norm)
        p.build()
        _PROG_CACHE[key] = p
    p = _PROG_CACHE[key]
    maps = _host_layout(inp, set(p.din.keys()))
    if xoverride is not None:
        for c in range(8):
            maps[c]["xloc"] = np.ascontiguousarray(xoverride[c])
    maps = [{k: m[k] for k in p.din.keys()} for m in maps]
    res = run_bass_kernel_spmd(p.nc, maps, core_ids=list(range(8)))
    return [np.asarray(r["y"]) for r in res.results]


def kernel(**inputs):
    ys = _run(inputs, ["ret", "peer0", "sgu", "peer1"], True)
    out = np.stack(ys).reshape(2, 4 * TL, D)
    return out.astype(np.float32)


MAGIC = 12582912.0
INV2PI = 0.15915494309189535
C1 = 6.28125
C2 = 0.0019353071795864769


def _phase_ret(self):
    S, A, nc = self.S, self.A, self.nc
    posloc = self.dram("posloc", [1, TL], I32)
    xprev = self.dram("xprev", [NPREV, D])
    posprev = self.dram("posprev", [1, NPREV], I32)
    valid_d = self.dram("valid", [128, NPREV // 128])
    invf_d = self.dram("invf", [128, 1])
    dect_d = self.dram("dect", [128, 4, 128])
    xit_d = self.dram("xit", [128, 4, 128])
    zet_d = self.dram("zet", [128, 4])
    win_d = self.dram("retwin", [4, 128, 8, 1536])
    wo_d = self.dram("retwo", [4, 128, 4, 1024])
    gam = _CONSTS["gam128"]
    ps, TP = self.ps, self.TP
    L = 0
    sh1 = self.modT[:, L, 0:8]
    g1 = self.modT[:, L, 16:24]
    coef = self.coefA[:, L, 0, :]

    Tt = T("rtab")
    invf = A.alloc([1], F32)
    dect = A.alloc([4, 128], F32)
    xit = A.alloc([4, 128], F32)
    zet = A.alloc([4], F32)
    valid = A.alloc([NPREV // 128], F32)
    halfpi = A.alloc([1], F32)
    for dst, src in ((invf, invf_d), (dect, dect_d), (xit, xit_d), (zet, zet_d), (valid, valid_d)):
        S.dma("sp", dst, src, writes=[Tt])
    S.op("dve", lambda e: e.memset(halfpi, math.pi / 2), writes=[Tt])

    S4 = A.alloc([4, 2, 512], F32)
    TS4 = [T(f"S4_{h}") for h in range(4)]
    S.op("dve", lambda e: e.memset(S4, 0.0), writes=TS4)
    m0 = A.mark()
    Wkv = A.alloc([8, 4, 768], BF16)
    TWkv = T("Wkv")
    for h in range(4):
        for dc in range(8):
            S.dma("pool", Wkv[:, dc, h, :], win_d[h][:, dc, 256:1024], writes=[TWkv])
    if "sgu" in self.phases:
        self.issue_sgu_conv()
    for L_ in (0, 1):
        if ("peer%d" % L_) in self.phases:
            self.issue_conv(L_)
    pscr = self.norm_scratch(7, 128)
    xin = [A.alloc([D], F32) for _ in range(2)]
    Txin = [T("xin0"), T("xin1")]
    xpT = [A.alloc([8, 128], F32) for _ in range(2)]
    Txp = [T("xp0"), T("xp1")]
    hp = [A.alloc([8, 128], BF16) for _ in range(2)]
    Thp = [T("hp0"), T("hp1")]
    pi32p = A.alloc([128], I32)
    angp = A.alloc([128], F32)
    nnp = A.alloc([128], F32)
    csp = [A.alloc([2, 128], F32) for _ in range(2)]
    Tpp = T("posp")
    Tcsp = [T("csp0"), T("csp1")]
    prt = [[A.alloc([128], F32) for _ in range(2)] for _ in range(4)]
    Tprt = [[T(f"prt{i}{j}") for j in range(2)] for i in range(4)]
    pkT = [A.alloc([2, 128], F32) for _ in range(2)]
    Tpk = [T("pk0"), T("pk1")]
    pvb = [A.alloc([512], BF16) for _ in range(2)]
    Tpv = [T("pv0"), T("pv1")]
    pkz = [A.alloc([256], BF16) for _ in range(2)]
    Tpkz = [T("pkz0"), T("pkz1")]
    print("ret prepass arena words used", A.off, "of", A.n)
    def pre_work(ti):
        b = ti % 2
        S.dma("sp" if b else "act", xin[b], xprev[ti * 128:(ti + 1) * 128, :], writes=[Txin[b]])
        for half in range(2):
            pk = 5 + half
            for q4 in range(4):
                dc = half * 4 + q4
                self.tr(ps[pk][:, q4 * 128:(q4 + 1) * 128], xin[b][:, dc * 128:(dc + 1) * 128], [Txin[b]], [TP[pk]])
            self.cp("act" if half else "dve", xpT[b][:, half * 4:(half + 1) * 4, :],
                    ps[pk][:, :].rearrange("p (a b) -> p a b", b=128), [TP[pk]], [Txp[b]])
        self.norm_mod(xpT[b], [Txp[b]], 128, coef, sh1, hp[b], [Thp[b]], pscr)
        S.dma("sp", pi32p, posprev[0:1, ti * 128:(ti + 1) * 128].partition_broadcast(128), writes=[Tpp])
        self.cp("dve", angp, pi32p, [Tpp], [Tpp])
        self.ts("dve", angp, angp, invf[:, 0:1], None, ALU.mult, None, [Tpp, Tt], [Tpp])
        self.ts("dve", nnp, angp, INV2PI, MAGIC, ALU.mult, ALU.add, [Tpp], [Tpp])
        self.ts("dve", nnp, nnp, -MAGIC, None, ALU.add, None, [Tpp], [Tpp])
        self.stt(angp, nnp, -C1, angp, ALU.mult, ALU.add, [Tpp], [Tpp])
        self.stt(angp, nnp, -C2, angp, ALU.mult, ALU.add, [Tpp], [Tpp])
        self.ts("dve", angp, angp, 3.14159, -3.14159, ALU.min, ALU.max, [Tpp], [Tpp])
        self.act(csp[b][:, 1, :], angp, AF.Sin, [Tpp], [Tcsp[b]])
        self.act(nnp, angp, AF.Abs, [Tpp], [Tpp])
        self.act(csp[b][:, 0, :], nnp, AF.Sin, [Tpp, Tt], [Tcsp[b]], bias=halfpi[:, 0:1], scale=-1.0)

    def pre_proj(ti, h):
        b = ti % 2
        u = (ti * 4 + h) % 2
        pa = 0 if u == 0 else 2
        pv = 1 if u == 0 else 4
        for c in range(2):
            for dc in range(8):
                self.mm(ps[pa][:, c * 128:(c + 1) * 128], Wkv[:, dc, h, c * 128:(c + 1) * 128], hp[b][:, dc, :],
                        dc == 0, dc == 7, [TWkv, Thp[b]], [TP[pa]])
        for dc in range(8):
            self.mm(ps[pv][:, :], hp[b][:, dc, :], Wkv[:, dc, h, 256:768], dc == 0, dc == 7,
                    [TWkv, Thp[b]], [TP[pv]])

    def pre_rest(ti, h):
        b = ti % 2
        u = (ti * 4 + h) % 2
        pa = 0 if u == 0 else 2
        pv = 1 if u == 0 else 4
        cosP, sinP = csp[b][:, 0, :], csp[b][:, 1, :]
        x1 = ps[pa][:, 0:128]
        x2 = ps[pa][:, 128:256]
        self.tt("dve", prt[0][u], x1, cosP, ALU.mult, [TP[pa], Tcsp[b]], [Tprt[0][u]])
        self.tt("dve", prt[1][u], x2, sinP, ALU.mult, [TP[pa], Tcsp[b]], [Tprt[1][u]])
        self.tt("pool", pkT[u][:, 0, :], prt[0][u], prt[1][u], ALU.subtract, [Tprt[0][u], Tprt[1][u]], [Tpk[u]])
        self.tt("dve", prt[2][u], x1, sinP, ALU.mult, [TP[pa], Tcsp[b]], [Tprt[2][u]])
        self.tt("dve", prt[3][u], x2, cosP, ALU.mult, [TP[pa], Tcsp[b]], [Tprt[3][u]])
        self.tt("pool", pkT[u][:, 1, :], prt[2][u], prt[3][u], ALU.add, [Tprt[2][u], Tprt[3][u]], [Tpk[u]])
        self.cp("act", pvb[u], ps[pv][:, :], [TP[pv]], [Tpv[u]])
        for c in range(2):
            self.tr(ps[3][:, c * 128:(c + 1) * 128], pkT[u][:, c, :], [Tpk[u]], [TP[3]])
        self.ts("dve", pkz[u], ps[3][:, 0:256], zet[:, h:h + 1], valid[:, ti:ti + 1], ALU.mult, ALU.mult,
                [TP[3], Tt], [Tpkz[u]])
        for c in range(2):
            pd = 5 + c
            self.mm(ps[pd][:, :], pkz[u][:, c * 128:(c + 1) * 128], pvb[u], True, True, [Tpkz[u], Tpv[u]], [TP[pd]])
            self.stt(S4[:, h, c, :], S4[:, h, c, :], gam[h], ps[pd][:, :], ALU.mult, ALU.add,
                     [TS4[h], TP[pd]], [TS4[h]])

    bodies = [(ti, h) for ti in range(NPREV // 128) for h in range(4)]
    pre_work(0)
    pre_proj(*bodies[0])
    for n, (ti, h) in enumerate(bodies):
        if n + 1 < len(bodies):
            ti2, h2 = bodies[n + 1]
            if h2 == 0:
                pre_work(ti2)
            pre_proj(ti2, h2)
        pre_rest(ti, h)
    S.barrier()
    A.reset(m0)

    hnT = A.alloc([8, TL], BF16)
    Thn = [T(f"hn{g}") for g in range(4)]
    cosL = A.alloc([TL], F32)
    sinL = A.alloc([TL], F32)
    Tcs = T("cs")
    m1 = A.mark()
    scr = self.norm_scratch(7)
    for g in range(4):
        self.norm_mod(self.xT[:, :, g * 512:(g + 1) * 512], self.Tx[g * 4:(g + 1) * 4], 512, coef, sh1,
                      hnT[:, :, g * 512:(g + 1) * 512], [Thn[g]], scr)

    pi32 = A.alloc([512], I32)
    ang = A.alloc([512], F32)
    nn = A.alloc([512], F32)
    Tpos = T("pos")
    for g in range(4):
        sl = slice(g * 512, (g + 1) * 512)
        S.dma("sp", pi32, posloc[0:1, sl].partition_broadcast(128), writes=[Tpos])
        self.cp("dve", ang, pi32, [Tpos], [Tpos])
        self.ts("dve", ang, ang, invf[:, 0:1], None, ALU.mult, None, [Tpos, Tt], [Tpos])
        self.ts("dve", nn, ang, INV2PI, MAGIC, ALU.mult, ALU.add, [Tpos], [Tpos])
        self.ts("dve", nn, nn, -MAGIC, None, ALU.add, None, [Tpos], [Tpos])
        self.stt(ang, nn, -C1, ang, ALU.mult, ALU.add, [Tpos], [Tpos])
        self.stt(ang, nn, -C2, ang, ALU.mult, ALU.add, [Tpos], [Tpos])
        self.ts("dve", ang, ang, 3.14159, -3.14159, ALU.min, ALU.max, [Tpos], [Tpos])
        self.act(sinL[:, sl], ang, AF.Sin, [Tpos], [Tcs])
        self.act(nn, ang, AF.Abs, [Tpos], [Tpos])
        self.act(cosL[:, sl], nn, AF.Sin, [Tpos, Tt], [Tcs], bias=halfpi[:, 0:1], scale=-1.0)
    S.barrier()
    A.reset(m1)

    Wi = A.alloc([8, 1536], BF16)
    Wo = A.alloc([4, 1024], BF16)
    TWi, TWo = T("Wi"), T("Wo")
    S32 = A.alloc([2, 512], F32)
    Sbf = A.alloc([2, 512], BF16)
    TS32, TSbf = T("S32"), T("Sbf")
    NBF = 2

    def dbl(shape, dt, name):
        return [A.alloc(shape, dt) for _ in range(NBF)], [T(f"{name}{i}") for i in range(NBF)]
    rt, Trt = [], []
    for i in range(4):
        a_, t_ = dbl([128], F32, f"rt{i}_")
        rt.append(a_)
        Trt.append(t_)
    kTr, Tk = dbl([2, 128], F32, "kTr")
    kTb, Tkb = dbl([2, 128], BF16, "kTb")
    qTr, Tq = dbl([2, 128], BF16, "qTr")
    qx, Tqx = dbl([2, 128], BF16, "qx")
    vb, Tv = dbl([512], BF16, "vb")
    gs, Tgs = dbl([512], F32, "gs")
    SD, TSD = dbl([128], BF16, "SD")
    kz, Tkz = dbl([256], BF16, "kz")
    junk = A.alloc([512], F32)
    ss, Tss = dbl([1], F32, "ss")
    gy, Tgy = dbl([512], F32, "gy")
    gyT, TgyT = dbl([4, 128], BF16, "gyT")
    print("ret arena words used", A.off, "of", A.n)

    def tile_body(h, tl, full):
        b = tl % NBF
        hsrc, Th = hnT[:, :, tl * 128:(tl + 1) * 128], [Thn[tl // 4]]
        cosT = cosL[:, tl * 128:(tl + 1) * 128]
        sinT = sinL[:, tl * 128:(tl + 1) * 128]
        pa = 0 if b == 0 else 2
        for c in range(2):
            for dc in range(8):
                self.mm(ps[pa][:, c * 128:(c + 1) * 128], Wi[:, dc, 256 + c * 128:256 + (c + 1) * 128],
                        hsrc[:, dc, :], dc == 0, dc == 7, [TWi] + Th, [TP[pa]])
        if full:
            for c in range(2):
                for dc in range(8):
                    self.mm(ps[pa][:, 256 + c * 128:256 + (c + 1) * 128], Wi[:, dc, c * 128:(c + 1) * 128],
                            hsrc[:, dc, :], dc == 0, dc == 7, [TWi] + Th, [TP[pa]])
        for dc in range(8):
            self.mm(ps[1][:, :], hsrc[:, dc, :], Wi[:, dc, 512:1024], dc == 0, dc == 7, [TWi] + Th, [TP[1]])

        def rotary(base, o1, o2, To):
            x1 = ps[pa][:, base:base + 128]
            x2 = ps[pa][:, base + 128:base + 256]
            self.tt("dve", rt[0][b], x1, cosT, ALU.mult, [TP[pa], Tcs], [Trt[0][b]])
            self.tt("dve", rt[1][b], x2, sinT, ALU.mult, [TP[pa], Tcs], [Trt[1][b]])
            self.tt("dve", o1, rt[0][b], rt[1][b], ALU.subtract, [Trt[0][b], Trt[1][b]], [To])
            self.tt("dve", rt[2][b], x1, sinT, ALU.mult, [TP[pa], Tcs], [Trt[2][b]])
            self.tt("dve", rt[3][b], x2, cosT, ALU.mult, [TP[pa], Tcs], [Trt[3][b]])
            self.tt("dve", o2, rt[2][b], rt[3][b], ALU.add, [Trt[2][b], Trt[3][b]], [To])
        rotary(0, kTr[b][:, 0, :], kTr[b][:, 1, :], Tk[b])
        self.cp("act", vb[b], ps[1][:, :], [TP[1]], [Tv[b]])
        if full:
            for dc in range(8):
                self.mm(ps[1][:, :], hsrc[:, dc, :], Wi[:, dc, 1024:1536], dc == 0, dc == 7,
                        [TWi] + Th, [TP[1]])
            self.cp("act", kTb[b], kTr[b], [Tk[b]], [Tkb[b]])
            rotary(256, qTr[b][:, 0, :], qTr[b][:, 1, :], Tq[b])
            self.act(gs[b], ps[1][:, :], AF.Silu, [TP[1]], [Tgs[b]])
            for c in range(2):
                self.mm(ps[3][:, 0:128], kTb[b][:, c, :], qTr[b][:, c, :], c == 0, c == 1, [Tkb[b], Tq[b]], [TP[3]])
            self.tt("dve", SD[b], ps[3][:, 0:128], dect[:, h, :], ALU.mult, [TP[3], Tt], [TSD[b]])
            for c in range(2):
                self.tt("dve", qx[b][:, c, :], qTr[b][:, c, :], xit[:, h, :], ALU.mult, [Tq[b], Tt], [Tqx[b]])
            self.mm(ps[4][:, :], SD[b], vb[b], True, False, [TSD[b], Tv[b]], [TP[4]])
            for c in range(2):
                self.mm(ps[4][:, :], qx[b][:, c, :], Sbf[:, c, :], False, c == 1, [Tqx[b], TSbf], [TP[4]])
            self.act(junk, ps[4][:, :], AF.Square, [TP[4]], [Tss[b]], accum_out=ss[b][:, 0:1])
            self.act(ss[b], ss[b], AF.Sqrt, [Tss[b], self.Tc], [Tss[b]], bias=self.epsb[:, 0:1], scale=1.0 / 512)
            S.op("dve", lambda e: e.reciprocal(ss[b], ss[b]), [Tss[b]], [Tss[b]])
            self.stt(gy[b], ps[4][:, :], ss[b][:, 0:1], gs[b], ALU.mult, ALU.mult, [TP[4], Tss[b], Tgs[b]], [Tgy[b]])
            for fc in range(4):
                self.tr(ps[7][:, fc * 128:(fc + 1) * 128], gy[b][:, fc * 128:(fc + 1) * 128], [Tgy[b]], [TP[7]])
            self.cp("act", gyT[b], ps[7][:, :].rearrange("p (a b) -> p a b", b=128), [TP[7]], [TgyT[b]])
        for c in range(2):
            self.tr(ps[3][:, 128 + c * 128:128 + (c + 1) * 128], kTr[b][:, c, :], [Tk[b]], [TP[3]])
        self.ts("dve", kz[b], ps[3][:, 128:384], zet[:, h:h + 1], None, ALU.mult, None, [TP[3], Tt], [Tkz[b]])
        for c in range(2):
            self.mm(ps[5 + c][:, :], kz[b][:, c * 128:(c + 1) * 128], vb[b], True, True, [Tkz[b], Tv[b]], [TP[5 + c]])
            self.stt(S32[:, c, :], S32[:, c, :], gam[h], ps[5 + c][:, :], ALU.mult, ALU.add,
                     [TS32, TP[5 + c]], [TS32])
        if full:
            self.cp("act", Sbf, S32, [TS32], [TSbf])
            for dc in range(8):
                pk = 5 + dc // 4
                for fc in range(4):
                    self.mm(ps[pk][:, (dc % 4) * 128:(dc % 4 + 1) * 128], Wo[:, fc, dc * 128:(dc + 1) * 128],
                            gyT[b][:, fc, :], fc == 0, fc == 3, [TWo, TgyT[b]], [TP[pk]])
            for dc in range(8):
                pk = 5 + dc // 4
                xs = self.xT[:, dc, tl * 128:(tl + 1) * 128]
                self.stt(xs, ps[pk][:, (dc % 4) * 128:(dc % 4 + 1) * 128], g1[:, dc:dc + 1], xs,
                         ALU.mult, ALU.add, [TP[pk], self.Tm, self.Tx[tl]], [self.Tx[tl]])

    for h in range(4):
        S.dma("pool", Wi, win_d[h], writes=[TWi], max_dma_last_dim=2048)
        S.dma("pool", Wo, wo_d[h], writes=[TWo], max_dma_last_dim=2048)
        self.cp("dve", S32, S4[:, h, :, :], [TS4[h]], [TS32])
        self.cp("act", Sbf, S32, [TS32], [TSbf])
        for tl in range(TL // 128):
            tile_body(h, tl, True)


Prog.phase_ret = _phase_ret


def _phase_peer(self, L):
    S, A, nc = self.S, self.A, self.nc
    ps, TP = self.ps, self.TP
    wq_d = self.dram("peerq", [2, 16, 128, 8, 128])
    kk_d = self.dram("peerk", [2, 128, 16, 128])
    u_d = self.dram("peeru", [2, 128, 128, 8, 128])
    v_d = self.dram("peerv", [2, 128, 128, 1024])
    iota_d = self.dram("iota", [128, 128])
    TB = 256
    NB = TL // TB
    self.issue_conv(L)
    ubf, vbf, Tcv = self.conv[L]
    sh2 = self.modT[:, L, 24:32]
    g2 = self.modT[:, L, 40:48]
    coef = self.coefA[:, L, 1, :]

    Tt = T("ptab")
    keysT = A.alloc([16, 128], BF16)
    iota = A.alloc([128], F32)
    S.dma("pool", keysT, kk_d[L], writes=[Tt])
    S.dma("sp", iota, iota_d, writes=[Tt])
    Gt = A.alloc([TB, 128], BF16)
    TG = T("Gt")
    hn = A.alloc([8, TB], BF16)
    Thn = T("hn")
    m_scr = A.mark()
    qTP = A.alloc([16, TB], BF16)
    NS = 16
    Pb = [A.alloc([NS, 128], BF16) for _ in range(2)]
    TqP = T("qTP")
    TPh = [T("P0"), T("P1")]
    scr = self.norm_scratch(7, TB)
    wqb = [A.alloc([8, 128], BF16) for _ in range(2)]
    Twq = [T("wq0"), T("wq1")]
    sc = A.alloc([16, 128], F32)
    Tsc = T("sc")
    eq2 = sc.rearrange("p a b -> p (a b)").rearrange("p (h r k) -> p h r k", r=16, k=16)
    mr = A.alloc([256], F32)
    Tmr = T("mr")
    mr2 = [mr, A.alloc([256], F32)]
    Tmr2 = [Tmr, T("mrB")]
    Tsth = [T(f"st{i}") for i in range(16)]
    Tbh = [T(f"bh{i}") for i in range(8)]
    stop = A.alloc([16, 16], F32)
    itop = A.alloc([16, 16], U32)
    itopf = A.alloc([16, 16], F32)
    Tst = T("stop")
    cand = A.alloc([8, 256], F32)
    Tcd = T("cand")
    eq = cand.rearrange("p h (r k) -> p h r k", k=16)
    best = A.alloc([8, 16], F32)
    pos = A.alloc([8, 16], U32)
    k1u = A.alloc([8, 16], U32)
    k2u = A.alloc([8, 16], U32)
    k1f = A.alloc([8, 16], F32)
    k2f = A.alloc([8, 16], F32)
    ee = A.alloc([8, 16], F32)
    zz = A.alloc([8], F32)
    Tb = T("best")
    ijg = A.alloc([3, 128], F32)
    Tijg = T("ijg")
    ijgT = A.alloc([3, 128], F32)
    TijgT = T("ijgT")
    Qgb = [A.alloc([NS, 128], BF16) for _ in range(2)]
    TQb = [T("Qg0"), T("Qg1")]
    m_scr_end = A.mark()
    A.reset(m_scr)
    NBUF = 7
    ub = [A.alloc([2, 8, 128], BF16) for _ in range(NBUF)]
    vb = [A.alloc([2, 1024], BF16) for _ in range(NBUF)]
    Tub = [T(f"ub{i}") for i in range(NBUF)]
    Tvb = [T(f"vb{i}") for i in range(NBUF)]
    gel = [A.alloc([TB], BF16) for _ in range(3)]
    Ab = [A.alloc([TB], BF16) for _ in range(3)]
    print("peer arena words used", A.off, m_scr_end, "of", A.n)
    A.off = max(A.off, m_scr_end)
    Tgel = [T("gel0"), T("gel1"), T("gel2")]
    TAb = [T("Ab0"), T("Ab1"), T("Ab2")]
    stop4 = stop.rearrange("p (h two) k -> p h two k", two=2)
    itop4 = itopf.rearrange("p (h two) k -> p h two k", two=2)
    iota16 = iota[:, 0:16].unsqueeze(1).unsqueeze(1).to_broadcast([128, 8, 16, 16])

    for blk in range(NB):
        t0 = blk * TB
        Txb = self.Tx[2 * blk:2 * blk + 2]
        self.norm_mod(self.xT[:, :, t0:t0 + TB], Txb, TB, coef, sh2, hn, [Thn], scr)
        for fc in range(16):
            i = fc % 2
            S.dma("pool", wqb[i], wq_d[L, fc], writes=[Twq[i]])
            for dc in range(8):
                self.mm(ps[i][:, 0:TB], wqb[i][:, dc, :], hn[:, dc, :], dc == 0, dc == 7, [Twq[i], Thn], [TP[i]])
            self.cp("act" if i else "dve", qTP[:, fc, :], ps[i][:, 0:TB], [TP[i]], [TqP])
        for tt in range(2):
            for hp in range(16):
                pk = 2 + hp // 4
                self.mm(ps[pk][:, (hp % 4) * 128:(hp % 4 + 1) * 128], qTP[:, hp, tt * 128:(tt + 1) * 128],
                        keysT[:, hp, :], True, True, [TqP, Tt], [TP[pk]])
            for q4 in range(4):
                self.cp("act" if q4 % 2 else "dve", sc[:, 4 * q4:4 * q4 + 4, :],
                        ps[2 + q4][:, :].rearrange("p (a b) -> p a b", b=128), [TP[2 + q4]], [Tsc])
            V = S
            for hp0 in range(0, 16, 2):
                for step in range(5):
                    for m in range(2):
                        hp = hp0 + m
                        row = sc[:, hp, :]
                        mrm, Tm_, Th_ = mr2[m], Tmr2[m], Tsth[hp]
                        if step == 0:
                            V.op("dve", lambda e, hp=hp, row=row: e.max(stop[:, hp, 0:8], row), [Tsc], [Th_])
                        elif step == 1:
                            V.op("dve", lambda e, hp=hp, row=row: e.max_index(itop[:, hp, 0:8], stop[:, hp, 0:8], row),
                                 [Tsc, Th_], [Th_])
                        elif step == 2:
                            V.op("dve", lambda e, hp=hp, row=row, mrm=mrm: e.match_replace(mrm[:, 0:128], stop[:, hp, 0:8], row, NEG),
                                 [Tsc, Th_], [Tm_])
                        elif step == 3:
                            V.op("dve", lambda e, hp=hp, mrm=mrm: e.max(stop[:, hp, 8:16], mrm[:, 0:128]), [Tm_], [Th_])
                        else:
                            V.op("dve", lambda e, hp=hp, mrm=mrm: e.max_index(itop[:, hp, 8:16], stop[:, hp, 8:16], mrm[:, 0:128]),
                                 [Tm_, Th_], [Th_])
            self.cp("dve", itopf, itop, Tsth, [Tst])
            self.tt("dve", eq, stop4[:, :, 0, :].unsqueeze(3).to_broadcast([128, 8, 16, 16]),
                    stop4[:, :, 1, :].unsqueeze(2).to_broadcast([128, 8, 16, 16]), ALU.add, Tsth + [Tst], [Tcd])
            for h0 in range(0, 8, 2):
                for step in range(5):
                    for m in range(2):
                        h = h0 + m
                        row = cand[:, h, :]
                        mrm, Tm_, Th_ = mr2[m], Tmr2[m], Tbh[h]
                        if step == 0:
                            V.op("dve", lambda e, h=h, row=row: e.max(best[:, h, 0:8], row), [Tcd, Tb], [Th_])
                        elif step == 1:
                            V.op("dve", lambda e, h=h, row=row: e.max_index(pos[:, h, 0:8], best[:, h, 0:8], row),
                                 [Tcd, Th_], [Th_])
                        elif step == 2:
                            V.op("dve", lambda e, h=h, row=row, mrm=mrm: e.match_replace(mrm[:, 0:256], best[:, h, 0:8], row, NEG),
                                 [Tcd, Th_], [Tm_])
                        elif step == 3:
                            V.op("dve", lambda e, h=h, mrm=mrm: e.max(best[:, h, 8:16], mrm[:, 0:256]), [Tm_], [Th_])
                        else:
                            V.op("dve", lambda e, h=h, mrm=mrm: e.max_index(pos[:, h, 8:16], best[:, h, 8:16], mrm[:, 0:256]),
                                 [Tm_, Th_], [Th_])
            self.cp("dve", k2f, pos, Tbh, [Tb])
            gcf = ijg[:, 2, :].rearrange("p (h r) -> p h r", r=16)
            self.tt("dve", ee, best, best[:, :, 0:1].to_broadcast([128, 8, 16]), ALU.subtract, Tbh + [Tb], [Tb])
            self.act(ee, ee, AF.Exp, [Tb], [Tb])
            V.op("dve", lambda e: e.tensor_reduce(zz, ee, AX.X, ALU.add), [Tb], [Tb])
            V.op("dve", lambda e: e.reciprocal(zz, zz), [Tb], [Tb])
            self.tt("dve", gcf, ee, zz.unsqueeze(2).to_broadcast([128, 8, 16]), ALU.mult, [Tb], [Tijg])
            self.ts("dve", k1f, k2f, 0.0625, -0.46875, ALU.mult, ALU.add, [Tb], [Tb])
            self.ts("dve", k1f, k1f, MAGIC, None, ALU.add, None, [Tb], [Tb])
            self.ts("dve", k1f, k1f, -MAGIC, None, ALU.add, None, [Tb], [Tb])
            self.stt(k2f, k1f, -16.0, k2f, ALU.mult, ALU.add, [Tb], [Tb])
            for which, kf, eqb, Teq in ((0, k1f, eq, Tcd), (1, k2f, eq2, Tsc)):
                self.tt("dve", eqb, kf.unsqueeze(3).to_broadcast([128, 8, 16, 16]), iota16, ALU.is_equal,
                        [Tb, Tt], [Teq])
                self.tt("dve", eqb, eqb, itop4[:, :, which, :].unsqueeze(2).to_broadcast([128, 8, 16, 16]),
                        ALU.mult, [Teq, Tst], [Teq])
                dst = ijg[:, which, :].rearrange("p (h r) -> p h r", r=16)
                V.op("dve", lambda e, dst=dst, eqb=eqb: e.tensor_reduce(dst, eqb, AX.X, ALU.add), [Teq], [Tijg])
            for w in range(3):
                self.tr(ps[6][:, w * 128:(w + 1) * 128], ijg[:, w, :], [Tijg], [TP[6]])
            self.cp("act", ijgT, ps[6][:, 0:384].rearrange("p (a b) -> p a b", b=128), [TP[6]], [TijgT])
            for sub in range(128 // NS):
                tsl = slice(sub * NS, (sub + 1) * NS)
                iob = iota.unsqueeze(1).to_broadcast([128, NS, 128])
                P, Qg, TPs, TQ = Pb[sub % 2], Qgb[sub % 2], TPh[sub % 2], TQb[sub % 2]
                self.tt("dve", P, iob, ijgT[:, 0, tsl].unsqueeze(2).to_broadcast([128, NS, 128]), ALU.is_equal,
                        [Tt, TijgT], [TPs])
                self.tt("dve", Qg, iob, ijgT[:, 1, tsl].unsqueeze(2).to_broadcast([128, NS, 128]), ALU.is_equal,
                        [Tt, TijgT], [TQ])
                self.tt("pool", Qg, Qg, ijgT[:, 2, tsl].unsqueeze(2).to_broadcast([128, NS, 128]), ALU.mult,
                        [TQ, TijgT], [TQ])
                for t4 in range(NS // 4):
                    pk = t4 % 2
                    for q in range(4):
                        t = t4 * 4 + q
                        self.mm(ps[pk][:, q * 128:(q + 1) * 128], Qg[:, t, :], P[:, t, :], True, True,
                                [TQ, TPs], [TP[pk]])
                    tok = tt * 128 + sub * NS + t4 * 4
                    self.cp("act", Gt[:, tok:tok + 4, :],
                            ps[pk][:, :].rearrange("p (t i) -> p t i", i=128), [TP[pk]], [TG])
        S.barrier()
        dq = ("sp", "act", "pool")
        for i in range(128):
            k = (i // 2) % NBUF
            if i % 2 == 0:
                S.dma(dq[(i // 2) % 3], ub[k], ubf[i:i + 2].rearrange("i p f -> p i f"),
                      reads=[Tcv[i // 8]], writes=[Tub[k]])
                S.dma(dq[(i // 2 + 1) % 3], vb[k], vbf[i:i + 2].rearrange("i p f -> p i f"),
                      reads=[Tcv[i // 8]], writes=[Tvb[k]])
            sp = 4 + i % 3
            for dc in range(8):
                self.mm(ps[sp][:, 0:TB], ub[k][:, i % 2, dc, :], hn[:, dc, :], dc == 0, dc == 7,
                        [Tub[k], Thn], [TP[sp]])
            g = i % 3
            self.act(gel[g], ps[sp][:, 0:TB], AF.Gelu_apprx_tanh, [TP[sp]], [Tgel[g]])
            self.tt("dve", Ab[g], gel[g], Gt[:, :, i], ALU.mult, [Tgel[g], TG], [TAb[g]])

            def vmm(i):
                k = (i // 2) % NBUF
                g = i % 3
                for dc in range(8):
                    pk = dc // 2
                    self.mm(ps[pk][:, (dc % 2) * TB:(dc % 2 + 1) * TB], vb[k][:, i % 2, dc * 128:(dc + 1) * 128],
                            Ab[g], i == 0 and dc % 2 == 0, i == 127, [Tvb[k], TAb[g]], [TP[pk]])
            if i >= 2:
                vmm(i - 2)
            if i == 127:
                vmm(126)
                vmm(127)
        for dc in range(8):
            pk = dc // 2
            for hh in range(2):
                xs = self.xT[:, dc, t0 + hh * 128:t0 + (hh + 1) * 128]
                self.stt(xs, ps[pk][:, (dc % 2) * TB + hh * 128:(dc % 2) * TB + (hh + 1) * 128], g2[:, dc:dc + 1],
                         xs, ALU.mult, ALU.add, [TP[pk], self.Tm, Txb[hh]], [Txb[hh]])
        S.barrier()


def _issue_conv(self, L):
    if not hasattr(self, "conv"):
        self.conv = {}
    if L in self.conv:
        return
    nc, S = self.nc, self.S
    u_d = self.dram("peeru", [2, 128, 128, 8, 128])
    v_d = self.dram("peerv", [2, 128, 128, 1024])
    ubf = nc.dram_tensor(f"ubf{L}", [128, 128, 1024], BF16, kind="Internal").ap()
    vbf = nc.dram_tensor(f"vbf{L}", [128, 128, 1024], BF16, kind="Internal").ap()
    Tcv = [T(f"cv{L}_{i}") for i in range(16)]
    for k in range(16):
        S.dma("pool", ubf[8 * k:8 * k + 8].rearrange("i p f -> (i p) f"),
              u_d[L, 8 * k:8 * k + 8].rearrange("i p c j -> (i p) (c j)"), writes=[Tcv[k]],
              max_dma_last_dim=2048)
        S.dma("pool", vbf[8 * k:8 * k + 8].rearrange("i p f -> (i p) f"),
              v_d[L, 8 * k:8 * k + 8].rearrange("i p f -> (i p) f"), writes=[Tcv[k]],
              max_dma_last_dim=2048)
    self.conv[L] = (ubf, vbf, Tcv)


def _issue_sgu_conv(self):
    if hasattr(self, "sconv"):
        return
    nc, S = self.nc, self.S
    wu_d = self.dram("sguwu", [24, 128, 8, 128])
    wv_d = self.dram("sguwv", [6, 128, 8, 512])
    wo_d = self.dram("sguwo", [24, 128, 1024])
    wu_b = nc.dram_tensor("sguwu_bf", [24, 128, 1024], BF16, kind="Internal").ap()
    wv_b = nc.dram_tensor("sguwv_bf", [6, 128, 4096], BF16, kind="Internal").ap()
    wo_b = nc.dram_tensor("sguwo_bf", [24, 128, 1024], BF16, kind="Internal").ap()
    Tu, Tv, To = T("cvu"), T("cvv"), T("cvo")
    for k in range(3):
        S.dma("pool", wu_b[8 * k:8 * k + 8].rearrange("f p x -> (f p) x"),
              wu_d[8 * k:8 * k + 8].rearrange("f p c m -> (f p) (c m)"), writes=[Tu], max_dma_last_dim=2048)
        S.dma("pool", wo_b[8 * k:8 * k + 8].rearrange("f p x -> (f p) x"),
              wo_d[8 * k:8 * k + 8].rearrange("f p x -> (f p) x"), writes=[To], max_dma_last_dim=2048)
    for g in range(6):
        S.dma("pool", wv_b[g].rearrange("p (a x) -> (p a) x", a=2),
              wv_d[g].rearrange("p (a c) n -> (p a) (c n)", a=2), writes=[Tv], max_dma_last_dim=2048)
    self.sconv = (wu_b, wv_b, wo_b, Tu, Tv, To)


Prog.issue_sgu_conv = _issue_sgu_conv
Prog.issue_conv = _issue_conv
Prog.phase_peer = _phase_peer


def _phase_sgu(self):
    S, A = self.S, self.A
    ps, TP = self.ps, self.TP
    wu_d = self.dram("sguwu", [24, 128, 8, 128])
    wv_d = self.dram("sguwv", [6, 128, 8, 512])
    bu_d = self.dram("sgubu", [128, 24])
    bv_d = self.dram("sgubv", [1, 3072])
    lng_d = self.dram("sgulng", [128, 24])
    lnb2_d = self.dram("sgulnb2", [2, 3072])
    ws_d = self.dram("sguws", [128, 8, 128])
    bs_d = self.dram("sgubs", [1, 8, 128])
    wo_d = self.dram("sguwo", [24, 128, 1024])
    mask_d = self.dram("sgumask", [128, 128])
    L = 1
    TB = 256
    NB = TL // TB
    sh1 = self.modT[:, L, 0:8]
    g1 = self.modT[:, L, 16:24]
    coef = self.coefA[:, L, 0, :]
    self.issue_sgu_conv()
    wu_b, wv_b, wo_b, Tcu, Tcv_, Tco = self.sconv
    dq = ("sp", "act", "pool")

    Tt = T("stab")
    bu = A.alloc([24], F32)
    lng = A.alloc([24], F32)
    bv = A.alloc([3072], F32)
    lnb2 = A.alloc([3072], F32)
    rhs2 = A.alloc([8, 128], F32)
    WmT = A.alloc([8, 128], BF16)
    Btab = A.alloc([24, 128], F32)
    S.dma("sp", bu, bu_d, writes=[Tt])
    S.dma("sp", lng, lng_d, writes=[Tt])
    S.dma("sp", bv[0:1, :], bv_d, writes=[Tt])
    S.dma("sp", lnb2[0:2, :], lnb2_d, writes=[Tt])
    S.dma("sp", rhs2[1:2, :, :], bs_d, writes=[Tt])
    m1 = A.mark()
    ws = A.alloc([8, 128], F32)
    msk = A.alloc([128], F32)
    wm32 = A.alloc([8, 128], F32)
    Tws = T("ws")
    S.dma("act", ws, ws_d, writes=[Tws])
    S.dma("act", msk, mask_d, writes=[Tws])
    self.tt("dve", ws, ws, msk.unsqueeze(1).to_broadcast([128, 8, 128]), ALU.mult, [Tws], [Tws])
    for half in range(2):
        for q in range(4):
            g = half * 4 + q
            self.tr(ps[6 + half][:, q * 128:(q + 1) * 128], ws[:, g, :], [Tws], [TP[6 + half]])
        self.cp("dve", wm32[:, half * 4:(half + 1) * 4, :], ps[6 + half][:, :].rearrange("p (a b) -> p a b", b=128),
                [TP[6 + half]], [Tws])
    self.cp("act", WmT, wm32, [Tws], [Tt])
    for half in range(2):
        self.mm(ps[6 + half][0:1, :], self.ones[:, 0:1], wm32[:, half * 4:(half + 1) * 4, :].rearrange("p a b -> p (a b)"),
                True, True, [Tws, self.Tc], [TP[6 + half]])
        self.cp("dve", rhs2[0:1, half * 4:(half + 1) * 4, :], ps[6 + half][0:1, :].rearrange("p (a b) -> p a b", b=128),
                [TP[6 + half]], [Tt])
    for fc in range(24):
        pk = 6 + (fc // 4) % 2
        self.mm(ps[pk][:, (fc % 4) * 128:(fc % 4 + 1) * 128], lnb2[0:2, fc * 128:(fc + 1) * 128],
                rhs2[0:2, fc // 3, :], True, True, [Tt], [TP[pk]])
        if fc % 4 == 3:
            self.cp("dve", Btab[:, fc - 3:fc + 1, :], ps[pk][:, :].rearrange("p (a b) -> p a b", b=128),
                    [TP[pk]], [Tt])
    S.barrier()
    A.reset(m1)

    hn = A.alloc([8, TB], BF16)
    Thn = T("hn")
    scr = self.norm_scratch(7, TB)
    uT = A.alloc([24, TB], BF16)
    TuT = T("uT")
    vf = [A.alloc([3072], F32) for _ in range(2)]
    vn = [A.alloc([3072], BF16) for _ in range(2)]
    Tvf = [T("vf0"), T("vf1")]
    Tvn = [T("vn0"), T("vn1")]
    NWU = 4
    wub = [A.alloc([8, 128], BF16) for _ in range(NWU)]
    Twu = [T(f"wu{i}") for i in range(NWU)]
    wvb = [A.alloc([8, 512], BF16) for _ in range(2)]
    Twv = [T("wv0"), T("wv1")]
    NWO = 4
    wob = [A.alloc([1024], BF16) for _ in range(NWO)]
    Two = [T(f"wo{i}") for i in range(NWO)]
    s1 = A.alloc([8], F32)
    s2 = A.alloc([1], F32)
    mu = A.alloc([1], F32)
    var = A.alloc([1], F32)
    nb = A.alloc([1], F32)
    Tst = T("lnstat")
    tmp = [A.alloc([128], F32) for _ in range(2)]
    Ttmp = [T("t0"), T("t1")]
    print("sgu arena words used", A.off, "of", A.n)

    for blk in range(NB):
        t0 = blk * TB
        Txb = self.Tx[2 * blk:2 * blk + 2]
        self.norm_mod(self.xT[:, :, t0:t0 + TB], Txb, TB, coef, sh1, hn, [Thn], scr)
        for fc in range(24):
            i = fc % 2
            w = fc % NWU
            S.dma(dq[fc % 3], wub[w], wu_b[fc], reads=[Tcu], writes=[Twu[w]])
            for dc in range(8):
                self.mm(ps[4 + i][:, 0:TB], wub[w][:, dc, :], hn[:, dc, :], dc == 0, dc == 7, [Twu[w], Thn], [TP[4 + i]])
            self.act(uT[:, fc, :], ps[4 + i][:, 0:TB], AF.Gelu_apprx_tanh, [TP[4 + i], Tt], [TuT], bias=bu[:, fc:fc + 1])
        for cg in range(6):
            i = cg % 2
            S.dma(dq[cg % 3], wvb[i], wv_b[cg], reads=[Tcv_], writes=[Twv[i]])
            for tt in range(2):
                pk = 4 + tt
                self.mm(ps[pk][:, :], self.ones[0:1, :], bv[0:1, cg * 512:(cg + 1) * 512], True, False,
                        [self.Tc, Tt], [TP[pk]])
                for dc in range(8):
                    self.mm(ps[pk][:, :], hn[:, dc, tt * 128:(tt + 1) * 128], wvb[i][:, dc, :], False, dc == 7,
                            [Twv[i], Thn], [TP[pk]])
                self.act(vf[tt][:, cg * 512:(cg + 1) * 512], ps[pk][:, :], AF.Gelu_apprx_tanh, [TP[pk]], [Tvf[tt], Tst],
                         accum_out=s1[:, tt * 4 + cg // 2 * 0 + 0:tt * 4 + 1] if False else None)
        for tt in range(2):
            S.op("dve", lambda e, tt=tt: e.tensor_reduce(mu, vf[tt], AX.X, ALU.add), [Tvf[tt]], [Tst])
            self.act(vn[tt], vf[tt], AF.Square, [Tvf[tt]], [Tvn[tt], Tst], accum_out=s2[:, 0:1])
            self.ts("dve", mu, mu, 1.0 / 3072, None, ALU.mult, None, [Tst], [Tst])
            self.tt("dve", var, mu, mu, ALU.mult, [Tst], [Tst])
            self.stt(var, s2, 1.0 / 3072, var, ALU.mult, ALU.subtract, [Tst], [Tst])
            self.act(var, var, AF.Sqrt, [Tst, self.Tc], [Tst], bias=self.epsb[:, 0:1], scale=1.0)
            S.op("dve", lambda e: e.reciprocal(var, var), [Tst], [Tst])
            self.stt(nb, mu, -1.0, var, ALU.mult, ALU.mult, [Tst], [Tst])
            self.act(vn[tt], vf[tt], AF.Identity, [Tvf[tt], Tst], [Tvn[tt]], bias=nb[:, 0:1], scale=var[:, 0:1])
        for tt in range(2):
            for fc in range(24):
                pk = 6 + fc % 2
                q = (fc // 2) % 4
                self.mm(ps[pk][:, q * 128:(q + 1) * 128], vn[tt][:, fc * 128:(fc + 1) * 128], WmT[:, fc // 3, :],
                        True, True, [Tvn[tt], Tt], [TP[pk]])
                i = fc % 2
                self.stt(tmp[i], ps[pk][:, q * 128:(q + 1) * 128], lng[:, fc:fc + 1], Btab[:, fc, :],
                         ALU.mult, ALU.add, [TP[pk], Tt], [Ttmp[i]])
                usl = uT[:, fc, tt * 128:(tt + 1) * 128]
                self.tt("pool", usl, usl, tmp[i], ALU.mult, [Ttmp[i], TuT], [TuT])
        for fc in range(24):
            k = fc % NWO
            S.dma(dq[fc % 3], wob[k], wo_b[fc], reads=[Tco], writes=[Two[k]])
            for dc in range(8):
                pk = dc // 2
                self.mm(ps[pk][:, (dc % 2) * TB:(dc % 2 + 1) * TB], wob[k][:, dc * 128:(dc + 1) * 128], uT[:, fc, :],
                        fc == 0 and dc % 2 == 0, fc == 23, [Two[k], TuT], [TP[pk]])
        for dc in range(8):
            pk = dc // 2
            for hh in range(2):
                xs = self.xT[:, dc, t0 + hh * 128:t0 + (hh + 1) * 128]
                self.stt(xs, ps[pk][:, (dc % 2) * TB + hh * 128:(dc % 2) * TB + (hh + 1) * 128], g1[:, dc:dc + 1],
                         xs, ALU.mult, ALU.add, [TP[pk], self.Tm, Txb[hh]], [Txb[hh]])


Prog.phase_sgu = _phase_sgu
```

```python
import math
from contextlib import ExitStack

import numpy as np
import concourse.bass as bass
import concourse.mybir as mybir
from concourse.bass_utils import run_bass_kernel_spmd

F32 = mybir.dt.float32
BF16 = mybir.dt.bfloat16
I32 = mybir.dt.int32
U32 = mybir.dt.uint32
ALU = mybir.AluOpType
AF = mybir.ActivationFunctionType
AX = mybir.AxisListType

D = 1024
DC = 8
TL = 2048
NPREV = 6144
EPS = 1e-6
ENGS = ("pe", "act", "dve", "pool", "sp")
NDMA_SEM = 8
SAME_ENGINE_SYNC = True
NEG = -1.0e30


class T:
    __slots__ = ("name", "lw", "rd")

    def __init__(self, name=""):
        self.name = name
        self.lw = None
        self.rd = {}


class Sched:
    def __init__(self, nc, stack):
        self.nc = nc
        self._stack = stack
        self.sem = {e: stack.enter_context(nc.semaphore("s_" + e)) for e in ENGS}
        self.dsem = {}
        for q in ("sp", "act", "pool"):
            for k in range(NDMA_SEM):
                self.dsem[(q, k)] = stack.enter_context(nc.semaphore(f"d_{q}{k}"))
        self.cnt = {e: 0 for e in ENGS}
        self.dcnt = {q: 0 for q in ("sp", "act", "pool")}
        self.waited = {e: {} for e in ENGS}
        self.lists = {e: [] for e in ENGS}

    def _deps(self, eng, reads, writes):
        deps = {}

        def add(sv):
            if sv is None:
                return
            s, v = sv
            if deps.get(s, 0) < v:
                deps[s] = v
        for r in reads:
            add(r.lw)
        for w in writes:
            add(w.lw)
            for s, v in w.rd.items():
                add((s, v))
        out = []
        for s, v in deps.items():
            if s == eng and (eng == "pe" or not SAME_ENGINE_SYNC):
                continue
            if self.waited[eng].get(s, 0) >= v:
                continue
            self.waited[eng][s] = v
            out.append((s, v))
        return out

    def _semof(self, s):
        return self.sem[s] if isinstance(s, str) else self.dsem[s]

    def op(self, eng, fn, reads=(), writes=()):
        waits = self._deps(eng, reads, writes)
        self.cnt[eng] += 1
        v = self.cnt[eng]
        self.lists[eng].append((waits, fn, (self.sem[eng], 1)))
        for r in reads:
            r.rd[eng] = v
        for w in writes:
            w.lw = (eng, v)
            w.rd = {}

    def dma(self, q, out, in_, reads=(), writes=(), **kw):
        j = self.dcnt[q]
        self.dcnt[q] += 1
        src = (q, j % NDMA_SEM)
        val = 16 * (j // NDMA_SEM + 1)
        waits = self._deps(q, reads, writes)
        if j >= NDMA_SEM and self.waited[q].get(src, 0) < val - 16:
            self.waited[q][src] = val - 16
            waits.append((src, val - 16))

        def fn(e, out=out, in_=in_, kw=kw):
            return e.dma_start(out=out, in_=in_, **kw)
        self.lists[q].append((waits, fn, (self.dsem[src], 16)))
        for r in reads:
            r.rd[src] = val
        for w in writes:
            w.lw = (src, val)
            w.rd = {}

    def cc(self, fn, reads=(), writes=()):
        if ("cc", 0) not in self.dsem:
            self.dsem[("cc", 0)] = self._stack.enter_context(self.nc.semaphore("cc"))
            self.ccn = 0
        waits = self._deps("pool", reads, writes)
        for k in range(NDMA_SEM):
            n = (self.dcnt["pool"] - k + NDMA_SEM - 1) // NDMA_SEM
            if n > 0 and 16 * n > self.waited["pool"].get(("pool", k), 0):
                self.waited["pool"][("pool", k)] = 16 * n
                waits.append((("pool", k), 16 * n))
        self.ccn += 1
        src, val = ("cc", 0), self.ccn
        self.lists["pool"].append((waits, fn, (self.dsem[src], 1)))
        self.lists["pool"].append(([(src, val)], None, None))
        self.waited["pool"][src] = val
        for r in reads:
            r.rd[src] = val
        for w in writes:
            w.lw = (src, val)
            w.rd = {}

    def barrier(self):
        for e in ENGS:
            waits = []
            for s in ENGS:
                v = self.cnt[s]
                if s != e and v > self.waited[e].get(s, 0):
                    self.waited[e][s] = v
                    waits.append((s, v))
            for q in ("sp", "act", "pool"):
                for k in range(NDMA_SEM):
                    n = (self.dcnt[q] - k + NDMA_SEM - 1) // NDMA_SEM
                    v = 16 * n
                    if n > 0 and v > self.waited[e].get((q, k), 0):
                        self.waited[e][(q, k)] = v
                        waits.append(((q, k), v))
            if waits:
                self.lists[e].append((waits, None, None))

    def emit(self):
        nc = self.nc
        handles = {"pe": "tensor", "act": "scalar", "dve": "vector", "pool": "gpsimd", "sp": "sync"}
        with nc.Block() as block:
            for e in ENGS:
                def body(eng, lst=self.lists[e]):
                    for waits, fn, inc in lst:
                        for s, v in waits:
                            eng.wait_ge(self._semof(s), v)
                        if fn is not None:
                            fn(eng).then_inc(inc[0], inc[1])
                getattr(block, handles[e])(body)


class Arena:
    def __init__(self, ap, nwords):
        self.ap = ap
        self.n = nwords
        self.off = 0

    def mark(self):
        return self.off

    def reset(self, m):
        self.off = m

    def alloc(self, shape, dtype):
        nel = int(np.prod(shape))
        bpe = 2 if dtype == BF16 else 4
        nw = (nel * bpe + 3) // 4
        nw = (nw + 7) // 8 * 8
        assert self.off + nw <= self.n, f"SBUF arena overflow {self.off}+{nw}>{self.n}"
        v = self.ap[:, self.off:self.off + nw]
        self.off += nw
        if dtype != F32:
            v = v.bitcast(dtype)
        v = v[:, 0:nel]
        if len(shape) == 2:
            v = v.rearrange("p (a b) -> p a b", b=shape[1])
        elif len(shape) == 3:
            v = v.rearrange("p (a b c) -> p a b c", b=shape[1], c=shape[2])
        return v


class Prog:
    def __init__(self, phases, final_norm=True):
        self.phases = phases
        self.final_norm = final_norm
        self.nc = bass.Bass("TRN2", target_bir_lowering=False)
        self.din = {}

    def dram(self, name, shape, dtype=F32):
        if name in self.din:
            return self.din[name]
        t = self.nc.dram_tensor(name, list(shape), dtype, kind="ExternalInput").ap()
        self.din[name] = t
        return t

    def mm(self, out, lhsT, rhs, start, stop, reads, writes):
        self.S.op("pe", lambda e: e.matmul(out, lhsT, rhs, start=start, stop=stop,
                                           skip_group_check=True), reads, writes)

    def tr(self, out, in_, reads, writes):
        idn = self.ident
        self.S.op("pe", lambda e: e.transpose(out, in_, idn), list(reads) + [self.Tc], writes)

    def tt(self, eng, out, in0, in1, op, reads, writes):
        self.S.op(eng, lambda e: e.tensor_tensor(out, in0, in1, op), reads, writes)

    def ts(self, eng, out, in0, s1, s2, op0, op1, reads, writes):
        if s2 is None:
            self.S.op(eng, lambda e: e.tensor_scalar(out, in0, s1, None, op0), reads, writes)
        else:
            self.S.op(eng, lambda e: e.tensor_scalar(out, in0, s1, s2, op0, op1), reads, writes)

    def stt(self, out, in0, sc, in1, op0, op1, reads, writes):
        self.S.op("dve", lambda e: e.scalar_tensor_tensor(out, in0, sc, in1, op0, op1), reads, writes)

    def act(self, out, in_, func, reads, writes, bias=None, scale=None, accum_out=None):
        kw = {}
        if bias is not None:
            kw["bias"] = bias
        if scale is not None:
            kw["scale"] = scale
        if accum_out is not None:
            kw["accum_out"] = accum_out
        self.S.op("act", lambda e: e.activation(out, in_, func, **kw), reads, writes)

    def cp(self, eng, out, in_, reads, writes):
        if eng == "act":
            self.S.op("act", lambda e: e.copy(out, in_), reads, writes)
        else:
            self.S.op(eng, lambda e: e.tensor_copy(out, in_), reads, writes)

    def build(self):
        nc = self.nc
        with ExitStack() as st:
            self.st = st
            self.S = S = Sched(nc, st)
            NW = 52992
            arena_t = st.enter_context(nc.sbuf_tensor("arena", [128, NW], F32))
            self.A = A = Arena(arena_t, NW)
            self.ps = [st.enter_context(nc.psum_tensor(f"ps{k}", [128, 512], F32)) for k in range(8)]
            self.TP = [T(f"ps{k}") for k in range(8)]
            self.Tc = T("consts")

            xloc = self.dram("xloc", [TL, D])
            self.y = nc.dram_tensor("y", [TL, D], F32, kind="ExternalOutput").ap()
            ident_d = self.dram("ident", [128, 128])
            cT_d = self.dram("cT", [128, 8])
            nm_d = self.dram("nmT", [128, 2, 8])
            nf_d = self.dram("nfT", [128, 2, 8])
            nfin_d = self.dram("nfinT", [128, 8])
            adaw_d = self.dram("adaw", [2, 48, 128, 8, 128])
            adab_d = self.dram("adab", [128, 2, 48])

            self.ident = A.alloc([128], F32)
            self.ones = A.alloc([128], F32)
            self.onesb = A.alloc([128], BF16)
            self.epsb = A.alloc([1], F32)
            self.xT = A.alloc([8, TL], F32)
            self.Tx = [T(f"x{g}") for g in range(TL // 128)]
            self.modT = A.alloc([2, 48], F32)
            self.nm = A.alloc([2, 8], F32)
            self.nf = A.alloc([2, 8], F32)
            self.nfin = A.alloc([8], F32)
            self.coefA = A.alloc([2, 2, 8], F32)
            cTt = A.alloc([8], F32)
            adab = A.alloc([2, 48], F32)

            S.dma("sp", self.ident, ident_d, writes=[self.Tc])
            S.op("dve", lambda e: e.memset(self.ones, 1.0), writes=[self.Tc])
            S.op("dve", lambda e: e.memset(self.onesb, 1.0), writes=[self.Tc])
            S.op("dve", lambda e: e.memset(self.epsb, EPS), writes=[self.Tc])
            Tm = T("mod")
            S.dma("sp", cTt, cT_d, writes=[Tm])
            S.dma("sp", self.nm, nm_d, writes=[Tm])
            S.dma("sp", self.nf, nf_d, writes=[Tm])
            S.dma("sp", self.nfin, nfin_d, writes=[Tm])
            S.dma("sp", adab, adab_d, writes=[Tm])
            self.act(cTt, cTt, AF.Silu, [Tm], [Tm])

            m0 = A.mark()
            wbuf = [A.alloc([8, 128], F32) for _ in range(3)]
            Tw = [T(f"adaw{i}") for i in range(3)]
            qs = ("sp", "act")
            for l in range(2):
                for fc in range(48):
                    i = (l * 48 + fc) % 3
                    S.dma(qs[fc % 2], wbuf[i], adaw_d[l, fc], writes=[Tw[i]])
                    for dc in range(8):
                        self.mm(self.ps[0][:, fc:fc + 1], wbuf[i][:, dc, :], cTt[:, dc:dc + 1],
                                dc == 0, dc == 7, [Tw[i], Tm], [self.TP[0]])
                self.tt("dve", self.modT[:, l, :], self.ps[0][:, 0:48], adab[:, l, :], ALU.add,
                        [self.TP[0], Tm], [Tm])
            for l in range(2):
                for sub, gn in ((0, self.nm), (1, self.nf)):
                    sc = self.modT[:, l, (3 * sub + 1) * 8:(3 * sub + 2) * 8]
                    self.stt(self.coefA[:, l, sub, :], sc, 1.0, gn[:, l, :], ALU.add, ALU.mult, [Tm], [Tm])
            self.Tm = Tm
            S.barrier()
            A.reset(m0)

            m0 = A.mark()
            xin = [A.alloc([D], F32) for _ in range(2)]
            Txin = [T("xin0"), T("xin1")]
            for tl in range(TL // 128):
                b = tl % 2
                S.dma(qs[b], xin[b], xloc[tl * 128:(tl + 1) * 128, :], writes=[Txin[b]])
                for half in range(2):
                    pk = 2 * b + half
                    for q4 in range(4):
                        dc = half * 4 + q4
                        self.tr(self.ps[pk][:, q4 * 128:(q4 + 1) * 128], xin[b][:, dc * 128:(dc + 1) * 128],
                                [Txin[b]], [self.TP[pk]])
                    self.cp("act" if half else "dve",
                            self.xT[:, half * 4:(half + 1) * 4, tl * 128:(tl + 1) * 128],
                            self.ps[pk][:, :].rearrange("p (a b) -> p a b", b=128),
                            [self.TP[pk]], [self.Tx[tl]])
            S.barrier()
            A.reset(m0)

            for ph in self.phases:
                m0 = A.mark()
                if ph == "ret":
                    self.phase_ret()
                elif ph == "peer0":
                    self.phase_peer(0)
                elif ph == "sgu":
                    self.phase_sgu()
                elif ph == "peer1":
                    self.phase_peer(1)
                S.barrier()
                A.reset(m0)

            self.phase_final()
            S.emit()
        return nc

    def norm_mod(self, src, Tsrc, n, coef, shift, dst, Tdst, scr):
        S = self.S
        sq, Tsq, rstd, Trs, tmp, Ttmp, pk = scr
        for dc in range(8):
            i = dc % 2
            self.act(sq[i][:, :n], src[:, dc, :], AF.Square, Tsrc, [Tsq[i]])
            self.mm(self.ps[pk][:, :n], self.ones, sq[i][:, :n], dc == 0, dc == 7,
                    [Tsq[i], self.Tc], [self.TP[pk]])
        self.act(rstd[:, :n], self.ps[pk][:, :n], AF.Sqrt, [self.TP[pk], self.Tc], [Trs],
                 bias=self.epsb[:, 0:1], scale=1.0 / D)
        S.op("dve", lambda e: e.reciprocal(rstd[:, :n], rstd[:, :n]), [Trs], [Trs])
        for dc in range(8):
            i = dc % 2
            self.tt("dve", tmp[i][:, :n], src[:, dc, :], rstd[:, :n], ALU.mult, list(Tsrc) + [Trs], [Ttmp[i]])
            if shift is None:
                self.act(dst[:, dc, :], tmp[i][:, :n], AF.Copy, [Ttmp[i], self.Tm], Tdst,
                         scale=coef[:, dc:dc + 1])
            else:
                self.act(dst[:, dc, :], tmp[i][:, :n], AF.Identity, [Ttmp[i], self.Tm], Tdst,
                         bias=shift[:, dc:dc + 1], scale=coef[:, dc:dc + 1])

    def norm_scratch(self, pk, n=512):
        A = self.A
        sq = [A.alloc([n], F32) for _ in range(2)]
        rstd = A.alloc([n], F32)
        tmp = [A.alloc([n], F32) for _ in range(2)]
        return (sq, [T("sq0"), T("sq1")], rstd, T("rstd"), tmp, [T("tmp0"), T("tmp1")], pk)

    def phase_final(self):
        S, A = self.S, self.A
        m0 = A.mark()
        scr = self.norm_scratch(0)
        xn = [A.alloc([8, 512], F32) for _ in range(1)]
        Txn = T("xn")
        ob = [A.alloc([D], F32) for _ in range(2)]
        Tob = [T("ob0"), T("ob1")]
        Tout = T("yout")
        for g in range(TL // 512):
            src = self.xT[:, :, g * 512:(g + 1) * 512]
            Tsrc = self.Tx[g * 4:(g + 1) * 4]
            if self.final_norm:
                self.norm_mod(src, Tsrc, 512, self.nfin, None, xn[0], [Txn], scr)
                s2, Ts2 = xn[0], [Txn]
            else:
                s2, Ts2 = src, Tsrc
            for tt in range(4):
                b = tt % 2
                for half in range(2):
                    pk = 2 + 2 * b + half
                    for q4 in range(4):
                        dc = half * 4 + q4
                        self.tr(self.ps[pk][:, q4 * 128:(q4 + 1) * 128], s2[:, dc, tt * 128:(tt + 1) * 128],
                                Ts2, [self.TP[pk]])
                    self.cp("act" if half else "dve", ob[b][:, half * 512:(half + 1) * 512],
                            self.ps[pk][:, :], [self.TP[pk]], [Tob[b]])
                r0 = g * 512 + tt * 128
                S.dma("sp" if b else "act", self.y[r0:r0 + 128, :], ob[b], reads=[Tob[b]], writes=[Tout])
        waits = S._deps("sp", [Tout], ())
        S.lists["sp"].append((waits, None, None))
        A.reset(m0)


def _consts():
    c = {}
    c["ident"] = np.eye(128, dtype=np.float32)
    half = 128
    c["invf"] = (1.0 / (np.float32(10000.0) ** (np.arange(half, dtype=np.float32) / np.float32(half)))
                 ).astype(np.float32).reshape(128, 1)
    H = 4
    lg = np.log(1.0 - 2.0 ** (-5.0 - np.arange(H, dtype=np.float64)))
    idx = np.arange(128, dtype=np.float64)
    ch = (np.arange(128) // 64)
    dect = np.zeros((128, H, 128), np.float64)
    for h in range(H):
        i = idx[None, :]
        j = idx[:, None]
        same = (ch[None, :] == ch[:, None])
        earlier = (ch[:, None] < ch[None, :])
        w = np.where(same, np.exp(lg[h] * np.abs(i - j)), np.where(earlier, np.exp(lg[h] * (i - j)), 0.0))
        dect[:, h, :] = w / 16.0
    c["dect"] = dect.astype(np.float32)
    xi = np.exp(lg[:, None] * (idx[None, :] + 1.0))
    c["xit"] = np.broadcast_to(xi[None], (128, H, 128)).astype(np.float32).copy()
    ze = np.exp(lg[None, :] * (127.0 - idx[:, None])) / 16.0
    c["zet"] = ze.astype(np.float32)
    c["gam128"] = [float(np.exp(lg[h] * 128.0)) for h in range(H)]
    c["iota"] = np.broadcast_to(np.arange(128, dtype=np.float32)[None], (128, 128)).copy()
    pc = np.arange(128) // 64
    c["sgumask"] = (pc[None, :] <= pc[:, None]).astype(np.float32)
    return c


_CONSTS = _consts()


def _host_layout(inp, names):
    f = np.float32
    x = np.asarray(inp["x"], f)
    pos = np.asarray(inp["positions"], np.int32)
    shared = {}

    def need(n):
        return n in names
    for k in ("ident", "invf", "dect", "xit", "zet", "iota", "sgumask"):
        if need(k):
            shared[k] = _CONSTS[k]
    if need("nmT"):
        shared["nmT"] = np.ascontiguousarray(np.asarray(inp["norm_mix"], f).reshape(2, 8, 128).transpose(2, 0, 1))
        shared["nfT"] = np.ascontiguousarray(np.asarray(inp["norm_ffn"], f).reshape(2, 8, 128).transpose(2, 0, 1))
        shared["nfinT"] = np.ascontiguousarray(np.asarray(inp["norm_final"], f).reshape(8, 128).T)
        aw = np.asarray(inp["ada_w"], f).reshape(2, 8, 128, 48, 128)
        shared["adaw"] = np.ascontiguousarray(aw.transpose(0, 3, 2, 1, 4))
        shared["adab"] = np.ascontiguousarray(np.asarray(inp["ada_b"], f).reshape(2, 48, 128).transpose(2, 0, 1))
    if need("retwin"):
        w = np.asarray(inp["ret_w_in"], f)[0].reshape(8, 128, 6144)
        parts = []
        for h in range(4):
            cols = np.concatenate([np.arange(h * 256, (h + 1) * 256), 1024 + np.arange(h * 256, (h + 1) * 256),
                                   2048 + np.arange(h * 512, (h + 1) * 512), 4096 + np.arange(h * 512, (h + 1) * 512)])
            parts.append(w[:, :, cols].transpose(1, 0, 2))
        shared["retwin"] = np.ascontiguousarray(np.stack(parts))
        wo = np.asarray(inp["ret_w_out"], f)[0].reshape(4, 4, 128, 1024)
        shared["retwo"] = np.ascontiguousarray(wo.transpose(0, 2, 1, 3))
    if need("sguwu"):
        w = np.asarray(inp["sgu_w_in"], f)[0].reshape(8, 128, 6144)
        shared["sguwu"] = np.ascontiguousarray(w[:, :, :3072].reshape(8, 128, 24, 128).transpose(2, 1, 0, 3))
        shared["sguwv"] = np.ascontiguousarray(w[:, :, 3072:].reshape(8, 128, 6, 512).transpose(2, 1, 0, 3))
        b = np.asarray(inp["sgu_b_in"], f)[0]
        shared["sgubu"] = np.ascontiguousarray(b[:3072].reshape(24, 128).T)
        shared["sgubv"] = np.ascontiguousarray(b[3072:].reshape(1, 3072))
        shared["sgulng"] = np.ascontiguousarray(np.asarray(inp["sgu_ln_g"], f)[0].reshape(24, 128).T)
        lb = np.asarray(inp["sgu_ln_b"], f)[0]
        shared["sgulnb2"] = np.ascontiguousarray(np.stack([lb, np.ones_like(lb)]))
        shared["sguws"] = np.ascontiguousarray(np.asarray(inp["sgu_w_s"], f)[0].transpose(1, 0, 2))
        shared["sgubs"] = np.ascontiguousarray(np.asarray(inp["sgu_b_s"], f)[0].reshape(1, 8, 128))
        shared["sguwo"] = np.ascontiguousarray(np.asarray(inp["sgu_w_out"], f)[0].reshape(24, 128, 1024))
    if need("peerq"):
        wq = np.asarray(inp["peer_w_query"], f).reshape(2, 8, 128, 16, 128)
        shared["peerq"] = np.ascontiguousarray(wq.transpose(0, 3, 2, 1, 4))
        sk = np.asarray(inp["peer_sub_keys"], f).reshape(2, 16, 128, 128)
        shared["peerk"] = np.ascontiguousarray(sk.transpose(0, 3, 1, 2))
        u = np.asarray(inp["peer_u"], f).reshape(2, 128, 128, 8, 128)
        shared["peeru"] = np.ascontiguousarray(u.transpose(0, 1, 4, 3, 2))
        shared["peerv"] = np.ascontiguousarray(np.asarray(inp["peer_v"], f).reshape(2, 128, 128, 1024))
    maps = []
    for c in range(8):
        b, s = c // 4, c % 4
        m = dict(shared)
        m["xloc"] = np.ascontiguousarray(x[b, s * TL:(s + 1) * TL])
        m["cT"] = np.ascontiguousarray(np.asarray(inp["c"], f)[b].reshape(8, 128).T)
        if need("wst"):
            lg = np.log(1.0 - 2.0 ** (-5.0 - np.arange(4, dtype=np.float64)))
            w = np.zeros((128, 32), f)
            for c2 in range(8):
                b2, s2 = c2 // 4, c2 % 4
                if b2 == b and s2 < s:
                    for h in range(4):
                        w[:, c2 * 4 + h] = np.exp(lg[h] * TL * (s - s2 - 1))
            m["wst"] = w
            m["posloc"] = np.ascontiguousarray(pos[b, s * TL:(s + 1) * TL].reshape(1, TL))
        if need("xprev"):
            xp = np.zeros((NPREV, D), f)
            pp = np.zeros((1, NPREV), np.int32)
            n = s * TL
            if n:
                xp[NPREV - n:] = x[b, :n]
                pp[0, NPREV - n:] = pos[b, :n]
            m["xprev"] = xp
            m["posprev"] = pp
            m["posloc"] = np.ascontiguousarray(pos[b, s * TL:(s + 1) * TL].reshape(1, TL))
            val = np.zeros((128, NPREV // 128), f)
            val[:, (NPREV - n) // 128:] = 1.0
            m["valid"] = val
        maps.append(m)
    return maps


_PROG_CACHE = {}


def _run(inp, phases, final_norm=True, xoverride=None):
    key = (tuple(phases), final_norm)
    if key not in _PROG_CACHE:
        p = Prog(phases, final_norm)
        p.build()
        _PROG_CACHE[key] = p
    p = _PROG_CACHE[key]
    maps = _host_layout(inp, set(p.din.keys()))
    if xoverride is not None:
        for c in range(8):
            maps[c]["xloc"] = np.ascontiguousarray(xoverride[c])
    maps = [{k: m[k] for k in p.din.keys()} for m in maps]
    res = run_bass_kernel_spmd(p.nc, maps, core_ids=list(range(8)))
    return [np.asarray(r["y"]) for r in res.results]


def kernel(**inputs):
    ys = _run(inputs, ["ret", "peer0", "sgu", "peer1"], True)
    out = np.stack(ys).reshape(2, 4 * TL, D)
    return out.astype(np.float32)


MAGIC = 12582912.0
INV2PI = 0.15915494309189535
C1 = 6.28125
C2 = 0.0019353071795864769


def _phase_ret(self):
    S, A, nc = self.S, self.A, self.nc
    posloc = self.dram("posloc", [1, TL], I32)
    xprev = self.dram("xprev", [NPREV, D])
    posprev = self.dram("posprev", [1, NPREV], I32)
    valid_d = self.dram("valid", [128, NPREV // 128])
    invf_d = self.dram("invf", [128, 1])
    dect_d = self.dram("dect", [128, 4, 128])
    xit_d = self.dram("xit", [128, 4, 128])
    zet_d = self.dram("zet", [128, 4])
    win_d = self.dram("retwin", [4, 128, 8, 1536])
    wo_d = self.dram("retwo", [4, 128, 4, 1024])
    gam = _CONSTS["gam128"]
    ps, TP = self.ps, self.TP
    L = 0
    sh1 = self.modT[:, L, 0:8]
    g1 = self.modT[:, L, 16:24]
    coef = self.coefA[:, L, 0, :]

    Tt = T("rtab")
    invf = A.alloc([1], F32)
    dect = A.alloc([4, 128], F32)
    xit = A.alloc([4, 128], F32)
    zet = A.alloc([4], F32)
    valid = A.alloc([NPREV // 128], F32)
    halfpi = A.alloc([1], F32)
    for dst, src in ((invf, invf_d), (dect, dect_d), (xit, xit_d), (zet, zet_d), (valid, valid_d)):
        S.dma("sp", dst, src, writes=[Tt])
    S.op("dve", lambda e: e.memset(halfpi, math.pi / 2), writes=[Tt])

    S4 = A.alloc([4, 2, 512], F32)
    TS4 = [T(f"S4_{h}") for h in range(4)]
    S.op("dve", lambda e: e.memset(S4, 0.0), writes=TS4)
    m0 = A.mark()
    Wkv = A.alloc([8, 4, 768], BF16)
    TWkv = T("Wkv")
    for h in range(4):
        for dc in range(8):
            S.dma("pool", Wkv[:, dc, h, :], win_d[h][:, dc, 256:1024], writes=[TWkv])
    if "sgu" in self.phases:
        self.issue_sgu_conv()
    for L_ in (0, 1):
        if ("peer%d" % L_) in self.phases:
            self.issue_conv(L_)
    pscr = self.norm_scratch(7, 128)
    xin = [A.alloc([D], F32) for _ in range(2)]
    Txin = [T("xin0"), T("xin1")]
    xpT = [A.alloc([8, 128], F32) for _ in range(2)]
    Txp = [T("xp0"), T("xp1")]
    hp = [A.alloc([8, 128], BF16) for _ in range(2)]
    Thp = [T("hp0"), T("hp1")]
    pi32p = A.alloc([128], I32)
    angp = A.alloc([128], F32)
    nnp = A.alloc([128], F32)
    csp = [A.alloc([2, 128], F32) for _ in range(2)]
    Tpp = T("posp")
    Tcsp = [T("csp0"), T("csp1")]
    prt = [[A.alloc([128], F32) for _ in range(2)] for _ in range(4)]
    Tprt = [[T(f"prt{i}{j}") for j in range(2)] for i in range(4)]
    pkT = [A.alloc([2, 128], F32) for _ in range(2)]
    Tpk = [T("pk0"), T("pk1")]
    pvb = [A.alloc([512], BF16) for _ in range(2)]
    Tpv = [T("pv0"), T("pv1")]
    pkz = [A.alloc([256], BF16) for _ in range(2)]
    Tpkz = [T("pkz0"), T("pkz1")]
    print("ret prepass arena words used", A.off, "of", A.n)
    def pre_work(ti):
        b = ti % 2
        S.dma("sp" if b else "act", xin[b], xprev[ti * 128:(ti + 1) * 128, :], writes=[Txin[b]])
        for half in range(2):
            pk = 5 + half
            for q4 in range(4):
                dc = half * 4 + q4
                self.tr(ps[pk][:, q4 * 128:(q4 + 1) * 128], xin[b][:, dc * 128:(dc + 1) * 128], [Txin[b]], [TP[pk]])
            self.cp("act" if half else "dve", xpT[b][:, half * 4:(half + 1) * 4, :],
                    ps[pk][:, :].rearrange("p (a b) -> p a b", b=128), [TP[pk]], [Txp[b]])
        self.norm_mod(xpT[b], [Txp[b]], 128, coef, sh1, hp[b], [Thp[b]], pscr)
        S.dma("sp", pi32p, posprev[0:1, ti * 128:(ti + 1) * 128].partition_broadcast(128), writes=[Tpp])
        self.cp("dve", angp, pi32p, [Tpp], [Tpp])
        self.ts("dve", angp, angp, invf[:, 0:1], None, ALU.mult, None, [Tpp, Tt], [Tpp])
        self.ts("dve", nnp, angp, INV2PI, MAGIC, ALU.mult, ALU.add, [Tpp], [Tpp])
        self.ts("dve", nnp, nnp, -MAGIC, None, ALU.add, None, [Tpp], [Tpp])
        self.stt(angp, nnp, -C1, angp, ALU.mult, ALU.add, [Tpp], [Tpp])
        self.stt(angp, nnp, -C2, angp, ALU.mult, ALU.add, [Tpp], [Tpp])
        self.ts("dve", angp, angp, 3.14159, -3.14159, ALU.min, ALU.max, [Tpp], [Tpp])
        self.act(csp[b][:, 1, :], angp, AF.Sin, [Tpp], [Tcsp[b]])
        self.act(nnp, angp, AF.Abs, [Tpp], [Tpp])
        self.act(csp[b][:, 0, :], nnp, AF.Sin, [Tpp, Tt], [Tcsp[b]], bias=halfpi[:, 0:1], scale=-1.0)

    def pre_proj(ti, h):
        b = ti % 2
        u = (ti * 4 + h) % 2
        pa = 0 if u == 0 else 2
        pv = 1 if u == 0 else 4
        for c in range(2):
            for dc in range(8):
                self.mm(ps[pa][:, c * 128:(c + 1) * 128], Wkv[:, dc, h, c * 128:(c + 1) * 128], hp[b][:, dc, :],
                        dc == 0, dc == 7, [TWkv, Thp[b]], [TP[pa]])
        for dc in range(8):
            self.mm(ps[pv][:, :], hp[b][:, dc, :], Wkv[:, dc, h, 256:768], dc == 0, dc == 7,
                    [TWkv, Thp[b]], [TP[pv]])

    def pre_rest(ti, h):
        b = ti % 2
        u = (ti * 4 + h) % 2
        pa = 0 if u == 0 else 2
        pv = 1 if u == 0 else 4
        cosP, sinP = csp[b][:, 0, :], csp[b][:, 1, :]
        x1 = ps[pa][:, 0:128]
        x2 = ps[pa][:, 128:256]
        self.tt("dve", prt[0][u], x1, cosP, ALU.mult, [TP[pa], Tcsp[b]], [Tprt[0][u]])
        self.tt("dve", prt[1][u], x2, sinP, ALU.mult, [TP[pa], Tcsp[b]], [Tprt[1][u]])
        self.tt("pool", pkT[u][:, 0, :], prt[0][u], prt[1][u], ALU.subtract, [Tprt[0][u], Tprt[1][u]], [Tpk[u]])
        self.tt("dve", prt[2][u], x1, sinP, ALU.mult, [TP[pa], Tcsp[b]], [Tprt[2][u]])
        self.tt("dve", prt[3][u], x2, cosP, ALU.mult, [TP[pa], Tcsp[b]], [Tprt[3][u]])
        self.tt("pool", pkT[u][:, 1, :], prt[2][u], prt[3][u], ALU.add, [Tprt[2][u], Tprt[3][u]], [Tpk[u]])
        self.cp("act", pvb[u], ps[pv][:, :], [TP[pv]], [Tpv[u]])
        for c in range(2):
            self.tr(ps[3][:, c * 128:(c + 1) * 128], pkT[u][:, c, :], [Tpk[u]], [TP[3]])
        self.ts("dve", pkz[u], ps[3][:, 0:256], zet[:, h:h + 1], valid[:, ti:ti + 1], ALU.mult, ALU.mult,
                [TP[3], Tt], [Tpkz[u]])
        for c in range(2):
            pd = 5 + c
            self.mm(ps[pd][:, :], pkz[u][:, c * 128:(c + 1) * 128], pvb[u], True, True, [Tpkz[u], Tpv[u]], [TP[pd]])
            self.stt(S4[:, h, c, :], S4[:, h, c, :], gam[h], ps[pd][:, :], ALU.mult, ALU.add,
                     [TS4[h], TP[pd]], [TS4[h]])

    bodies = [(ti, h) for ti in range(NPREV // 128) for h in range(4)]
    pre_work(0)
    pre_proj(*bodies[0])
    for n, (ti, h) in enumerate(bodies):
        if n + 1 < len(bodies):
            ti2, h2 = bodies[n + 1]
            if h2 == 0:
                pre_work(ti2)
            pre_proj(ti2, h2)
        pre_rest(ti, h)
    S.barrier()
    A.reset(m0)

    hnT = A.alloc([8, TL], BF16)
    Thn = [T(f"hn{g}") for g in range(4)]
    cosL = A.alloc([TL], F32)
    sinL = A.alloc([TL], F32)
    Tcs = T("cs")
    m1 = A.mark()
    scr = self.norm_scratch(7)
    for g in range(4):
        self.norm_mod(self.xT[:, :, g * 512:(g + 1) * 512], self.Tx[g * 4:(g + 1) * 4], 512, coef, sh1,
                      hnT[:, :, g * 512:(g + 1) * 512], [Thn[g]], scr)

    pi32 = A.alloc([512], I32)
    ang = A.alloc([512], F32)
    nn = A.alloc([512], F32)
    Tpos = T("pos")
    for g in range(4):
        sl = slice(g * 512, (g + 1) * 512)
        S.dma("sp", pi32, posloc[0:1, sl].partition_broadcast(128), writes=[Tpos])
        self.cp("dve", ang, pi32, [Tpos], [Tpos])
        self.ts("dve", ang, ang, invf[:, 0:1], None, ALU.mult, None, [Tpos, Tt], [Tpos])
        self.ts("dve", nn, ang, INV2PI, MAGIC, ALU.mult, ALU.add, [Tpos], [Tpos])
        self.ts("dve", nn, nn, -MAGIC, None, ALU.add, None, [Tpos], [Tpos])
        self.stt(ang, nn, -C1, ang, ALU.mult, ALU.add, [Tpos], [Tpos])
        self.stt(ang, nn, -C2, ang, ALU.mult, ALU.add, [Tpos], [Tpos])
        self.ts("dve", ang, ang, 3.14159, -3.14159, ALU.min, ALU.max, [Tpos], [Tpos])
        self.act(sinL[:, sl], ang, AF.Sin, [Tpos], [Tcs])
        self.act(nn, ang, AF.Abs, [Tpos], [Tpos])
        self.act(cosL[:, sl], nn, AF.Sin, [Tpos, Tt], [Tcs], bias=halfpi[:, 0:1], scale=-1.0)
    S.barrier()
    A.reset(m1)

    Wi = A.alloc([8, 1536], BF16)
    Wo = A.alloc([4, 1024], BF16)
    TWi, TWo = T("Wi"), T("Wo")
    S32 = A.alloc([2, 512], F32)
    Sbf = A.alloc([2, 512], BF16)
    TS32, TSbf = T("S32"), T("Sbf")
    NBF = 2

    def dbl(shape, dt, name):
        return [A.alloc(shape, dt) for _ in range(NBF)], [T(f"{name}{i}") for i in range(NBF)]
    rt, Trt = [], []
    for i in range(4):
        a_, t_ = dbl([128], F32, f"rt{i}_")
        rt.append(a_)
        Trt.append(t_)
    kTr, Tk = dbl([2, 128], F32, "kTr")
    kTb, Tkb = dbl([2, 128], BF16, "kTb")
    qTr, Tq = dbl([2, 128], BF16, "qTr")
    qx, Tqx = dbl([2, 128], BF16, "qx")
    vb, Tv = dbl([512], BF16, "vb")
    gs, Tgs = dbl([512], F32, "gs")
    SD, TSD = dbl([128], BF16, "SD")
    kz, Tkz = dbl([256], BF16, "kz")
    junk = A.alloc([512], F32)
    ss, Tss = dbl([1], F32, "ss")
    gy, Tgy = dbl([512], F32, "gy")
    gyT, TgyT = dbl([4, 128], BF16, "gyT")
    print("ret arena words used", A.off, "of", A.n)

    def tile_body(h, tl, full):
        b = tl % NBF
        hsrc, Th = hnT[:, :, tl * 128:(tl + 1) * 128], [Thn[tl // 4]]
        cosT = cosL[:, tl * 128:(tl + 1) * 128]
        sinT = sinL[:, tl * 128:(tl + 1) * 128]
        pa = 0 if b == 0 else 2
        for c in range(2):
            for dc in range(8):
                self.mm(ps[pa][:, c * 128:(c + 1) * 128], Wi[:, dc, 256 + c * 128:256 + (c + 1) * 128],
                        hsrc[:, dc, :], dc == 0, dc == 7, [TWi] + Th, [TP[pa]])
        if full:
            for c in range(2):
                for dc in range(8):
                    self.mm(ps[pa][:, 256 + c * 128:256 + (c + 1) * 128], Wi[:, dc, c * 128:(c + 1) * 128],
                            hsrc[:, dc, :], dc == 0, dc == 7, [TWi] + Th, [TP[pa]])
        for dc in range(8):
            self.mm(ps[1][:, :], hsrc[:, dc, :], Wi[:, dc, 512:1024], dc == 0, dc == 7, [TWi] + Th, [TP[1]])

        def rotary(base, o1, o2, To):
            x1 = ps[pa][:, base:base + 128]
            x2 = ps[pa][:, base + 128:base + 256]
            self.tt("dve", rt[0][b], x1, cosT, ALU.mult, [TP[pa], Tcs], [Trt[0][b]])
            self.tt("dve", rt[1][b], x2, sinT, ALU.mult, [TP[pa], Tcs], [Trt[1][b]])
            self.tt("dve", o1, rt[0][b], rt[1][b], ALU.subtract, [Trt[0][b], Trt[1][b]], [To])
            self.tt("dve", rt[2][b], x1, sinT, ALU.mult, [TP[pa], Tcs], [Trt[2][b]])
            self.tt("dve", rt[3][b], x2, cosT, ALU.mult, [TP[pa], Tcs], [Trt[3][b]])
            self.tt("dve", o2, rt[2][b], rt[3][b], ALU.add, [Trt[2][b], Trt[3][b]], [To])
        rotary(0, kTr[b][:, 0, :], kTr[b][:, 1, :], Tk[b])
        self.cp("act", vb[b], ps[1][:, :], [TP[1]], [Tv[b]])
        if full:
            for dc in range(8):
                self.mm(ps[1][:, :], hsrc[:, dc, :], Wi[:, dc, 1024:1536], dc == 0, dc == 7,
                        [TWi] + Th, [TP[1]])
            self.cp("act", kTb[b], kTr[b], [Tk[b]], [Tkb[b]])
            rotary(256, qTr[b][:, 0, :], qTr[b][:, 1, :], Tq[b])
            self.act(gs[b], ps[1][:, :], AF.Silu, [TP[1]], [Tgs[b]])
            for c in range(2):
                self.mm(ps[3][:, 0:128], kTb[b][:, c, :], qTr[b][:, c, :], c == 0, c == 1, [Tkb[b], Tq[b]], [TP[3]])
            self.tt("dve", SD[b], ps[3][:, 0:128], dect[:, h, :], ALU.mult, [TP[3], Tt], [TSD[b]])
            for c in range(2):
                self.tt("dve", qx[b][:, c, :], qTr[b][:, c, :], xit[:, h, :], ALU.mult, [Tq[b], Tt], [Tqx[b]])
            self.mm(ps[4][:, :], SD[b], vb[b], True, False, [TSD[b], Tv[b]], [TP[4]])
            for c in range(2):
                self.mm(ps[4][:, :], qx[b][:, c, :], Sbf[:, c, :], False, c == 1, [Tqx[b], TSbf], [TP[4]])
            self.act(junk, ps[4][:, :], AF.Square, [TP[4]], [Tss[b]], accum_out=ss[b][:, 0:1])
            self.act(ss[b], ss[b], AF.Sqrt, [Tss[b], self.Tc], [Tss[b]], bias=self.epsb[:, 0:1], scale=1.0 / 512)
            S.op("dve", lambda e: e.reciprocal(ss[b], ss[b]), [Tss[b]], [Tss[b]])
            self.stt(gy[b], ps[4][:, :], ss[b][:, 0:1], gs[b], ALU.mult, ALU.mult, [TP[4], Tss[b], Tgs[b]], [Tgy[b]])
            for fc in range(4):
                self.tr(ps[7][:, fc * 128:(fc + 1) * 128], gy[b][:, fc * 128:(fc + 1) * 128], [Tgy[b]], [TP[7]])
            self.cp("act", gyT[b], ps[7][:, :].rearrange("p (a b) -> p a b", b=128), [TP[7]], [TgyT[b]])
        for c in range(2):
            self.tr(ps[3][:, 128 + c * 128:128 + (c + 1) * 128], kTr[b][:, c, :], [Tk[b]], [TP[3]])
        self.ts("dve", kz[b], ps[3][:, 128:384], zet[:, h:h + 1], None, ALU.mult, None, [TP[3], Tt], [Tkz[b]])
        for c in range(2):
            self.mm(ps[5 + c][:, :], kz[b][:, c * 128:(c + 1) * 128], vb[b], True, True, [Tkz[b], Tv[b]], [TP[5 + c]])
            self.stt(S32[:, c, :], S32[:, c, :], gam[h], ps[5 + c][:, :], ALU.mult, ALU.add,
                     [TS32, TP[5 + c]], [TS32])
        if full:
            self.cp("act", Sbf, S32, [TS32], [TSbf])
            for dc in range(8):
                pk = 5 + dc // 4
                for fc in range(4):
                    self.mm(ps[pk][:, (dc % 4) * 128:(dc % 4 + 1) * 128], Wo[:, fc, dc * 128:(dc + 1) * 128],
                            gyT[b][:, fc, :], fc == 0, fc == 3, [TWo, TgyT[b]], [TP[pk]])
            for dc in range(8):
                pk = 5 + dc // 4
                xs = self.xT[:, dc, tl * 128:(tl + 1) * 128]
                self.stt(xs, ps[pk][:, (dc % 4) * 128:(dc % 4 + 1) * 128], g1[:, dc:dc + 1], xs,
                         ALU.mult, ALU.add, [TP[pk], self.Tm, self.Tx[tl]], [self.Tx[tl]])

    for h in range(4):
        S.dma("pool", Wi, win_d[h], writes=[TWi], max_dma_last_dim=2048)
        S.dma("pool", Wo, wo_d[h], writes=[TWo], max_dma_last_dim=2048)
        self.cp("dve", S32, S4[:, h, :, :], [TS4[h]], [TS32])
        self.cp("act", Sbf, S32, [TS32], [TSbf])
        for tl in range(TL // 128):
            tile_body(h, tl, True)


Prog.phase_ret = _phase_ret


def _phase_peer(self, L):
    S, A, nc = self.S, self.A, self.nc
    ps, TP = self.ps, self.TP
    wq_d = self.dram("peerq", [2, 16, 128, 8, 128])
    kk_d = self.dram("peerk", [2, 128, 16, 128])
    u_d = self.dram("peeru", [2, 128, 128, 8, 128])
    v_d = self.dram("peerv", [2, 128, 128, 1024])
    iota_d = self.dram("iota", [128, 128])
    TB = 256
    NB = TL // TB
    self.issue_conv(L)
    ubf, vbf, Tcv = self.conv[L]
    sh2 = self.modT[:, L, 24:32]
    g2 = self.modT[:, L, 40:48]
    coef = self.coefA[:, L, 1, :]

    Tt = T("ptab")
    keysT = A.alloc([16, 128], BF16)
    iota = A.alloc([128], F32)
    S.dma("pool", keysT, kk_d[L], writes=[Tt])
    S.dma("sp", iota, iota_d, writes=[Tt])
    Gt = A.alloc([TB, 128], BF16)
    TG = T("Gt")
    hn = A.alloc([8, TB], BF16)
    Thn = T("hn")
    m_scr = A.mark()
    qTP = A.alloc([16, TB], BF16)
    NS = 16
    Pb = [A.alloc([NS, 128], BF16) for _ in range(2)]
    TqP = T("qTP")
    TPh = [T("P0"), T("P1")]
    scr = self.norm_scratch(7, TB)
    wqb = [A.alloc([8, 128], BF16) for _ in range(2)]
    Twq = [T("wq0"), T("wq1")]
    sc = A.alloc([16, 128], F32)
    Tsc = T("sc")
    eq2 = sc.rearrange("p a b -> p (a b)").rearrange("p (h r k) -> p h r k", r=16, k=16)
    mr = A.alloc([256], F32)
    Tmr = T("mr")
    mr2 = [mr, A.alloc([256], F32)]
    Tmr2 = [Tmr, T("mrB")]
    Tsth = [T(f"st{i}") for i in range(16)]
    Tbh = [T(f"bh{i}") for i in range(8)]
    stop = A.alloc([16, 16], F32)
    itop = A.alloc([16, 16], U32)
    itopf = A.alloc([16, 16], F32)
    Tst = T("stop")
    cand = A.alloc([8, 256], F32)
    Tcd = T("cand")
    eq = cand.rearrange("p h (r k) -> p h r k", k=16)
    best = A.alloc([8, 16], F32)
    pos = A.alloc([8, 16], U32)
    k1u = A.alloc([8, 16], U32)
    k2u = A.alloc([8, 16], U32)
    k1f = A.alloc([8, 16], F32)
    k2f = A.alloc([8, 16], F32)
    ee = A.alloc([8, 16], F32)
    zz = A.alloc([8], F32)
    Tb = T("best")
    ijg = A.alloc([3, 128], F32)
    Tijg = T("ijg")
    ijgT = A.alloc([3, 128], F32)
    TijgT = T("ijgT")
    Qgb = [A.alloc([NS, 128], BF16) for _ in range(2)]
    TQb = [T("Qg0"), T("Qg1")]
    m_scr_end = A.mark()
    A.reset(m_scr)
    NBUF = 8
    ub = [A.alloc([2, 8, 128], BF16) for _ in range(NBUF)]
    vb = [A.alloc([2, 1024], BF16) for _ in range(NBUF)]
    Tub = [T(f"ub{i}") for i in range(NBUF)]
    Tvb = [T(f"vb{i}") for i in range(NBUF)]
    gel = [A.alloc([TB], BF16) for _ in range(3)]
    Ab = [A.alloc([TB], BF16) for _ in range(3)]
    print("peer arena words used", A.off, m_scr_end, "of", A.n)
    A.off = max(A.off, m_scr_end)
    Tgel = [T("gel0"), T("gel1"), T("gel2")]
    TAb = [T("Ab0"), T("Ab1"), T("Ab2")]
    stop4 = stop.rearrange("p (h two) k -> p h two k", two=2)
    itop4 = itopf.rearrange("p (h two) k -> p h two k", two=2)
    iota16 = iota[:, 0:16].unsqueeze(1).unsqueeze(1).to_broadcast([128, 8, 16, 16])

    for blk in range(NB):
        t0 = blk * TB
        Txb = self.Tx[2 * blk:2 * blk + 2]
        self.norm_mod(self.xT[:, :, t0:t0 + TB], Txb, TB, coef, sh2, hn, [Thn], scr)
        for fc in range(16):
            i = fc % 2
            S.dma("pool", wqb[i], wq_d[L, fc], writes=[Twq[i]])
            for dc in range(8):
                self.mm(ps[i][:, 0:TB], wqb[i][:, dc, :], hn[:, dc, :], dc == 0, dc == 7, [Twq[i], Thn], [TP[i]])
            self.cp("act" if i else "dve", qTP[:, fc, :], ps[i][:, 0:TB], [TP[i]], [TqP])
        for tt in range(2):
            for hp in range(16):
                pk = 2 + hp // 4
                self.mm(ps[pk][:, (hp % 4) * 128:(hp % 4 + 1) * 128], qTP[:, hp, tt * 128:(tt + 1) * 128],
                        keysT[:, hp, :], True, True, [TqP, Tt], [TP[pk]])
            for q4 in range(4):
                self.cp("act" if q4 % 2 else "dve", sc[:, 4 * q4:4 * q4 + 4, :],
                        ps[2 + q4][:, :].rearrange("p (a b) -> p a b", b=128), [TP[2 + q4]], [Tsc])
            V = S
            for hp0 in range(0, 16, 2):
                for step in range(5):
                    for m in range(2):
                        hp = hp0 + m
                        row = sc[:, hp, :]
                        mrm, Tm_, Th_ = mr2[m], Tmr2[m], Tsth[hp]
                        if step == 0:
                            V.op("dve", lambda e, hp=hp, row=row: e.max(stop[:, hp, 0:8], row), [Tsc], [Th_])
                        elif step == 1:
                            V.op("dve", lambda e, hp=hp, row=row: e.max_index(itop[:, hp, 0:8], stop[:, hp, 0:8], row),
                                 [Tsc, Th_], [Th_])
                        elif step == 2:
                            V.op("dve", lambda e, hp=hp, row=row, mrm=mrm: e.match_replace(mrm[:, 0:128], stop[:, hp, 0:8], row, NEG),
                                 [Tsc, Th_], [Tm_])
                        elif step == 3:
                            V.op("dve", lambda e, hp=hp, mrm=mrm: e.max(stop[:, hp, 8:16], mrm[:, 0:128]), [Tm_], [Th_])
                        else:
                            V.op("dve", lambda e, hp=hp, mrm=mrm: e.max_index(itop[:, hp, 8:16], stop[:, hp, 8:16], mrm[:, 0:128]),
                                 [Tm_, Th_], [Th_])
            self.cp("dve", itopf, itop, Tsth, [Tst])
            self.tt("dve", eq, stop4[:, :, 0, :].unsqueeze(3).to_broadcast([128, 8, 16, 16]),
                    stop4[:, :, 1, :].unsqueeze(2).to_broadcast([128, 8, 16, 16]), ALU.add, Tsth + [Tst], [Tcd])
            for h0 in range(0, 8, 2):
                for step in range(5):
                    for m in range(2):
                        h = h0 + m
                        row = cand[:, h, :]
                        mrm, Tm_, Th_ = mr2[m], Tmr2[m], Tbh[h]
                        if step == 0:
                            V.op("dve", lambda e, h=h, row=row: e.max(best[:, h, 0:8], row), [Tcd, Tb], [Th_])
                        elif step == 1:
                            V.op("dve", lambda e, h=h, row=row: e.max_index(pos[:, h, 0:8], best[:, h, 0:8], row),
                                 [Tcd, Th_], [Th_])
                        elif step == 2:
                            V.op("dve", lambda e, h=h, row=row, mrm=mrm: e.match_replace(mrm[:, 0:256], best[:, h, 0:8], row, NEG),
                                 [Tcd, Th_], [Tm_])
                        elif step == 3:
                            V.op("dve", lambda e, h=h, mrm=mrm: e.max(best[:, h, 8:16], mrm[:, 0:256]), [Tm_], [Th_])
                        else:
                            V.op("dve", lambda e, h=h, mrm=mrm: e.max_index(pos[:, h, 8:16], best[:, h, 8:16], mrm[:, 0:256]),
                                 [Tm_, Th_], [Th_])
            self.cp("dve", k2f, pos, Tbh, [Tb])
            gcf = ijg[:, 2, :].rearrange("p (h r) -> p h r", r=16)
            self.tt("dve", ee, best, best[:, :, 0:1].to_broadcast([128, 8, 16]), ALU.subtract, Tbh + [Tb], [Tb])
            self.act(ee, ee, AF.Exp, [Tb], [Tb])
            V.op("dve", lambda e: e.tensor_reduce(zz, ee, AX.X, ALU.add), [Tb], [Tb])
            V.op("dve", lambda e: e.reciprocal(zz, zz), [Tb], [Tb])
            self.tt("dve", gcf, ee, zz.unsqueeze(2).to_broadcast([128, 8, 16]), ALU.mult, [Tb], [Tijg])
            self.ts("dve", k1f, k2f, 0.0625, -0.46875, ALU.mult, ALU.add, [Tb], [Tb])
            self.ts("dve", k1f, k1f, MAGIC, None, ALU.add, None, [Tb], [Tb])
            self.ts("dve", k1f, k1f, -MAGIC, None, ALU.add, None, [Tb], [Tb])
            self.stt(k2f, k1f, -16.0, k2f, ALU.mult, ALU.add, [Tb], [Tb])
            for which, kf, eqb, Teq in ((0, k1f, eq, Tcd), (1, k2f, eq2, Tsc)):
                self.tt("dve", eqb, kf.unsqueeze(3).to_broadcast([128, 8, 16, 16]), iota16, ALU.is_equal,
                        [Tb, Tt], [Teq])
                self.tt("dve", eqb, eqb, itop4[:, :, which, :].unsqueeze(2).to_broadcast([128, 8, 16, 16]),
                        ALU.mult, [Teq, Tst], [Teq])
                dst = ijg[:, which, :].rearrange("p (h r) -> p h r", r=16)
                V.op("dve", lambda e, dst=dst, eqb=eqb: e.tensor_reduce(dst, eqb, AX.X, ALU.add), [Teq], [Tijg])
            for w in range(3):
                self.tr(ps[6][:, w * 128:(w + 1) * 128], ijg[:, w, :], [Tijg], [TP[6]])
            self.cp("act", ijgT, ps[6][:, 0:384].rearrange("p (a b) -> p a b", b=128), [TP[6]], [TijgT])
            for sub in range(128 // NS):
                tsl = slice(sub * NS, (sub + 1) * NS)
                iob = iota.unsqueeze(1).to_broadcast([128, NS, 128])
                P, Qg, TPs, TQ = Pb[sub % 2], Qgb[sub % 2], TPh[sub % 2], TQb[sub % 2]
                self.tt("dve", P, iob, ijgT[:, 0, tsl].unsqueeze(2).to_broadcast([128, NS, 128]), ALU.is_equal,
                        [Tt, TijgT], [TPs])
                self.tt("dve", Qg, iob, ijgT[:, 1, tsl].unsqueeze(2).to_broadcast([128, NS, 128]), ALU.is_equal,
                        [Tt, TijgT], [TQ])
                self.tt("pool", Qg, Qg, ijgT[:, 2, tsl].unsqueeze(2).to_broadcast([128, NS, 128]), ALU.mult,
                        [TQ, TijgT], [TQ])
                for t4 in range(NS // 4):
                    pk = t4 % 2
                    for q in range(4):
                        t = t4 * 4 + q
                        self.mm(ps[pk][:, q * 128:(q + 1) * 128], Qg[:, t, :], P[:, t, :], True, True,
                                [TQ, TPs], [TP[pk]])
                    tok = tt * 128 + sub * NS + t4 * 4
                    self.cp("act", Gt[:, tok:tok + 4, :],
                            ps[pk][:, :].rearrange("p (t i) -> p t i", i=128), [TP[pk]], [TG])
        S.barrier()
        dq = ("sp", "act", "pool")
        for i in range(128):
            k = (i // 2) % NBUF
            if i % 2 == 0:
                S.dma(dq[(i // 2) % 3], ub[k], ubf[i:i + 2].rearrange("i p f -> p i f"),
                      reads=[Tcv[i // 8]], writes=[Tub[k]])
                S.dma(dq[(i // 2 + 1) % 3], vb[k], vbf[i:i + 2].rearrange("i p f -> p i f"),
                      reads=[Tcv[i // 8]], writes=[Tvb[k]])
            sp = 4 + i % 3
            for dc in range(8):
                self.mm(ps[sp][:, 0:TB], ub[k][:, i % 2, dc, :], hn[:, dc, :], dc == 0, dc == 7,
                        [Tub[k], Thn], [TP[sp]])
            g = i % 3
            self.act(gel[g], ps[sp][:, 0:TB], AF.Gelu_apprx_tanh, [TP[sp]], [Tgel[g]])
            self.tt("dve", Ab[g], gel[g], Gt[:, :, i], ALU.mult, [Tgel[g], TG], [TAb[g]])

            def vmm(i):
                k = (i // 2) % NBUF
                g = i % 3
                for dc in range(8):
                    pk = dc // 2
                    self.mm(ps[pk][:, (dc % 2) * TB:(dc % 2 + 1) * TB], vb[k][:, i % 2, dc * 128:(dc + 1) * 128],
                            Ab[g], i == 0 and dc % 2 == 0, i == 127, [Tvb[k], TAb[g]], [TP[pk]])
            if i >= 2:
                vmm(i - 2)
            if i == 127:
                vmm(126)
                vmm(127)
        for dc in range(8):
            pk = dc // 2
            for hh in range(2):
                xs = self.xT[:, dc, t0 + hh * 128:t0 + (hh + 1) * 128]
                self.stt(xs, ps[pk][:, (dc % 2) * TB + hh * 128:(dc % 2) * TB + (hh + 1) * 128], g2[:, dc:dc + 1],
                         xs, ALU.mult, ALU.add, [TP[pk], self.Tm, Txb[hh]], [Txb[hh]])
        S.barrier()


def _issue_conv(self, L):
    if not hasattr(self, "conv"):
        self.conv = {}
    if L in self.conv:
        return
    nc, S = self.nc, self.S
    u_d = self.dram("peeru", [2, 128, 128, 8, 128])
    v_d = self.dram("peerv", [2, 128, 128, 1024])
    ubf = nc.dram_tensor(f"ubf{L}", [128, 128, 1024], BF16, kind="Internal").ap()
    vbf = nc.dram_tensor(f"vbf{L}", [128, 128, 1024], BF16, kind="Internal").ap()
    Tcv = [T(f"cv{L}_{i}") for i in range(16)]
    for k in range(16):
        S.dma("pool", ubf[8 * k:8 * k + 8].rearrange("i p f -> (i p) f"),
              u_d[L, 8 * k:8 * k + 8].rearrange("i p c j -> (i p) (c j)"), writes=[Tcv[k]],
              max_dma_last_dim=2048)
        S.dma("pool", vbf[8 * k:8 * k + 8].rearrange("i p f -> (i p) f"),
              v_d[L, 8 * k:8 * k + 8].rearrange("i p f -> (i p) f"), writes=[Tcv[k]],
              max_dma_last_dim=2048)
    self.conv[L] = (ubf, vbf, Tcv)


def _issue_sgu_conv(self):
    if hasattr(self, "sconv"):
        return
    nc, S = self.nc, self.S
    wu_d = self.dram("sguwu", [24, 128, 8, 128])
    wv_d = self.dram("sguwv", [6, 128, 8, 512])
    wo_d = self.dram("sguwo", [24, 128, 1024])
    wu_b = nc.dram_tensor("sguwu_bf", [24, 128, 1024], BF16, kind="Internal").ap()
    wv_b = nc.dram_tensor("sguwv_bf", [6, 128, 4096], BF16, kind="Internal").ap()
    wo_b = nc.dram_tensor("sguwo_bf", [24, 128, 1024], BF16, kind="Internal").ap()
    Tu, Tv, To = T("cvu"), T("cvv"), T("cvo")
    for k in range(3):
        S.dma("pool", wu_b[8 * k:8 * k + 8].rearrange("f p x -> (f p) x"),
              wu_d[8 * k:8 * k + 8].rearrange("f p c m -> (f p) (c m)"), writes=[Tu], max_dma_last_dim=2048)
        S.dma("pool", wo_b[8 * k:8 * k + 8].rearrange("f p x -> (f p) x"),
              wo_d[8 * k:8 * k + 8].rearrange("f p x -> (f p) x"), writes=[To], max_dma_last_dim=2048)
    for g in range(6):
        S.dma("pool", wv_b[g].rearrange("p (a x) -> (p a) x", a=2),
              wv_d[g].rearrange("p (a c) n -> (p a) (c n)", a=2), writes=[Tv], max_dma_last_dim=2048)
    self.sconv = (wu_b, wv_b, wo_b, Tu, Tv, To)


Prog.issue_sgu_conv = _issue_sgu_conv
Prog.issue_conv = _issue_conv
Prog.phase_peer = _phase_peer


def _phase_sgu(self):
    S, A = self.S, self.A
    ps, TP = self.ps, self.TP
    wu_d = self.dram("sguwu", [24, 128, 8, 128])
    wv_d = self.dram("sguwv", [6, 128, 8, 512])
    bu_d = self.dram("sgubu", [128, 24])
    bv_d = self.dram("sgubv", [1, 3072])
    lng_d = self.dram("sgulng", [128, 24])
    lnb2_d = self.dram("sgulnb2", [2, 3072])
    ws_d = self.dram("sguws", [128, 8, 128])
    bs_d = self.dram("sgubs", [1, 8, 128])
    wo_d = self.dram("sguwo", [24, 128, 1024])
    mask_d = self.dram("sgumask", [128, 128])
    L = 1
    TB = 256
    NB = TL // TB
    sh1 = self.modT[:, L, 0:8]
    g1 = self.modT[:, L, 16:24]
    coef = self.coefA[:, L, 0, :]
    self.issue_sgu_conv()
    wu_b, wv_b, wo_b, Tcu, Tcv_, Tco = self.sconv
    dq = ("sp", "act", "pool")

    Tt = T("stab")
    bu = A.alloc([24], F32)
    lng = A.alloc([24], F32)
    bv = A.alloc([3072], F32)
    lnb2 = A.alloc([3072], F32)
    rhs2 = A.alloc([8, 128], F32)
    WmT = A.alloc([8, 128], BF16)
    Btab = A.alloc([24, 128], F32)
    S.dma("sp", bu, bu_d, writes=[Tt])
    S.dma("sp", lng, lng_d, writes=[Tt])
    S.dma("sp", bv[0:1, :], bv_d, writes=[Tt])
    S.dma("sp", lnb2[0:2, :], lnb2_d, writes=[Tt])
    S.dma("sp", rhs2[1:2, :, :], bs_d, writes=[Tt])
    m1 = A.mark()
    ws = A.alloc([8, 128], F32)
    msk = A.alloc([128], F32)
    wm32 = A.alloc([8, 128], F32)
    Tws = T("ws")
    S.dma("act", ws, ws_d, writes=[Tws])
    S.dma("act", msk, mask_d, writes=[Tws])
    self.tt("dve", ws, ws, msk.unsqueeze(1).to_broadcast([128, 8, 128]), ALU.mult, [Tws], [Tws])
    for half in range(2):
        for q in range(4):
            g = half * 4 + q
            self.tr(ps[6 + half][:, q * 128:(q + 1) * 128], ws[:, g, :], [Tws], [TP[6 + half]])
        self.cp("dve", wm32[:, half * 4:(half + 1) * 4, :], ps[6 + half][:, :].rearrange("p (a b) -> p a b", b=128),
                [TP[6 + half]], [Tws])
    self.cp("act", WmT, wm32, [Tws], [Tt])
    for half in range(2):
        self.mm(ps[6 + half][0:1, :], self.ones[:, 0:1], wm32[:, half * 4:(half + 1) * 4, :].rearrange("p a b -> p (a b)"),
                True, True, [Tws, self.Tc], [TP[6 + half]])
        self.cp("dve", rhs2[0:1, half * 4:(half + 1) * 4, :], ps[6 + half][0:1, :].rearrange("p (a b) -> p a b", b=128),
                [TP[6 + half]], [Tt])
    for fc in range(24):
        pk = 6 + (fc // 4) % 2
        self.mm(ps[pk][:, (fc % 4) * 128:(fc % 4 + 1) * 128], lnb2[0:2, fc * 128:(fc + 1) * 128],
                rhs2[0:2, fc // 3, :], True, True, [Tt], [TP[pk]])
        if fc % 4 == 3:
            self.cp("dve", Btab[:, fc - 3:fc + 1, :], ps[pk][:, :].rearrange("p (a b) -> p a b", b=128),
                    [TP[pk]], [Tt])
    S.barrier()
    A.reset(m1)

    hn = A.alloc([8, TB], BF16)
    Thn = T("hn")
    scr = self.norm_scratch(7, TB)
    uT = A.alloc([24, TB], BF16)
    TuT = T("uT")
    vf = [A.alloc([3072], F32) for _ in range(2)]
    vn = [A.alloc([3072], BF16) for _ in range(2)]
    Tvf = [T("vf0"), T("vf1")]
    Tvn = [T("vn0"), T("vn1")]
    NWU = 4
    wub = [A.alloc([8, 128], BF16) for _ in range(NWU)]
    Twu = [T(f"wu{i}") for i in range(NWU)]
    wvb = [A.alloc([8, 512], BF16) for _ in range(2)]
    Twv = [T("wv0"), T("wv1")]
    NWO = 4
    wob = [A.alloc([1024], BF16) for _ in range(NWO)]
    Two = [T(f"wo{i}") for i in range(NWO)]
    s1 = A.alloc([8], F32)
    s2 = A.alloc([1], F32)
    mu = A.alloc([1], F32)
    var = A.alloc([1], F32)
    nb = A.alloc([1], F32)
    Tst = T("lnstat")
    tmp = [A.alloc([128], F32) for _ in range(2)]
    Ttmp = [T("t0"), T("t1")]
    print("sgu arena words used", A.off, "of", A.n)

    for blk in range(NB):
        t0 = blk * TB
        Txb = self.Tx[2 * blk:2 * blk + 2]
        self.norm_mod(self.xT[:, :, t0:t0 + TB], Txb, TB, coef, sh1, hn, [Thn], scr)
        for fc in range(24):
            i = fc % 2
            w = fc % NWU
            S.dma(dq[fc % 3], wub[w], wu_b[fc], reads=[Tcu], writes=[Twu[w]])
            for dc in range(8):
                self.mm(ps[4 + i][:, 0:TB], wub[w][:, dc, :], hn[:, dc, :], dc == 0, dc == 7, [Twu[w], Thn], [TP[4 + i]])
            self.act(uT[:, fc, :], ps[4 + i][:, 0:TB], AF.Gelu_apprx_tanh, [TP[4 + i], Tt], [TuT], bias=bu[:, fc:fc + 1])
        for cg in range(6):
            i = cg % 2
            S.dma(dq[cg % 3], wvb[i], wv_b[cg], reads=[Tcv_], writes=[Twv[i]])
            for tt in range(2):
                pk = 4 + tt
                self.mm(ps[pk][:, :], self.ones[0:1, :], bv[0:1, cg * 512:(cg + 1) * 512], True, False,
                        [self.Tc, Tt], [TP[pk]])
                for dc in range(8):
                    self.mm(ps[pk][:, :], hn[:, dc, tt * 128:(tt + 1) * 128], wvb[i][:, dc, :], False, dc == 7,
                            [Twv[i], Thn], [TP[pk]])
                self.act(vf[tt][:, cg * 512:(cg + 1) * 512], ps[pk][:, :], AF.Gelu_apprx_tanh, [TP[pk]], [Tvf[tt], Tst],
                         accum_out=s1[:, tt * 4 + cg // 2 * 0 + 0:tt * 4 + 1] if False else None)
        for tt in range(2):
            S.op("dve", lambda e, tt=tt: e.tensor_reduce(mu, vf[tt], AX.X, ALU.add), [Tvf[tt]], [Tst])
            self.act(vn[tt], vf[tt], AF.Square, [Tvf[tt]], [Tvn[tt], Tst], accum_out=s2[:, 0:1])
            self.ts("dve", mu, mu, 1.0 / 3072, None, ALU.mult, None, [Tst], [Tst])
            self.tt("dve", var, mu, mu, ALU.mult, [Tst], [Tst])
            self.stt(var, s2, 1.0 / 3072, var, ALU.mult, ALU.subtract, [Tst], [Tst])
            self.act(var, var, AF.Sqrt, [Tst, self.Tc], [Tst], bias=self.epsb[:, 0:1], scale=1.0)
            S.op("dve", lambda e: e.reciprocal(var, var), [Tst], [Tst])
            self.stt(nb, mu, -1.0, var, ALU.mult, ALU.mult, [Tst], [Tst])
            self.act(vn[tt], vf[tt], AF.Identity, [Tvf[tt], Tst], [Tvn[tt]], bias=nb[:, 0:1], scale=var[:, 0:1])
        for tt in range(2):
            for fc in range(24):
                pk = 6 + fc % 2
                q = (fc // 2) % 4
                self.mm(ps[pk][:, q * 128:(q + 1) * 128], vn[tt][:, fc * 128:(fc + 1) * 128], WmT[:, fc // 3, :],
                        True, True, [Tvn[tt], Tt], [TP[pk]])
                i = fc % 2
                self.stt(tmp[i], ps[pk][:, q * 128:(q + 1) * 128], lng[:, fc:fc + 1], Btab[:, fc, :],
                         ALU.mult, ALU.add, [TP[pk], Tt], [Ttmp[i]])
                usl = uT[:, fc, tt * 128:(tt + 1) * 128]
                self.tt("pool", usl, usl, tmp[i], ALU.mult, [Ttmp[i], TuT], [TuT])
        for fc in range(24):
            k = fc % NWO
            S.dma(dq[fc % 3], wob[k], wo_b[fc], reads=[Tco], writes=[Two[k]])
            for dc in range(8):
                pk = dc // 2
                self.mm(ps[pk][:, (dc % 2) * TB:(dc % 2 + 1) * TB], wob[k][:, dc * 128:(dc + 1) * 128], uT[:, fc, :],
                        fc == 0 and dc % 2 == 0, fc == 23, [Two[k], TuT], [TP[pk]])
        for dc in range(8):
            pk = dc // 2
            for hh in range(2):
                xs = self.xT[:, dc, t0 + hh * 128:t0 + (hh + 1) * 128]
                self.stt(xs, ps[pk][:, (dc % 2) * TB + hh * 128:(dc % 2) * TB + (hh + 1) * 128], g1[:, dc:dc + 1],
                         xs, ALU.mult, ALU.add, [TP[pk], self.Tm, Txb[hh]], [Txb[hh]])


Prog.phase_sgu = _phase_sgu
```

```python
import math
from contextlib import ExitStack

import numpy as np
import concourse.bass as bass
import concourse.mybir as mybir
from concourse.bass_utils import run_bass_kernel_spmd

F32 = mybir.dt.float32
BF16 = mybir.dt.bfloat16
I32 = mybir.dt.int32
U32 = mybir.dt.uint32
ALU = mybir.AluOpType
AF = mybir.ActivationFunctionType
AX = mybir.AxisListType

D = 1024
DC = 8
TL = 2048
NPREV = 6144
EPS = 1e-6
ENGS = ("pe", "act", "dve", "pool", "sp")
NDMA_SEM = 12
SAME_ENGINE_SYNC = True
NEG = -1.0e30


class T:
    __slots__ = ("name", "lw", "rd")

    def __init__(self, name=""):
        self.name = name
        self.lw = None
        self.rd = {}


class Sched:
    def __init__(self, nc, stack):
        self.nc = nc
        self._stack = stack
        self.sem = {e: stack.enter_context(nc.semaphore("s_" + e)) for e in ENGS}
        self.dsem = {}
        for q in ("sp", "act", "pool"):
            for k in range(NDMA_SEM):
                self.dsem[(q, k)] = stack.enter_context(nc.semaphore(f"d_{q}{k}"))
        self.cnt = {e: 0 for e in ENGS}
        self.dcnt = {q: 0 for q in ("sp", "act", "pool")}
        self.waited = {e: {} for e in ENGS}
        self.lists = {e: [] for e in ENGS}

    def _deps(self, eng, reads, writes):
        deps = {}

        def add(sv):
            if sv is None:
                return
            s, v = sv
            if deps.get(s, 0) < v:
                deps[s] = v
        for r in reads:
            add(r.lw)
        for w in writes:
            add(w.lw)
            for s, v in w.rd.items():
                add((s, v))
        out = []
        for s, v in deps.items():
            if s == eng and (eng == "pe" or not SAME_ENGINE_SYNC):
                continue
            if self.waited[eng].get(s, 0) >= v:
                continue
            self.waited[eng][s] = v
            out.append((s, v))
        return out

    def _semof(self, s):
        return self.sem[s] if isinstance(s, str) else self.dsem[s]

    def op(self, eng, fn, reads=(), writes=()):
        waits = self._deps(eng, reads, writes)
        self.cnt[eng] += 1
        v = self.cnt[eng]
        self.lists[eng].append((waits, fn, (self.sem[eng], 1)))
        for r in reads:
            r.rd[eng] = v
        for w in writes:
            w.lw = (eng, v)
            w.rd = {}

    def dma(self, q, out, in_, reads=(), writes=(), **kw):
        j = self.dcnt[q]
        self.dcnt[q] += 1
        src = (q, j % NDMA_SEM)
        val = 16 * (j // NDMA_SEM + 1)
        waits = self._deps(q, reads, writes)
        if j >= NDMA_SEM and self.waited[q].get(src, 0) < val - 16:
            self.waited[q][src] = val - 16
            waits.append((src, val - 16))

        def fn(e, out=out, in_=in_, kw=kw):
            return e.dma_start(out=out, in_=in_, **kw)
        self.lists[q].append((waits, fn, (self.dsem[src], 16)))
        for r in reads:
            r.rd[src] = val
        for w in writes:
            w.lw = (src, val)
            w.rd = {}

    def cc(self, fn, reads=(), writes=()):
        if ("cc", 0) not in self.dsem:
            self.dsem[("cc", 0)] = self._stack.enter_context(self.nc.semaphore("cc"))
            self.ccn = 0
        waits = self._deps("pool", reads, writes)
        for k in range(NDMA_SEM):
            n = (self.dcnt["pool"] - k + NDMA_SEM - 1) // NDMA_SEM
            if n > 0 and 16 * n > self.waited["pool"].get(("pool", k), 0):
                self.waited["pool"][("pool", k)] = 16 * n
                waits.append((("pool", k), 16 * n))
        self.ccn += 1
        src, val = ("cc", 0), self.ccn
        self.lists["pool"].append((waits, fn, (self.dsem[src], 1)))
        self.lists["pool"].append(([(src, val)], None, None))
        self.waited["pool"][src] = val
        for r in reads:
            r.rd[src] = val
        for w in writes:
            w.lw = (src, val)
            w.rd = {}

    def barrier(self):
        for e in ENGS:
            waits = []
            for s in ENGS:
                v = self.cnt[s]
                if s != e and v > self.waited[e].get(s, 0):
                    self.waited[e][s] = v
                    waits.append((s, v))
            for q in ("sp", "act", "pool"):
                for k in range(NDMA_SEM):
                    n = (self.dcnt[q] - k + NDMA_SEM - 1) // NDMA_SEM
                    v = 16 * n
                    if n > 0 and v > self.waited[e].get((q, k), 0):
                        self.waited[e][(q, k)] = v
                        waits.append(((q, k), v))
            if waits:
                self.lists[e].append((waits, None, None))

    def emit(self):
        nc = self.nc
        handles = {"pe": "tensor", "act": "scalar", "dve": "vector", "pool": "gpsimd", "sp": "sync"}
        with nc.Block() as block:
            for e in ENGS:
                def body(eng, lst=self.lists[e]):
                    for waits, fn, inc in lst:
                        for s, v in waits:
                            eng.wait_ge(self._semof(s), v)
                        if fn is not None:
                            fn(eng).then_inc(inc[0], inc[1])
                getattr(block, handles[e])(body)


class Arena:
    def __init__(self, ap, nwords):
        self.ap = ap
        self.n = nwords
        self.off = 0

    def mark(self):
        return self.off

    def reset(self, m):
        self.off = m

    def alloc(self, shape, dtype):
        nel = int(np.prod(shape))
        bpe = 2 if dtype == BF16 else 4
        nw = (nel * bpe + 3) // 4
        nw = (nw + 7) // 8 * 8
        assert self.off + nw <= self.n, f"SBUF arena overflow {self.off}+{nw}>{self.n}"
        v = self.ap[:, self.off:self.off + nw]
        self.off += nw
        if dtype != F32:
            v = v.bitcast(dtype)
        v = v[:, 0:nel]
        if len(shape) == 2:
            v = v.rearrange("p (a b) -> p a b", b=shape[1])
        elif len(shape) == 3:
            v = v.rearrange("p (a b c) -> p a b c", b=shape[1], c=shape[2])
        return v


class Prog:
    def __init__(self, phases, final_norm=True):
        self.phases = phases
        self.final_norm = final_norm
        self.nc = bass.Bass("TRN2", target_bir_lowering=False)
        self.din = {}

    def dram(self, name, shape, dtype=F32):
        if name in self.din:
            return self.din[name]
        t = self.nc.dram_tensor(name, list(shape), dtype, kind="ExternalInput").ap()
        self.din[name] = t
        return t

    def mm(self, out, lhsT, rhs, start, stop, reads, writes):
        self.S.op("pe", lambda e: e.matmul(out, lhsT, rhs, start=start, stop=stop,
                                           skip_group_check=True), reads, writes)

    def tr(self, out, in_, reads, writes):
        idn = self.ident
        self.S.op("pe", lambda e: e.transpose(out, in_, idn), list(reads) + [self.Tc], writes)

    def tt(self, eng, out, in0, in1, op, reads, writes):
        self.S.op(eng, lambda e: e.tensor_tensor(out, in0, in1, op), reads, writes)

    def ts(self, eng, out, in0, s1, s2, op0, op1, reads, writes):
        if s2 is None:
            self.S.op(eng, lambda e: e.tensor_scalar(out, in0, s1, None, op0), reads, writes)
        else:
            self.S.op(eng, lambda e: e.tensor_scalar(out, in0, s1, s2, op0, op1), reads, writes)

    def stt(self, out, in0, sc, in1, op0, op1, reads, writes):
        self.S.op("dve", lambda e: e.scalar_tensor_tensor(out, in0, sc, in1, op0, op1), reads, writes)

    def act(self, out, in_, func, reads, writes, bias=None, scale=None, accum_out=None):
        kw = {}
        if bias is not None:
            kw["bias"] = bias
        if scale is not None:
            kw["scale"] = scale
        if accum_out is not None:
            kw["accum_out"] = accum_out
        self.S.op("act", lambda e: e.activation(out, in_, func, **kw), reads, writes)

    def cp(self, eng, out, in_, reads, writes):
        if eng == "act":
            self.S.op("act", lambda e: e.copy(out, in_), reads, writes)
        else:
            self.S.op(eng, lambda e: e.tensor_copy(out, in_), reads, writes)

    def build(self):
        nc = self.nc
        with ExitStack() as st:
            self.st = st
            self.S = S = Sched(nc, st)
            NW = 52992
            arena_t = st.enter_context(nc.sbuf_tensor("arena", [128, NW], F32))
            self.A = A = Arena(arena_t, NW)
            self.ps = [st.enter_context(nc.psum_tensor(f"ps{k}", [128, 512], F32)) for k in range(8)]
            self.TP = [T(f"ps{k}") for k in range(8)]
            self.Tc = T("consts")

            xloc = self.dram("xloc", [TL, D])
            self.y = nc.dram_tensor("y", [TL, D], F32, kind="ExternalOutput").ap()
            ident_d = self.dram("ident", [128, 128])
            cT_d = self.dram("cT", [128, 8])
            nm_d = self.dram("nmT", [128, 2, 8])
            nf_d = self.dram("nfT", [128, 2, 8])
            nfin_d = self.dram("nfinT", [128, 8])
            adaw_d = self.dram("adaw", [2, 48, 128, 8, 128])
            adab_d = self.dram("adab", [128, 2, 48])

            self.ident = A.alloc([128], F32)
            self.ones = A.alloc([128], F32)
            self.onesb = A.alloc([128], BF16)
            self.epsb = A.alloc([1], F32)
            self.xT = A.alloc([8, TL], F32)
            self.Tx = [T(f"x{g}") for g in range(TL // 128)]
            self.modT = A.alloc([2, 48], F32)
            self.nm = A.alloc([2, 8], F32)
            self.nf = A.alloc([2, 8], F32)
            self.nfin = A.alloc([8], F32)
            self.coefA = A.alloc([2, 2, 8], F32)
            cTt = A.alloc([8], F32)
            adab = A.alloc([2, 48], F32)

            S.dma("sp", self.ident, ident_d, writes=[self.Tc])
            S.op("dve", lambda e: e.memset(self.ones, 1.0), writes=[self.Tc])
            S.op("dve", lambda e: e.memset(self.onesb, 1.0), writes=[self.Tc])
            S.op("dve", lambda e: e.memset(self.epsb, EPS), writes=[self.Tc])
            Tm = T("mod")
            S.dma("sp", cTt, cT_d, writes=[Tm])
            S.dma("sp", self.nm, nm_d, writes=[Tm])
            S.dma("sp", self.nf, nf_d, writes=[Tm])
            S.dma("sp", self.nfin, nfin_d, writes=[Tm])
            S.dma("sp", adab, adab_d, writes=[Tm])
            self.act(cTt, cTt, AF.Silu, [Tm], [Tm])

            m0 = A.mark()
            wbuf = [A.alloc([8, 128], F32) for _ in range(3)]
            Tw = [T(f"adaw{i}") for i in range(3)]
            qs = ("sp", "act")
            for l in range(2):
                for fc in range(48):
                    i = (l * 48 + fc) % 3
                    S.dma(qs[fc % 2], wbuf[i], adaw_d[l, fc], writes=[Tw[i]])
                    for dc in range(8):
                        self.mm(self.ps[0][:, fc:fc + 1], wbuf[i][:, dc, :], cTt[:, dc:dc + 1],
                                dc == 0, dc == 7, [Tw[i], Tm], [self.TP[0]])
                self.tt("dve", self.modT[:, l, :], self.ps[0][:, 0:48], adab[:, l, :], ALU.add,
                        [self.TP[0], Tm], [Tm])
            for l in range(2):
                for sub, gn in ((0, self.nm), (1, self.nf)):
                    sc = self.modT[:, l, (3 * sub + 1) * 8:(3 * sub + 2) * 8]
                    self.stt(self.coefA[:, l, sub, :], sc, 1.0, gn[:, l, :], ALU.add, ALU.mult, [Tm], [Tm])
            self.Tm = Tm
            S.barrier()
            A.reset(m0)

            m0 = A.mark()
            xin = [A.alloc([D], F32) for _ in range(2)]
            Txin = [T("xin0"), T("xin1")]
            for tl in range(TL // 128):
                b = tl % 2
                S.dma(qs[b], xin[b], xloc[tl * 128:(tl + 1) * 128, :], writes=[Txin[b]])
                for half in range(2):
                    pk = 2 * b + half
                    for q4 in range(4):
                        dc = half * 4 + q4
                        self.tr(self.ps[pk][:, q4 * 128:(q4 + 1) * 128], xin[b][:, dc * 128:(dc + 1) * 128],
                                [Txin[b]], [self.TP[pk]])
                    self.cp("act" if half else "dve",
                            self.xT[:, half * 4:(half + 1) * 4, tl * 128:(tl + 1) * 128],
                            self.ps[pk][:, :].rearrange("p (a b) -> p a b", b=128),
                            [self.TP[pk]], [self.Tx[tl]])
            S.barrier()
            A.reset(m0)

            for ph in self.phases:
                m0 = A.mark()
                if ph == "ret":
                    self.phase_ret()
                elif ph == "peer0":
                    self.phase_peer(0)
                elif ph == "sgu":
                    self.phase_sgu()
                elif ph == "peer1":
                    self.phase_peer(1)
                S.barrier()
                A.reset(m0)

            self.phase_final()
            S.emit()
        return nc

    def norm_mod(self, src, Tsrc, n, coef, shift, dst, Tdst, scr):
        S = self.S
        sq, Tsq, rstd, Trs, tmp, Ttmp, pk = scr
        for dc in range(8):
            i = dc % 2
            self.act(sq[i][:, :n], src[:, dc, :], AF.Square, Tsrc, [Tsq[i]])
            self.mm(self.ps[pk][:, :n], self.ones, sq[i][:, :n], dc == 0, dc == 7,
                    [Tsq[i], self.Tc], [self.TP[pk]])
        self.act(rstd[:, :n], self.ps[pk][:, :n], AF.Sqrt, [self.TP[pk], self.Tc], [Trs],
                 bias=self.epsb[:, 0:1], scale=1.0 / D)
        S.op("dve", lambda e: e.reciprocal(rstd[:, :n], rstd[:, :n]), [Trs], [Trs])
        for dc in range(8):
            i = dc % 2
            self.tt("dve", tmp[i][:, :n], src[:, dc, :], rstd[:, :n], ALU.mult, list(Tsrc) + [Trs], [Ttmp[i]])
            if shift is None:
                self.act(dst[:, dc, :], tmp[i][:, :n], AF.Copy, [Ttmp[i], self.Tm], Tdst,
                         scale=coef[:, dc:dc + 1])
            else:
                self.act(dst[:, dc, :], tmp[i][:, :n], AF.Identity, [Ttmp[i], self.Tm], Tdst,
                         bias=shift[:, dc:dc + 1], scale=coef[:, dc:dc + 1])

    def norm_scratch(self, pk, n=512):
        A = self.A
        sq = [A.alloc([n], F32) for _ in range(2)]
        rstd = A.alloc([n], F32)
        tmp = [A.alloc([n], F32) for _ in range(2)]
        return (sq, [T("sq0"), T("sq1")], rstd, T("rstd"), tmp, [T("tmp0"), T("tmp1")], pk)

    def phase_final(self):
        S, A = self.S, self.A
        m0 = A.mark()
        scr = self.norm_scratch(0)
        xn = [A.alloc([8, 512], F32) for _ in range(1)]
        Txn = T("xn")
        ob = [A.alloc([D], F32) for _ in range(2)]
        Tob = [T("ob0"), T("ob1")]
        Tout = T("yout")
        for g in range(TL // 512):
            src = self.xT[:, :, g * 512:(g + 1) * 512]
            Tsrc = self.Tx[g * 4:(g + 1) * 4]
            if self.final_norm:
                self.norm_mod(src, Tsrc, 512, self.nfin, None, xn[0], [Txn], scr)
                s2, Ts2 = xn[0], [Txn]
            else:
                s2, Ts2 = src, Tsrc
            for tt in range(4):
                b = tt % 2
                for half in range(2):
                    pk = 2 + 2 * b + half
                    for q4 in range(4):
                        dc = half * 4 + q4
                        self.tr(self.ps[pk][:, q4 * 128:(q4 + 1) * 128], s2[:, dc, tt * 128:(tt + 1) * 128],
                                Ts2, [self.TP[pk]])
                    self.cp("act" if half else "dve", ob[b][:, half * 512:(half + 1) * 512],
                            self.ps[pk][:, :], [self.TP[pk]], [Tob[b]])
                r0 = g * 512 + tt * 128
                S.dma("sp" if b else "act", self.y[r0:r0 + 128, :], ob[b], reads=[Tob[b]], writes=[Tout])
        waits = S._deps("sp", [Tout], ())
        S.lists["sp"].append((waits, None, None))
        A.reset(m0)


def _consts():
    c = {}
    c["ident"] = np.eye(128, dtype=np.float32)
    half = 128
    c["invf"] = (1.0 / (np.float32(10000.0) ** (np.arange(half, dtype=np.float32) / np.float32(half)))
                 ).astype(np.float32).reshape(128, 1)
    H = 4
    lg = np.log(1.0 - 2.0 ** (-5.0 - np.arange(H, dtype=np.float64)))
    idx = np.arange(128, dtype=np.float64)
    ch = (np.arange(128) // 64)
    dect = np.zeros((128, H, 128), np.float64)
    for h in range(H):
        i = idx[None, :]
        j = idx[:, None]
        same = (ch[None, :] == ch[:, None])
        earlier = (ch[:, None] < ch[None, :])
        w = np.where(same, np.exp(lg[h] * np.abs(i - j)), np.where(earlier, np.exp(lg[h] * (i - j)), 0.0))
        dect[:, h, :] = w / 16.0
    c["dect"] = dect.astype(np.float32)
    xi = np.exp(lg[:, None] * (idx[None, :] + 1.0))
    c["xit"] = np.broadcast_to(xi[None], (128, H, 128)).astype(np.float32).copy()
    ze = np.exp(lg[None, :] * (127.0 - idx[:, None])) / 16.0
    c["zet"] = ze.astype(np.float32)
    c["gam128"] = [float(np.exp(lg[h] * 128.0)) for h in range(H)]
    c["iota"] = np.broadcast_to(np.arange(128, dtype=np.float32)[None], (128, 128)).copy()
    pc = np.arange(128) // 64
    c["sgumask"] = (pc[None, :] <= pc[:, None]).astype(np.float32)
    return c


_CONSTS = _consts()


def _host_layout(inp, names):
    f = np.float32
    x = np.asarray(inp["x"], f)
    pos = np.asarray(inp["positions"], np.int32)
    shared = {}

    def need(n):
        return n in names
    for k in ("ident", "invf", "dect", "xit", "zet", "iota", "sgumask"):
        if need(k):
            shared[k] = _CONSTS[k]
    if need("nmT"):
        shared["nmT"] = np.ascontiguousarray(np.asarray(inp["norm_mix"], f).reshape(2, 8, 128).transpose(2, 0, 1))
        shared["nfT"] = np.ascontiguousarray(np.asarray(inp["norm_ffn"], f).reshape(2, 8, 128).transpose(2, 0, 1))
        shared["nfinT"] = np.ascontiguousarray(np.asarray(inp["norm_final"], f).reshape(8, 128).T)
        aw = np.asarray(inp["ada_w"], f).reshape(2, 8, 128, 48, 128)
        shared["adaw"] = np.ascontiguousarray(aw.transpose(0, 3, 2, 1, 4))
        shared["adab"] = np.ascontiguousarray(np.asarray(inp["ada_b"], f).reshape(2, 48, 128).transpose(2, 0, 1))
    if need("retwin"):
        w = np.asarray(inp["ret_w_in"], f)[0].reshape(8, 128, 6144)
        parts = []
        for h in range(4):
            cols = np.concatenate([np.arange(h * 256, (h + 1) * 256), 1024 + np.arange(h * 256, (h + 1) * 256),
                                   2048 + np.arange(h * 512, (h + 1) * 512), 4096 + np.arange(h * 512, (h + 1) * 512)])
            parts.append(w[:, :, cols].transpose(1, 0, 2))
        shared["retwin"] = np.ascontiguousarray(np.stack(parts))
        wo = np.asarray(inp["ret_w_out"], f)[0].reshape(4, 4, 128, 1024)
        shared["retwo"] = np.ascontiguousarray(wo.transpose(0, 2, 1, 3))
    if need("sguwu"):
        w = np.asarray(inp["sgu_w_in"], f)[0].reshape(8, 128, 6144)
        shared["sguwu"] = np.ascontiguousarray(w[:, :, :3072].reshape(8, 128, 24, 128).transpose(2, 1, 0, 3))
        shared["sguwv"] = np.ascontiguousarray(w[:, :, 3072:].reshape(8, 128, 6, 512).transpose(2, 1, 0, 3))
        b = np.asarray(inp["sgu_b_in"], f)[0]
        shared["sgubu"] = np.ascontiguousarray(b[:3072].reshape(24, 128).T)
        shared["sgubv"] = np.ascontiguousarray(b[3072:].reshape(1, 3072))
        shared["sgulng"] = np.ascontiguousarray(np.asarray(inp["sgu_ln_g"], f)[0].reshape(24, 128).T)
        lb = np.asarray(inp["sgu_ln_b"], f)[0]
        shared["sgulnb2"] = np.ascontiguousarray(np.stack([lb, np.ones_like(lb)]))
        shared["sguws"] = np.ascontiguousarray(np.asarray(inp["sgu_w_s"], f)[0].transpose(1, 0, 2))
        shared["sgubs"] = np.ascontiguousarray(np.asarray(inp["sgu_b_s"], f)[0].reshape(1, 8, 128))
        shared["sguwo"] = np.ascontiguousarray(np.asarray(inp["sgu_w_out"], f)[0].reshape(24, 128, 1024))
    if need("peerq"):
        wq = np.asarray(inp["peer_w_query"], f).reshape(2, 8, 128, 16, 128)
        shared["peerq"] = np.ascontiguousarray(wq.transpose(0, 3, 2, 1, 4))
        sk = np.asarray(inp["peer_sub_keys"], f).reshape(2, 16, 128, 128)
        shared["peerk"] = np.ascontiguousarray(sk.transpose(0, 3, 1, 2))
        u = np.asarray(inp["peer_u"], f).reshape(2, 128, 128, 8, 128)
        shared["peeru"] = np.ascontiguousarray(u.transpose(0, 1, 4, 3, 2))
        shared["peerv"] = np.ascontiguousarray(np.asarray(inp["peer_v"], f).reshape(2, 128, 128, 1024))
    maps = []
    for c in range(8):
        b, s = c // 4, c % 4
        m = dict(shared)
        m["xloc"] = np.ascontiguousarray(x[b, s * TL:(s + 1) * TL])
        m["cT"] = np.ascontiguousarray(np.asarray(inp["c"], f)[b].reshape(8, 128).T)
        if need("wst"):
            lg = np.log(1.0 - 2.0 ** (-5.0 - np.arange(4, dtype=np.float64)))
            w = np.zeros((128, 32), f)
            for c2 in range(8):
                b2, s2 = c2 // 4, c2 % 4
                if b2 == b and s2 < s:
                    for h in range(4):
                        w[:, c2 * 4 + h] = np.exp(lg[h] * TL * (s - s2 - 1))
            m["wst"] = w
            m["posloc"] = np.ascontiguousarray(pos[b, s * TL:(s + 1) * TL].reshape(1, TL))
        if need("xprev"):
            xp = np.zeros((NPREV, D), f)
            pp = np.zeros((1, NPREV), np.int32)
            n = s * TL
            if n:
                xp[NPREV - n:] = x[b, :n]
                pp[0, NPREV - n:] = pos[b, :n]
            m["xprev"] = xp
            m["posprev"] = pp
            m["posloc"] = np.ascontiguousarray(pos[b, s * TL:(s + 1) * TL].reshape(1, TL))
            val = np.zeros((128, NPREV // 128), f)
            val[:, (NPREV - n) // 128:] = 1.0
            m["valid"] = val
        maps.append(m)
    return maps


_PROG_CACHE = {}


def _run(inp, phases, final_norm=True, xoverride=None):
    key = (tuple(phases), final_norm)
    if key not in _PROG_CACHE:
        p = Prog(phases, final_norm)
        p.build()
        _PROG_CACHE[key] = p
    p = _PROG_CACHE[key]
    maps = _host_layout(inp, set(p.din.keys()))
    if xoverride is not None:
        for c in range(8):
            maps[c]["xloc"] = np.ascontiguousarray(xoverride[c])
    maps = [{k: m[k] for k in p.din.keys()} for m in maps]
    res = run_bass_kernel_spmd(p.nc, maps, core_ids=list(range(8)))
    return [np.asarray(r["y"]) for r in res.results]


def kernel(**inputs):
    ys = _run(inputs, ["ret", "peer0", "sgu", "peer1"], True)
    out = np.stack(ys).reshape(2, 4 * TL, D)
    return out.astype(np.float32)


MAGIC = 12582912.0
INV2PI = 0.15915494309189535
C1 = 6.28125
C2 = 0.0019353071795864769


def _phase_ret(self):
    S, A, nc = self.S, self.A, self.nc
    posloc = self.dram("posloc", [1, TL], I32)
    xprev = self.dram("xprev", [NPREV, D])
    posprev = self.dram("posprev", [1, NPREV], I32)
    valid_d = self.dram("valid", [128, NPREV // 128])
    invf_d = self.dram("invf", [128, 1])
    dect_d = self.dram("dect", [128, 4, 128])
    xit_d = self.dram("xit", [128, 4, 128])
    zet_d = self.dram("zet", [128, 4])
    win_d = self.dram("retwin", [4, 128, 8, 1536])
    wo_d = self.dram("retwo", [4, 128, 4, 1024])
    gam = _CONSTS["gam128"]
    ps, TP = self.ps, self.TP
    L = 0
    sh1 = self.modT[:, L, 0:8]
    g1 = self.modT[:, L, 16:24]
    coef = self.coefA[:, L, 0, :]

    Tt = T("rtab")
    invf = A.alloc([1], F32)
    dect = A.alloc([4, 128], F32)
    xit = A.alloc([4, 128], F32)
    zet = A.alloc([4], F32)
    valid = A.alloc([NPREV // 128], F32)
    halfpi = A.alloc([1], F32)
    for dst, src in ((invf, invf_d), (dect, dect_d), (xit, xit_d), (zet, zet_d), (valid, valid_d)):
        S.dma("sp", dst, src, writes=[Tt])
    S.op("dve", lambda e: e.memset(halfpi, math.pi / 2), writes=[Tt])

    S4 = A.alloc([4, 2, 512], F32)
    TS4 = [T(f"S4_{h}") for h in range(4)]
    S.op("dve", lambda e: e.memset(S4, 0.0), writes=TS4)
    m0 = A.mark()
    Wkv = A.alloc([8, 4, 768], BF16)
    TWkv = T("Wkv")
    for h in range(4):
        for dc in range(8):
            S.dma("pool", Wkv[:, dc, h, :], win_d[h][:, dc, 256:1024], writes=[TWkv])
    if "sgu" in self.phases:
        self.issue_sgu_conv()
    for L_ in (0, 1):
        if ("peer%d" % L_) in self.phases:
            self.issue_conv(L_)
    pscr = self.norm_scratch(7, 128)
    xin = [A.alloc([D], F32) for _ in range(2)]
    Txin = [T("xin0"), T("xin1")]
    xpT = [A.alloc([8, 128], F32) for _ in range(2)]
    Txp = [T("xp0"), T("xp1")]
    hp = [A.alloc([8, 128], BF16) for _ in range(2)]
    Thp = [T("hp0"), T("hp1")]
    pi32p = A.alloc([128], I32)
    angp = A.alloc([128], F32)
    nnp = A.alloc([128], F32)
    csp = [A.alloc([2, 128], F32) for _ in range(2)]
    Tpp = T("posp")
    Tcsp = [T("csp0"), T("csp1")]
    prt = [[A.alloc([128], F32) for _ in range(2)] for _ in range(4)]
    Tprt = [[T(f"prt{i}{j}") for j in range(2)] for i in range(4)]
    pkT = [A.alloc([2, 128], F32) for _ in range(2)]
    Tpk = [T("pk0"), T("pk1")]
    pvb = [A.alloc([512], BF16) for _ in range(2)]
    Tpv = [T("pv0"), T("pv1")]
    pkz = [A.alloc([256], BF16) for _ in range(2)]
    Tpkz = [T("pkz0"), T("pkz1")]
    print("ret prepass arena words used", A.off, "of", A.n)
    def pre_work(ti):
        b = ti % 2
        S.dma("sp" if b else "act", xin[b], xprev[ti * 128:(ti + 1) * 128, :], writes=[Txin[b]])
        for half in range(2):
            pk = 5 + half
            for q4 in range(4):
                dc = half * 4 + q4
                self.tr(ps[pk][:, q4 * 128:(q4 + 1) * 128], xin[b][:, dc * 128:(dc + 1) * 128], [Txin[b]], [TP[pk]])
            self.cp("act" if half else "dve", xpT[b][:, half * 4:(half + 1) * 4, :],
                    ps[pk][:, :].rearrange("p (a b) -> p a b", b=128), [TP[pk]], [Txp[b]])
        self.norm_mod(xpT[b], [Txp[b]], 128, coef, sh1, hp[b], [Thp[b]], pscr)
        S.dma("sp", pi32p, posprev[0:1, ti * 128:(ti + 1) * 128].partition_broadcast(128), writes=[Tpp])
        self.cp("dve", angp, pi32p, [Tpp], [Tpp])
        self.ts("dve", angp, angp, invf[:, 0:1], None, ALU.mult, None, [Tpp, Tt], [Tpp])
        self.ts("dve", nnp, angp, INV2PI, MAGIC, ALU.mult, ALU.add, [Tpp], [Tpp])
        self.ts("dve", nnp, nnp, -MAGIC, None, ALU.add, None, [Tpp], [Tpp])
        self.stt(angp, nnp, -C1, angp, ALU.mult, ALU.add, [Tpp], [Tpp])
        self.stt(angp, nnp, -C2, angp, ALU.mult, ALU.add, [Tpp], [Tpp])
        self.ts("dve", angp, angp, 3.14159, -3.14159, ALU.min, ALU.max, [Tpp], [Tpp])
        self.act(csp[b][:, 1, :], angp, AF.Sin, [Tpp], [Tcsp[b]])
        self.act(nnp, angp, AF.Abs, [Tpp], [Tpp])
        self.act(csp[b][:, 0, :], nnp, AF.Sin, [Tpp, Tt], [Tcsp[b]], bias=halfpi[:, 0:1], scale=-1.0)

    def pre_proj(ti, h):
        b = ti % 2
        u = (ti * 4 + h) % 2
        pa = 0 if u == 0 else 2
        pv = 1 if u == 0 else 4
        for c in range(2):
            for dc in range(8):
                self.mm(ps[pa][:, c * 128:(c + 1) * 128], Wkv[:, dc, h, c * 128:(c + 1) * 128], hp[b][:, dc, :],
                        dc == 0, dc == 7, [TWkv, Thp[b]], [TP[pa]])
        for dc in range(8):
            self.mm(ps[pv][:, :], hp[b][:, dc, :], Wkv[:, dc, h, 256:768], dc == 0, dc == 7,
                    [TWkv, Thp[b]], [TP[pv]])

    def pre_rest(ti, h):
        b = ti % 2
        u = (ti * 4 + h) % 2
        pa = 0 if u == 0 else 2
        pv = 1 if u == 0 else 4
        cosP, sinP = csp[b][:, 0, :], csp[b][:, 1, :]
        x1 = ps[pa][:, 0:128]
        x2 = ps[pa][:, 128:256]
        self.tt("dve", prt[0][u], x1, cosP, ALU.mult, [TP[pa], Tcsp[b]], [Tprt[0][u]])
        self.tt("dve", prt[1][u], x2, sinP, ALU.mult, [TP[pa], Tcsp[b]], [Tprt[1][u]])
        self.tt("pool", pkT[u][:, 0, :], prt[0][u], prt[1][u], ALU.subtract, [Tprt[0][u], Tprt[1][u]], [Tpk[u]])
        self.tt("dve", prt[2][u], x1, sinP, ALU.mult, [TP[pa], Tcsp[b]], [Tprt[2][u]])
        self.tt("dve", prt[3][u], x2, cosP, ALU.mult, [TP[pa], Tcsp[b]], [Tprt[3][u]])
        self.tt("pool", pkT[u][:, 1, :], prt[2][u], prt[3][u], ALU.add, [Tprt[2][u], Tprt[3][u]], [Tpk[u]])
        self.cp("act", pvb[u], ps[pv][:, :], [TP[pv]], [Tpv[u]])
        for c in range(2):
            self.tr(ps[3][:, c * 128:(c + 1) * 128], pkT[u][:, c, :], [Tpk[u]], [TP[3]])
        self.ts("dve", pkz[u], ps[3][:, 0:256], zet[:, h:h + 1], valid[:, ti:ti + 1], ALU.mult, ALU.mult,
                [TP[3], Tt], [Tpkz[u]])
        for c in range(2):
            pd = 5 + c
            self.mm(ps[pd][:, :], pkz[u][:, c * 128:(c + 1) * 128], pvb[u], True, True, [Tpkz[u], Tpv[u]], [TP[pd]])
            self.stt(S4[:, h, c, :], S4[:, h, c, :], gam[h], ps[pd][:, :], ALU.mult, ALU.add,
                     [TS4[h], TP[pd]], [TS4[h]])

    bodies = [(ti, h) for ti in range(NPREV // 128) for h in range(4)]
    pre_work(0)
    pre_proj(*bodies[0])
    for n, (ti, h) in enumerate(bodies):
        if n + 1 < len(bodies):
            ti2, h2 = bodies[n + 1]
            if h2 == 0:
                pre_work(ti2)
            pre_proj(ti2, h2)
        pre_rest(ti, h)
    S.barrier()
    A.reset(m0)

    hnT = A.alloc([8, TL], BF16)
    Thn = [T(f"hn{g}") for g in range(4)]
    cosL = A.alloc([TL], F32)
    sinL = A.alloc([TL], F32)
    Tcs = T("cs")
    m1 = A.mark()
    scr = self.norm_scratch(7)
    for g in range(4):
        self.norm_mod(self.xT[:, :, g * 512:(g + 1) * 512], self.Tx[g * 4:(g + 1) * 4], 512, coef, sh1,
                      hnT[:, :, g * 512:(g + 1) * 512], [Thn[g]], scr)

    pi32 = A.alloc([512], I32)
    ang = A.alloc([512], F32)
    nn = A.alloc([512], F32)
    Tpos = T("pos")
    for g in range(4):
        sl = slice(g * 512, (g + 1) * 512)
        S.dma("sp", pi32, posloc[0:1, sl].partition_broadcast(128), writes=[Tpos])
        self.cp("dve", ang, pi32, [Tpos], [Tpos])
        self.ts("dve", ang, ang, invf[:, 0:1], None, ALU.mult, None, [Tpos, Tt], [Tpos])
        self.ts("dve", nn, ang, INV2PI, MAGIC, ALU.mult, ALU.add, [Tpos], [Tpos])
        self.ts("dve", nn, nn, -MAGIC, None, ALU.add, None, [Tpos], [Tpos])
        self.stt(ang, nn, -C1, ang, ALU.mult, ALU.add, [Tpos], [Tpos])
        self.stt(ang, nn, -C2, ang, ALU.mult, ALU.add, [Tpos], [Tpos])
        self.ts("dve", ang, ang, 3.14159, -3.14159, ALU.min, ALU.max, [Tpos], [Tpos])
        self.act(sinL[:, sl], ang, AF.Sin, [Tpos], [Tcs])
        self.act(nn, ang, AF.Abs, [Tpos], [Tpos])
        self.act(cosL[:, sl], nn, AF.Sin, [Tpos, Tt], [Tcs], bias=halfpi[:, 0:1], scale=-1.0)
    S.barrier()
    A.reset(m1)

    Wi = A.alloc([8, 1536], BF16)
    Wo = A.alloc([4, 1024], BF16)
    TWi, TWo = T("Wi"), T("Wo")
    S32 = A.alloc([2, 512], F32)
    Sbf = A.alloc([2, 512], BF16)
    TS32, TSbf = T("S32"), T("Sbf")
    NBF = 2

    def dbl(shape, dt, name):
        return [A.alloc(shape, dt) for _ in range(NBF)], [T(f"{name}{i}") for i in range(NBF)]
    rt, Trt = [], []
    for i in range(4):
        a_, t_ = dbl([128], F32, f"rt{i}_")
        rt.append(a_)
        Trt.append(t_)
    kTr, Tk = dbl([2, 128], F32, "kTr")
    kTb, Tkb = dbl([2, 128], BF16, "kTb")
    qTr, Tq = dbl([2, 128], BF16, "qTr")
    qx, Tqx = dbl([2, 128], BF16, "qx")
    vb, Tv = dbl([512], BF16, "vb")
    gs, Tgs = dbl([512], F32, "gs")
    SD, TSD = dbl([128], BF16, "SD")
    kz, Tkz = dbl([256], BF16, "kz")
    junk = A.alloc([512], F32)
    ss, Tss = dbl([1], F32, "ss")
    gy, Tgy = dbl([512], F32, "gy")
    gyT, TgyT = dbl([4, 128], BF16, "gyT")
    print("ret arena words used", A.off, "of", A.n)

    def tile_body(h, tl, full):
        b = tl % NBF
        hsrc, Th = hnT[:, :, tl * 128:(tl + 1) * 128], [Thn[tl // 4]]
        cosT = cosL[:, tl * 128:(tl + 1) * 128]
        sinT = sinL[:, tl * 128:(tl + 1) * 128]
        pa = 0 if b == 0 else 2
        for c in range(2):
            for dc in range(8):
                self.mm(ps[pa][:, c * 128:(c + 1) * 128], Wi[:, dc, 256 + c * 128:256 + (c + 1) * 128],
                        hsrc[:, dc, :], dc == 0, dc == 7, [TWi] + Th, [TP[pa]])
        if full:
            for c in range(2):
                for dc in range(8):
                    self.mm(ps[pa][:, 256 + c * 128:256 + (c + 1) * 128], Wi[:, dc, c * 128:(c + 1) * 128],
                            hsrc[:, dc, :], dc == 0, dc == 7, [TWi] + Th, [TP[pa]])
        for dc in range(8):
            self.mm(ps[1][:, :], hsrc[:, dc, :], Wi[:, dc, 512:1024], dc == 0, dc == 7, [TWi] + Th, [TP[1]])

        def rotary(base, o1, o2, To):
            x1 = ps[pa][:, base:base + 128]
            x2 = ps[pa][:, base + 128:base + 256]
            self.tt("dve", rt[0][b], x1, cosT, ALU.mult, [TP[pa], Tcs], [Trt[0][b]])
            self.tt("dve", rt[1][b], x2, sinT, ALU.mult, [TP[pa], Tcs], [Trt[1][b]])
            self.tt("dve", o1, rt[0][b], rt[1][b], ALU.subtract, [Trt[0][b], Trt[1][b]], [To])
            self.tt("dve", rt[2][b], x1, sinT, ALU.mult, [TP[pa], Tcs], [Trt[2][b]])
            self.tt("dve", rt[3][b], x2, cosT, ALU.mult, [TP[pa], Tcs], [Trt[3][b]])
            self.tt("dve", o2, rt[2][b], rt[3][b], ALU.add, [Trt[2][b], Trt[3][b]], [To])
        rotary(0, kTr[b][:, 0, :], kTr[b][:, 1, :], Tk[b])
        self.cp("act", vb[b], ps[1][:, :], [TP[1]], [Tv[b]])
        if full:
            for dc in range(8):
                self.mm(ps[1][:, :], hsrc[:, dc, :], Wi[:, dc, 1024:1536], dc == 0, dc == 7,
                        [TWi] + Th, [TP[1]])
            self.cp("act", kTb[b], kTr[b], [Tk[b]], [Tkb[b]])
            rotary(256, qTr[b][:, 0, :], qTr[b][:, 1, :], Tq[b])
            self.act(gs[b], ps[1][:, :], AF.Silu, [TP[1]], [Tgs[b]])
            for c in range(2):
                self.mm(ps[3][:, 0:128], kTb[b][:, c, :], qTr[b][:, c, :], c == 0, c == 1, [Tkb[b], Tq[b]], [TP[3]])
            self.tt("dve", SD[b], ps[3][:, 0:128], dect[:, h, :], ALU.mult, [TP[3], Tt], [TSD[b]])
            for c in range(2):
                self.tt("dve", qx[b][:, c, :], qTr[b][:, c, :], xit[:, h, :], ALU.mult, [Tq[b], Tt], [Tqx[b]])
            self.mm(ps[4][:, :], SD[b], vb[b], True, False, [TSD[b], Tv[b]], [TP[4]])
            for c in range(2):
                self.mm(ps[4][:, :], qx[b][:, c, :], Sbf[:, c, :], False, c == 1, [Tqx[b], TSbf], [TP[4]])
            self.act(junk, ps[4][:, :], AF.Square, [TP[4]], [Tss[b]], accum_out=ss[b][:, 0:1])
            self.act(ss[b], ss[b], AF.Sqrt, [Tss[b], self.Tc], [Tss[b]], bias=self.epsb[:, 0:1], scale=1.0 / 512)
            S.op("dve", lambda e: e.reciprocal(ss[b], ss[b]), [Tss[b]], [Tss[b]])
            self.stt(gy[b], ps[4][:, :], ss[b][:, 0:1], gs[b], ALU.mult, ALU.mult, [TP[4], Tss[b], Tgs[b]], [Tgy[b]])
            for fc in range(4):
                self.tr(ps[7][:, fc * 128:(fc + 1) * 128], gy[b][:, fc * 128:(fc + 1) * 128], [Tgy[b]], [TP[7]])
            self.cp("act", gyT[b], ps[7][:, :].rearrange("p (a b) -> p a b", b=128), [TP[7]], [TgyT[b]])
        for c in range(2):
            self.tr(ps[3][:, 128 + c * 128:128 + (c + 1) * 128], kTr[b][:, c, :], [Tk[b]], [TP[3]])
        self.ts("dve", kz[b], ps[3][:, 128:384], zet[:, h:h + 1], None, ALU.mult, None, [TP[3], Tt], [Tkz[b]])
        for c in range(2):
            self.mm(ps[5 + c][:, :], kz[b][:, c * 128:(c + 1) * 128], vb[b], True, True, [Tkz[b], Tv[b]], [TP[5 + c]])
            self.stt(S32[:, c, :], S32[:, c, :], gam[h], ps[5 + c][:, :], ALU.mult, ALU.add,
                     [TS32, TP[5 + c]], [TS32])
        if full:
            self.cp("act", Sbf, S32, [TS32], [TSbf])
            for dc in range(8):
                pk = 5 + dc // 4
                for fc in range(4):
                    self.mm(ps[pk][:, (dc % 4) * 128:(dc % 4 + 1) * 128], Wo[:, fc, dc * 128:(dc + 1) * 128],
                            gyT[b][:, fc, :], fc == 0, fc == 3, [TWo, TgyT[b]], [TP[pk]])
            for dc in range(8):
                pk = 5 + dc // 4
                xs = self.xT[:, dc, tl * 128:(tl + 1) * 128]
                self.stt(xs, ps[pk][:, (dc % 4) * 128:(dc % 4 + 1) * 128], g1[:, dc:dc + 1], xs,
                         ALU.mult, ALU.add, [TP[pk], self.Tm, self.Tx[tl]], [self.Tx[tl]])

    for h in range(4):
        S.dma("pool", Wi, win_d[h], writes=[TWi], max_dma_last_dim=2048)
        S.dma("pool", Wo, wo_d[h], writes=[TWo], max_dma_last_dim=2048)
        self.cp("dve", S32, S4[:, h, :, :], [TS4[h]], [TS32])
        self.cp("act", Sbf, S32, [TS32], [TSbf])
        for tl in range(TL // 128):
            tile_body(h, tl, True)


Prog.phase_ret = _phase_ret


def _phase_peer(self, L):
    S, A, nc = self.S, self.A, self.nc
    ps, TP = self.ps, self.TP
    wq_d = self.dram("peerq", [2, 16, 128, 8, 128])
    kk_d = self.dram("peerk", [2, 128, 16, 128])
    u_d = self.dram("peeru", [2, 128, 128, 8, 128])
    v_d = self.dram("peerv", [2, 128, 128, 1024])
    iota_d = self.dram("iota", [128, 128])
    TB = 256
    NB = TL // TB
    self.issue_conv(L)
    ubf, vbf, Tcv = self.conv[L]
    sh2 = self.modT[:, L, 24:32]
    g2 = self.modT[:, L, 40:48]
    coef = self.coefA[:, L, 1, :]

    Tt = T("ptab")
    keysT = A.alloc([16, 128], BF16)
    iota = A.alloc([128], F32)
    S.dma("pool", keysT, kk_d[L], writes=[Tt])
    S.dma("sp", iota, iota_d, writes=[Tt])
    Gt = A.alloc([TB, 128], BF16)
    TG = T("Gt")
    hn = A.alloc([8, TB], BF16)
    Thn = T("hn")
    m_scr = A.mark()
    qTP = A.alloc([16, TB], BF16)
    NS = 16
    Pb = [A.alloc([NS, 128], BF16) for _ in range(2)]
    TqP = T("qTP")
    TPh = [T("P0"), T("P1")]
    scr = self.norm_scratch(7, TB)
    wqb = [A.alloc([8, 128], BF16) for _ in range(2)]
    Twq = [T("wq0"), T("wq1")]
    sc = A.alloc([16, 128], F32)
    Tsc = T("sc")
    eq2 = sc.rearrange("p a b -> p (a b)").rearrange("p (h r k) -> p h r k", r=16, k=16)
    mr = A.alloc([256], F32)
    Tmr = T("mr")
    mr2 = [mr, A.alloc([256], F32)]
    Tmr2 = [Tmr, T("mrB")]
    Tsth = [T(f"st{i}") for i in range(16)]
    Tbh = [T(f"bh{i}") for i in range(8)]
    stop = A.alloc([16, 16], F32)
    itop = A.alloc([16, 16], U32)
    itopf = A.alloc([16, 16], F32)
    Tst = T("stop")
    cand = A.alloc([8, 256], F32)
    Tcd = T("cand")
    eq = cand.rearrange("p h (r k) -> p h r k", k=16)
    best = A.alloc([8, 16], F32)
    pos = A.alloc([8, 16], U32)
    k1u = A.alloc([8, 16], U32)
    k2u = A.alloc([8, 16], U32)
    k1f = A.alloc([8, 16], F32)
    k2f = A.alloc([8, 16], F32)
    ee = A.alloc([8, 16], F32)
    zz = A.alloc([8], F32)
    Tb = T("best")
    ijg = A.alloc([3, 128], F32)
    Tijg = T("ijg")
    ijgT = A.alloc([3, 128], F32)
    TijgT = T("ijgT")
    Qgb = [A.alloc([NS, 128], BF16) for _ in range(2)]
    TQb = [T("Qg0"), T("Qg1")]
    m_scr_end = A.mark()
    A.reset(m_scr)
    NBUF = 8
    ub = [A.alloc([2, 8, 128], BF16) for _ in range(NBUF)]
    vb = [A.alloc([2, 1024], BF16) for _ in range(NBUF)]
    Tub = [T(f"ub{i}") for i in range(NBUF)]
    Tvb = [T(f"vb{i}") for i in range(NBUF)]
    gel = [A.alloc([TB], BF16) for _ in range(3)]
    Ab = [A.alloc([TB], BF16) for _ in range(3)]
    print("peer arena words used", A.off, m_scr_end, "of", A.n)
    A.off = max(A.off, m_scr_end)
    Tgel = [T("gel0"), T("gel1"), T("gel2")]
    TAb = [T("Ab0"), T("Ab1"), T("Ab2")]
    stop4 = stop.rearrange("p (h two) k -> p h two k", two=2)
    itop4 = itopf.rearrange("p (h two) k -> p h two k", two=2)
    iota16 = iota[:, 0:16].unsqueeze(1).unsqueeze(1).to_broadcast([128, 8, 16, 16])

    for blk in range(NB):
        t0 = blk * TB
        Txb = self.Tx[2 * blk:2 * blk + 2]
        self.norm_mod(self.xT[:, :, t0:t0 + TB], Txb, TB, coef, sh2, hn, [Thn], scr)
        for fc in range(16):
            i = fc % 2
            S.dma("pool", wqb[i], wq_d[L, fc], writes=[Twq[i]])
            for dc in range(8):
                self.mm(ps[i][:, 0:TB], wqb[i][:, dc, :], hn[:, dc, :], dc == 0, dc == 7, [Twq[i], Thn], [TP[i]])
            self.cp("act" if i else "dve", qTP[:, fc, :], ps[i][:, 0:TB], [TP[i]], [TqP])
        for tt in range(2):
            for hp in range(16):
                pk = 2 + hp // 4
                self.mm(ps[pk][:, (hp % 4) * 128:(hp % 4 + 1) * 128], qTP[:, hp, tt * 128:(tt + 1) * 128],
                        keysT[:, hp, :], True, True, [TqP, Tt], [TP[pk]])
            for q4 in range(4):
                self.cp("act" if q4 % 2 else "dve", sc[:, 4 * q4:4 * q4 + 4, :],
                        ps[2 + q4][:, :].rearrange("p (a b) -> p a b", b=128), [TP[2 + q4]], [Tsc])
            V = S
            for hp0 in range(0, 16, 2):
                for step in range(5):
                    for m in range(2):
                        hp = hp0 + m
                        row = sc[:, hp, :]
                        mrm, Tm_, Th_ = mr2[m], Tmr2[m], Tsth[hp]
                        if step == 0:
                            V.op("dve", lambda e, hp=hp, row=row: e.max(stop[:, hp, 0:8], row), [Tsc], [Th_])
                        elif step == 1:
                            V.op("dve", lambda e, hp=hp, row=row: e.max_index(itop[:, hp, 0:8], stop[:, hp, 0:8], row),
                                 [Tsc, Th_], [Th_])
                        elif step == 2:
                            V.op("dve", lambda e, hp=hp, row=row, mrm=mrm: e.match_replace(mrm[:, 0:128], stop[:, hp, 0:8], row, NEG),
                                 [Tsc, Th_], [Tm_])
                        elif step == 3:
                            V.op("dve", lambda e, hp=hp, mrm=mrm: e.max(stop[:, hp, 8:16], mrm[:, 0:128]), [Tm_], [Th_])
                        else:
                            V.op("dve", lambda e, hp=hp, mrm=mrm: e.max_index(itop[:, hp, 8:16], stop[:, hp, 8:16], mrm[:, 0:128]),
                                 [Tm_, Th_], [Th_])
            self.cp("dve", itopf, itop, Tsth, [Tst])
            self.tt("dve", eq, stop4[:, :, 0, :].unsqueeze(3).to_broadcast([128, 8, 16, 16]),
                    stop4[:, :, 1, :].unsqueeze(2).to_broadcast([128, 8, 16, 16]), ALU.add, Tsth + [Tst], [Tcd])
            for h0 in range(0, 8, 2):
                for step in range(5):
                    for m in range(2):
                        h = h0 + m
                        row = cand[:, h, :]
                        mrm, Tm_, Th_ = mr2[m], Tmr2[m], Tbh[h]
                        if step == 0:
                            V.op("dve", lambda e, h=h, row=row: e.max(best[:, h, 0:8], row), [Tcd, Tb], [Th_])
                        elif step == 1:
                            V.op("dve", lambda e, h=h, row=row: e.max_index(pos[:, h, 0:8], best[:, h, 0:8], row),
                                 [Tcd, Th_], [Th_])
                        elif step == 2:
                            V.op("dve", lambda e, h=h, row=row, mrm=mrm: e.match_replace(mrm[:, 0:256], best[:, h, 0:8], row, NEG),
                                 [Tcd, Th_], [Tm_])
                        elif step == 3:
                            V.op("dve", lambda e, h=h, mrm=mrm: e.max(best[:, h, 8:16], mrm[:, 0:256]), [Tm_], [Th_])
                        else:
                            V.op("dve", lambda e, h=h, mrm=mrm: e.max_index(pos[:, h, 8:16], best[:, h, 8:16], mrm[:, 0:256]),
                                 [Tm_, Th_], [Th_])
            self.cp("dve", k2f, pos, Tbh, [Tb])
            gcf = ijg[:, 2, :].rearrange("p (h r) -> p h r", r=16)
            self.tt("dve", ee, best, best[:, :, 0:1].to_broadcast([128, 8, 16]), ALU.subtract, Tbh + [Tb], [Tb])
            self.act(ee, ee, AF.Exp, [Tb], [Tb])
            V.op("dve", lambda e: e.tensor_reduce(zz, ee, AX.X, ALU.add), [Tb], [Tb])
            V.op("dve", lambda e: e.reciprocal(zz, zz), [Tb], [Tb])
            self.tt("dve", gcf, ee, zz.unsqueeze(2).to_broadcast([128, 8, 16]), ALU.mult, [Tb], [Tijg])
            self.ts("dve", k1f, k2f, 0.0625, -0.46875, ALU.mult, ALU.add, [Tb], [Tb])
            self.ts("dve", k1f, k1f, MAGIC, None, ALU.add, None, [Tb], [Tb])
            self.ts("dve", k1f, k1f, -MAGIC, None, ALU.add, None, [Tb], [Tb])
            self.stt(k2f, k1f, -16.0, k2f, ALU.mult, ALU.add, [Tb], [Tb])
            for which, kf, eqb, Teq in ((0, k1f, eq, Tcd), (1, k2f, eq2, Tsc)):
                self.tt("dve", eqb, kf.unsqueeze(3).to_broadcast([128, 8, 16, 16]), iota16, ALU.is_equal,
                        [Tb, Tt], [Teq])
                self.tt("dve", eqb, eqb, itop4[:, :, which, :].unsqueeze(2).to_broadcast([128, 8, 16, 16]),
                        ALU.mult, [Teq, Tst], [Teq])
                dst = ijg[:, which, :].rearrange("p (h r) -> p h r", r=16)
                V.op("dve", lambda e, dst=dst, eqb=eqb: e.tensor_reduce(dst, eqb, AX.X, ALU.add), [Teq], [Tijg])
            for w in range(3):
                self.tr(ps[6][:, w * 128:(w + 1) * 128], ijg[:, w, :], [Tijg], [TP[6]])
            self.cp("act", ijgT, ps[6][:, 0:384].rearrange("p (a b) -> p a b", b=128), [TP[6]], [TijgT])
            for sub in range(128 // NS):
                tsl = slice(sub * NS, (sub + 1) * NS)
                iob = iota.unsqueeze(1).to_broadcast([128, NS, 128])
                P, Qg, TPs, TQ = Pb[sub % 2], Qgb[sub % 2], TPh[sub % 2], TQb[sub % 2]
                self.tt("dve", P, iob, ijgT[:, 0, tsl].unsqueeze(2).to_broadcast([128, NS, 128]), ALU.is_equal,
                        [Tt, TijgT], [TPs])
                self.tt("dve", Qg, iob, ijgT[:, 1, tsl].unsqueeze(2).to_broadcast([128, NS, 128]), ALU.is_equal,
                        [Tt, TijgT], [TQ])
                self.tt("pool", Qg, Qg, ijgT[:, 2, tsl].unsqueeze(2).to_broadcast([128, NS, 128]), ALU.mult,
                        [TQ, TijgT], [TQ])
                for t4 in range(NS // 4):
                    pk = t4 % 2
                    for q in range(4):
                        t = t4 * 4 + q
                        self.mm(ps[pk][:, q * 128:(q + 1) * 128], Qg[:, t, :], P[:, t, :], True, True,
                                [TQ, TPs], [TP[pk]])
                    tok = tt * 128 + sub * NS + t4 * 4
                    self.cp("act", Gt[:, tok:tok + 4, :],
                            ps[pk][:, :].rearrange("p (t i) -> p t i", i=128), [TP[pk]], [TG])
        S.barrier()
        dq = ("sp", "act", "pool")
        for i in range(128):
            k = (i // 2) % NBUF
            if i % 2 == 0:
                S.dma(dq[(i // 2) % 3], ub[k], ubf[i:i + 2].rearrange("i p f -> p i f"),
                      reads=[Tcv[i // 8]], writes=[Tub[k]])
                S.dma(dq[(i // 2 + 1) % 3], vb[k], vbf[i:i + 2].rearrange("i p f -> p i f"),
                      reads=[Tcv[i // 8]], writes=[Tvb[k]])
            sp = 4 + i % 3
            for dc in range(8):
                self.mm(ps[sp][:, 0:TB], ub[k][:, i % 2, dc, :], hn[:, dc, :], dc == 0, dc == 7,
                        [Tub[k], Thn], [TP[sp]])
            g = i % 3
            self.act(gel[g], ps[sp][:, 0:TB], AF.Gelu_apprx_tanh, [TP[sp]], [Tgel[g]])
            self.tt("dve", Ab[g], gel[g], Gt[:, :, i], ALU.mult, [Tgel[g], TG], [TAb[g]])

            def vmm(i):
                k = (i // 2) % NBUF
                g = i % 3
                for dc in range(8):
                    pk = dc // 2
                    self.mm(ps[pk][:, (dc % 2) * TB:(dc % 2 + 1) * TB], vb[k][:, i % 2, dc * 128:(dc + 1) * 128],
                            Ab[g], i == 0 and dc % 2 == 0, i == 127, [Tvb[k], TAb[g]], [TP[pk]])
            if i >= 2:
                vmm(i - 2)
            if i == 127:
                vmm(126)
                vmm(127)
        for dc in range(8):
            pk = dc // 2
            for hh in range(2):
                xs = self.xT[:, dc, t0 + hh * 128:t0 + (hh + 1) * 128]
                self.stt(xs, ps[pk][:, (dc % 2) * TB + hh * 128:(dc % 2) * TB + (hh + 1) * 128], g2[:, dc:dc + 1],
                         xs, ALU.mult, ALU.add, [TP[pk], self.Tm, Txb[hh]], [Txb[hh]])
        S.barrier()


def _issue_conv(self, L):
    if not hasattr(self, "conv"):
        self.conv = {}
    if L in self.conv:
        return
    nc, S = self.nc, self.S
    u_d = self.dram("peeru", [2, 128, 128, 8, 128])
    v_d = self.dram("peerv", [2, 128, 128, 1024])
    ubf = nc.dram_tensor(f"ubf{L}", [128, 128, 1024], BF16, kind="Internal").ap()
    vbf = nc.dram_tensor(f"vbf{L}", [128, 128, 1024], BF16, kind="Internal").ap()
    Tcv = [T(f"cv{L}_{i}") for i in range(16)]
    for k in range(16):
        S.dma("pool", ubf[8 * k:8 * k + 8].rearrange("i p f -> (i p) f"),
              u_d[L, 8 * k:8 * k + 8].rearrange("i p c j -> (i p) (c j)"), writes=[Tcv[k]],
              max_dma_last_dim=2048)
        S.dma("pool", vbf[8 * k:8 * k + 8].rearrange("i p f -> (i p) f"),
              v_d[L, 8 * k:8 * k + 8].rearrange("i p f -> (i p) f"), writes=[Tcv[k]],
              max_dma_last_dim=2048)
    self.conv[L] = (ubf, vbf, Tcv)


def _issue_sgu_conv(self):
    if hasattr(self, "sconv"):
        return
    nc, S = self.nc, self.S
    wu_d = self.dram("sguwu", [24, 128, 8, 128])
    wv_d = self.dram("sguwv", [6, 128, 8, 512])
    wo_d = self.dram("sguwo", [24, 128, 1024])
    wu_b = nc.dram_tensor("sguwu_bf", [24, 128, 1024], BF16, kind="Internal").ap()
    wv_b = nc.dram_tensor("sguwv_bf", [6, 128, 4096], BF16, kind="Internal").ap()
    wo_b = nc.dram_tensor("sguwo_bf", [24, 128, 1024], BF16, kind="Internal").ap()
    Tu, Tv, To = T("cvu"), T("cvv"), T("cvo")
    for k in range(3):
        S.dma("pool", wu_b[8 * k:8 * k + 8].rearrange("f p x -> (f p) x"),
              wu_d[8 * k:8 * k + 8].rearrange("f p c m -> (f p) (c m)"), writes=[Tu], max_dma_last_dim=2048)
        S.dma("pool", wo_b[8 * k:8 * k + 8].rearrange("f p x -> (f p) x"),
              wo_d[8 * k:8 * k + 8].rearrange("f p x -> (f p) x"), writes=[To], max_dma_last_dim=2048)
    for g in range(6):
        S.dma("pool", wv_b[g].rearrange("p (a x) -> (p a) x", a=2),
              wv_d[g].rearrange("p (a c) n -> (p a) (c n)", a=2), writes=[Tv], max_dma_last_dim=2048)
    self.sconv = (wu_b, wv_b, wo_b, Tu, Tv, To)


Prog.issue_sgu_conv = _issue_sgu_conv
Prog.issue_conv = _issue_conv
Prog.phase_peer = _phase_peer


def _phase_sgu(self):
    S, A = self.S, self.A
    ps, TP = self.ps, self.TP
    wu_d = self.dram("sguwu", [24, 128, 8, 128])
    wv_d = self.dram("sguwv", [6, 128, 8, 512])
    bu_d = self.dram("sgubu", [128, 24])
    bv_d = self.dram("sgubv", [1, 3072])
    lng_d = self.dram("sgulng", [128, 24])
    lnb2_d = self.dram("sgulnb2", [2, 3072])
    ws_d = self.dram("sguws", [128, 8, 128])
    bs_d = self.dram("sgubs", [1, 8, 128])
    wo_d = self.dram("sguwo", [24, 128, 1024])
    mask_d = self.dram("sgumask", [128, 128])
    L = 1
    TB = 256
    NB = TL // TB
    sh1 = self.modT[:, L, 0:8]
    g1 = self.modT[:, L, 16:24]
    coef = self.coefA[:, L, 0, :]
    self.issue_sgu_conv()
    wu_b, wv_b, wo_b, Tcu, Tcv_, Tco = self.sconv
    dq = ("sp", "act", "pool")

    Tt = T("stab")
    bu = A.alloc([24], F32)
    lng = A.alloc([24], F32)
    bv = A.alloc([3072], F32)
    lnb2 = A.alloc([3072], F32)
    rhs2 = A.alloc([8, 128], F32)
    WmT = A.alloc([8, 128], BF16)
    Btab = A.alloc([24, 128], F32)
    S.dma("sp", bu, bu_d, writes=[Tt])
    S.dma("sp", lng, lng_d, writes=[Tt])
    S.dma("sp", bv[0:1, :], bv_d, writes=[Tt])
    S.dma("sp", lnb2[0:2, :], lnb2_d, writes=[Tt])
    S.dma("sp", rhs2[1:2, :, :], bs_d, writes=[Tt])
    m1 = A.mark()
    ws = A.alloc([8, 128], F32)
    msk = A.alloc([128], F32)
    wm32 = A.alloc([8, 128], F32)
    Tws = T("ws")
    S.dma("act", ws, ws_d, writes=[Tws])
    S.dma("act", msk, mask_d, writes=[Tws])
    self.tt("dve", ws, ws, msk.unsqueeze(1).to_broadcast([128, 8, 128]), ALU.mult, [Tws], [Tws])
    for half in range(2):
        for q in range(4):
            g = half * 4 + q
            self.tr(ps[6 + half][:, q * 128:(q + 1) * 128], ws[:, g, :], [Tws], [TP[6 + half]])
        self.cp("dve", wm32[:, half * 4:(half + 1) * 4, :], ps[6 + half][:, :].rearrange("p (a b) -> p a b", b=128),
                [TP[6 + half]], [Tws])
    self.cp("act", WmT, wm32, [Tws], [Tt])
    for half in range(2):
        self.mm(ps[6 + half][0:1, :], self.ones[:, 0:1], wm32[:, half * 4:(half + 1) * 4, :].rearrange("p a b -> p (a b)"),
                True, True, [Tws, self.Tc], [TP[6 + half]])
        self.cp("dve", rhs2[0:1, half * 4:(half + 1) * 4, :], ps[6 + half][0:1, :].rearrange("p (a b) -> p a b", b=128),
                [TP[6 + half]], [Tt])
    for fc in range(24):
        pk = 6 + (fc // 4) % 2
        self.mm(ps[pk][:, (fc % 4) * 128:(fc % 4 + 1) * 128], lnb2[0:2, fc * 128:(fc + 1) * 128],
                rhs2[0:2, fc // 3, :], True, True, [Tt], [TP[pk]])
        if fc % 4 == 3:
            self.cp("dve", Btab[:, fc - 3:fc + 1, :], ps[pk][:, :].rearrange("p (a b) -> p a b", b=128),
                    [TP[pk]], [Tt])
    S.barrier()
    A.reset(m1)

    hn = A.alloc([8, TB], BF16)
    Thn = T("hn")
    scr = self.norm_scratch(7, TB)
    uT = A.alloc([24, TB], BF16)
    TuT = T("uT")
    vf = [A.alloc([3072], F32) for _ in range(2)]
    vn = [A.alloc([3072], BF16) for _ in range(2)]
    Tvf = [T("vf0"), T("vf1")]
    Tvn = [T("vn0"), T("vn1")]
    NWU = 4
    wub = [A.alloc([8, 128], BF16) for _ in range(NWU)]
    Twu = [T(f"wu{i}") for i in range(NWU)]
    wvb = [A.alloc([8, 512], BF16) for _ in range(2)]
    Twv = [T("wv0"), T("wv1")]
    NWO = 4
    wob = [A.alloc([1024], BF16) for _ in range(NWO)]
    Two = [T(f"wo{i}") for i in range(NWO)]
    s1 = A.alloc([8], F32)
    s2 = A.alloc([1], F32)
    mu = A.alloc([1], F32)
    var = A.alloc([1], F32)
    nb = A.alloc([1], F32)
    Tst = T("lnstat")
    tmp = [A.alloc([128], F32) for _ in range(2)]
    Ttmp = [T("t0"), T("t1")]
    print("sgu arena words used", A.off, "of", A.n)

    for blk in range(NB):
        t0 = blk * TB
        Txb = self.Tx[2 * blk:2 * blk + 2]
        self.norm_mod(self.xT[:, :, t0:t0 + TB], Txb, TB, coef, sh1, hn, [Thn], scr)
        for fc in range(24):
            i = fc % 2
            w = fc % NWU
            S.dma(dq[fc % 3], wub[w], wu_b[fc], reads=[Tcu], writes=[Twu[w]])
            for dc in range(8):
                self.mm(ps[4 + i][:, 0:TB], wub[w][:, dc, :], hn[:, dc, :], dc == 0, dc == 7, [Twu[w], Thn], [TP[4 + i]])
            self.act(uT[:, fc, :], ps[4 + i][:, 0:TB], AF.Gelu_apprx_tanh, [TP[4 + i], Tt], [TuT], bias=bu[:, fc:fc + 1])
        for cg in range(6):
            i = cg % 2
            S.dma(dq[cg % 3], wvb[i], wv_b[cg], reads=[Tcv_], writes=[Twv[i]])
            for tt in range(2):
                pk = 4 + tt
                self.mm(ps[pk][:, :], self.ones[0:1, :], bv[0:1, cg * 512:(cg + 1) * 512], True, False,
                        [self.Tc, Tt], [TP[pk]])
                for dc in range(8):
                    self.mm(ps[pk][:, :], hn[:, dc, tt * 128:(tt + 1) * 128], wvb[i][:, dc, :], False, dc == 7,
                            [Twv[i], Thn], [TP[pk]])
                self.act(vf[tt][:, cg * 512:(cg + 1) * 512], ps[pk][:, :], AF.Gelu_apprx_tanh, [TP[pk]], [Tvf[tt], Tst],
                         accum_out=s1[:, tt * 4 + cg // 2 * 0 + 0:tt * 4 + 1] if False else None)
        for tt in range(2):
            S.op("dve", lambda e, tt=tt: e.tensor_reduce(mu, vf[tt], AX.X, ALU.add), [Tvf[tt]], [Tst])
            self.act(vn[tt], vf[tt], AF.Square, [Tvf[tt]], [Tvn[tt], Tst], accum_out=s2[:, 0:1])
            self.ts("dve", mu, mu, 1.0 / 3072, None, ALU.mult, None, [Tst], [Tst])
            self.tt("dve", var, mu, mu, ALU.mult, [Tst], [Tst])
            self.stt(var, s2, 1.0 / 3072, var, ALU.mult, ALU.subtract, [Tst], [Tst])
            self.act(var, var, AF.Sqrt, [Tst, self.Tc], [Tst], bias=self.epsb[:, 0:1], scale=1.0)
            S.op("dve", lambda e: e.reciprocal(var, var), [Tst], [Tst])
            self.stt(nb, mu, -1.0, var, ALU.mult, ALU.mult, [Tst], [Tst])
            self.act(vn[tt], vf[tt], AF.Identity, [Tvf[tt], Tst], [Tvn[tt]], bias=nb[:, 0:1], scale=var[:, 0:1])
        for tt in range(2):
            for fc in range(24):
                pk = 6 + fc % 2
                q = (fc // 2) % 4
                self.mm(ps[pk][:, q * 128:(q + 1) * 128], vn[tt][:, fc * 128:(fc + 1) * 128], WmT[:, fc // 3, :],
                        True, True, [Tvn[tt], Tt], [TP[pk]])
                i = fc % 2
                self.stt(tmp[i], ps[pk][:, q * 128:(q + 1) * 128], lng[:, fc:fc + 1], Btab[:, fc, :],
                         ALU.mult, ALU.add, [TP[pk], Tt], [Ttmp[i]])
                usl = uT[:, fc, tt * 128:(tt + 1) * 128]
                self.tt("pool", usl, usl, tmp[i], ALU.mult, [Ttmp[i], TuT], [TuT])
        for fc in range(24):
            k = fc % NWO
            S.dma(dq[fc % 3], wob[k], wo_b[fc], reads=[Tco], writes=[Two[k]])
            for dc in range(8):
                pk = dc // 2
                self.mm(ps[pk][:, (dc % 2) * TB:(dc % 2 + 1) * TB], wob[k][:, dc * 128:(dc + 1) * 128], uT[:, fc, :],
                        fc == 0 and dc % 2 == 0, fc == 23, [Two[k], TuT], [TP[pk]])
        for dc in range(8):
            pk = dc // 2
            for hh in range(2):
                xs = self.xT[:, dc, t0 + hh * 128:t0 + (hh + 1) * 128]
                self.stt(xs, ps[pk][:, (dc % 2) * TB + hh * 128:(dc % 2) * TB + (hh + 1) * 128], g1[:, dc:dc + 1],
                         xs, ALU.mult, ALU.add, [TP[pk], self.Tm, Txb[hh]], [Txb[hh]])


Prog.phase_sgu = _phase_sgu
```

```python
import math
from contextlib import ExitStack

import numpy as np
import concourse.bass as bass
import concourse.mybir as mybir
from concourse.bass_utils import run_bass_kernel_spmd

F32 = mybir.dt.float32
BF16 = mybir.dt.bfloat16
I32 = mybir.dt.int32
U32 = mybir.dt.uint32
ALU = mybir.AluOpType
AF = mybir.ActivationFunctionType
AX = mybir.AxisListType

D = 1024
DC = 8
TL = 2048
NPREV = 6144
EPS = 1e-6
ENGS = ("pe", "act", "dve", "pool", "sp")
NDMA_SEM = 16
SAME_ENGINE_SYNC = True
NEG = -1.0e30


class T:
    __slots__ = ("name", "lw", "rd")

    def __init__(self, name=""):
        self.name = name
        self.lw = None
        self.rd = {}


class Sched:
    def __init__(self, nc, stack):
        self.nc = nc
        self._stack = stack
        self.sem = {e: stack.enter_context(nc.semaphore("s_" + e)) for e in ENGS}
        self.dsem = {}
        for q in ("sp", "act", "pool"):
            for k in range(NDMA_SEM):
                self.dsem[(q, k)] = stack.enter_context(nc.semaphore(f"d_{q}{k}"))
        self.cnt = {e: 0 for e in ENGS}
        self.dcnt = {q: 0 for q in ("sp", "act", "pool")}
        self.waited = {e: {} for e in ENGS}
        self.lists = {e: [] for e in ENGS}

    def _deps(self, eng, reads, writes):
        deps = {}

        def add(sv):
            if sv is None:
                return
            s, v = sv
            if deps.get(s, 0) < v:
                deps[s] = v
        for r in reads:
            add(r.lw)
        for w in writes:
            add(w.lw)
            for s, v in w.rd.items():
                add((s, v))
        out = []
        for s, v in deps.items():
            if s == eng and (eng == "pe" or not SAME_ENGINE_SYNC):
                continue
            if self.waited[eng].get(s, 0) >= v:
                continue
            self.waited[eng][s] = v
            out.append((s, v))
        return out

    def _semof(self, s):
        return self.sem[s] if isinstance(s, str) else self.dsem[s]

    def op(self, eng, fn, reads=(), writes=()):
        waits = self._deps(eng, reads, writes)
        self.cnt[eng] += 1
        v = self.cnt[eng]
        self.lists[eng].append((waits, fn, (self.sem[eng], 1)))
        for r in reads:
            r.rd[eng] = v
        for w in writes:
            w.lw = (eng, v)
            w.rd = {}

    def dma(self, q, out, in_, reads=(), writes=(), **kw):
        j = self.dcnt[q]
        self.dcnt[q] += 1
        src = (q, j % NDMA_SEM)
        val = 16 * (j // NDMA_SEM + 1)
        waits = self._deps(q, reads, writes)
        if j >= NDMA_SEM and self.waited[q].get(src, 0) < val - 16:
            self.waited[q][src] = val - 16
            waits.append((src, val - 16))

        def fn(e, out=out, in_=in_, kw=kw):
            return e.dma_start(out=out, in_=in_, **kw)
        self.lists[q].append((waits, fn, (self.dsem[src], 16)))
        for r in reads:
            r.rd[src] = val
        for w in writes:
            w.lw = (src, val)
            w.rd = {}

    def cc(self, fn, reads=(), writes=()):
        if ("cc", 0) not in self.dsem:
            self.dsem[("cc", 0)] = self._stack.enter_context(self.nc.semaphore("cc"))
            self.ccn = 0
        waits = self._deps("pool", reads, writes)
        for k in range(NDMA_SEM):
            n = (self.dcnt["pool"] - k + NDMA_SEM - 1) // NDMA_SEM
            if n > 0 and 16 * n > self.waited["pool"].get(("pool", k), 0):
                self.waited["pool"][("pool", k)] = 16 * n
                waits.append((("pool", k), 16 * n))
        self.ccn += 1
        src, val = ("cc", 0), self.ccn
        self.lists["pool"].append((waits, fn, (self.dsem[src], 1)))
        self.lists["pool"].append(([(src, val)], None, None))
        self.waited["pool"][src] = val
        for r in reads:
            r.rd[src] = val
        for w in writes:
            w.lw = (src, val)
            w.rd = {}

    def barrier(self):
        for e in ENGS:
            waits = []
            for s in ENGS:
                v = self.cnt[s]
                if s != e and v > self.waited[e].get(s, 0):
                    self.waited[e][s] = v
                    waits.append((s, v))
            for q in ("sp", "act", "pool"):
                for k in range(NDMA_SEM):
                    n = (self.dcnt[q] - k + NDMA_SEM - 1) // NDMA_SEM
                    v = 16 * n
                    if n > 0 and v > self.waited[e].get((q, k), 0):
                        self.waited[e][(q, k)] = v
                        waits.append(((q, k), v))
            if waits:
                self.lists[e].append((waits, None, None))

    def emit(self):
        nc = self.nc
        handles = {"pe": "tensor", "act": "scalar", "dve": "vector", "pool": "gpsimd", "sp": "sync"}
        with nc.Block() as block:
            for e in ENGS:
                def body(eng, lst=self.lists[e]):
                    for waits, fn, inc in lst:
                        for s, v in waits:
                            eng.wait_ge(self._semof(s), v)
                        if fn is not None:
                            fn(eng).then_inc(inc[0], inc[1])
                getattr(block, handles[e])(body)


class Arena:
    def __init__(self, ap, nwords):
        self.ap = ap
        self.n = nwords
        self.off = 0

    def mark(self):
        return self.off

    def reset(self, m):
        self.off = m

    def alloc(self, shape, dtype):
        nel = int(np.prod(shape))
        bpe = 2 if dtype == BF16 else 4
        nw = (nel * bpe + 3) // 4
        nw = (nw + 7) // 8 * 8
        assert self.off + nw <= self.n, f"SBUF arena overflow {self.off}+{nw}>{self.n}"
        v = self.ap[:, self.off:self.off + nw]
        self.off += nw
        if dtype != F32:
            v = v.bitcast(dtype)
        v = v[:, 0:nel]
        if len(shape) == 2:
            v = v.rearrange("p (a b) -> p a b", b=shape[1])
        elif len(shape) == 3:
            v = v.rearrange("p (a b c) -> p a b c", b=shape[1], c=shape[2])
        return v


class Prog:
    def __init__(self, phases, final_norm=True):
        self.phases = phases
        self.final_norm = final_norm
        self.nc = bass.Bass("TRN2", target_bir_lowering=False)
        self.din = {}

    def dram(self, name, shape, dtype=F32):
        if name in self.din:
            return self.din[name]
        t = self.nc.dram_tensor(name, list(shape), dtype, kind="ExternalInput").ap()
        self.din[name] = t
        return t

    def mm(self, out, lhsT, rhs, start, stop, reads, writes):
        self.S.op("pe", lambda e: e.matmul(out, lhsT, rhs, start=start, stop=stop,
                                           skip_group_check=True), reads, writes)

    def tr(self, out, in_, reads, writes):
        idn = self.ident
        self.S.op("pe", lambda e: e.transpose(out, in_, idn), list(reads) + [self.Tc], writes)

    def tt(self, eng, out, in0, in1, op, reads, writes):
        self.S.op(eng, lambda e: e.tensor_tensor(out, in0, in1, op), reads, writes)

    def ts(self, eng, out, in0, s1, s2, op0, op1, reads, writes):
        if s2 is None:
            self.S.op(eng, lambda e: e.tensor_scalar(out, in0, s1, None, op0), reads, writes)
        else:
            self.S.op(eng, lambda e: e.tensor_scalar(out, in0, s1, s2, op0, op1), reads, writes)

    def stt(self, out, in0, sc, in1, op0, op1, reads, writes):
        self.S.op("dve", lambda e: e.scalar_tensor_tensor(out, in0, sc, in1, op0, op1), reads, writes)

    def act(self, out, in_, func, reads, writes, bias=None, scale=None, accum_out=None):
        kw = {}
        if bias is not None:
            kw["bias"] = bias
        if scale is not None:
            kw["scale"] = scale
        if accum_out is not None:
            kw["accum_out"] = accum_out
        self.S.op("act", lambda e: e.activation(out, in_, func, **kw), reads, writes)

    def cp(self, eng, out, in_, reads, writes):
        if eng == "act":
            self.S.op("act", lambda e: e.copy(out, in_), reads, writes)
        else:
            self.S.op(eng, lambda e: e.tensor_copy(out, in_), reads, writes)

    def build(self):
        nc = self.nc
        with ExitStack() as st:
            self.st = st
            self.S = S = Sched(nc, st)
            NW = 52992
            arena_t = st.enter_context(nc.sbuf_tensor("arena", [128, NW], F32))
            self.A = A = Arena(arena_t, NW)
            self.ps = [st.enter_context(nc.psum_tensor(f"ps{k}", [128, 512], F32)) for k in range(8)]
            self.TP = [T(f"ps{k}") for k in range(8)]
            self.Tc = T("consts")

            xloc = self.dram("xloc", [TL, D])
            self.y = nc.dram_tensor("y", [TL, D], F32, kind="ExternalOutput").ap()
            ident_d = self.dram("ident", [128, 128])
            cT_d = self.dram("cT", [128, 8])
            nm_d = self.dram("nmT", [128, 2, 8])
            nf_d = self.dram("nfT", [128, 2, 8])
            nfin_d = self.dram("nfinT", [128, 8])
            adaw_d = self.dram("adaw", [2, 48, 128, 8, 128])
            adab_d = self.dram("adab", [128, 2, 48])

            self.ident = A.alloc([128], F32)
            self.ones = A.alloc([128], F32)
            self.onesb = A.alloc([128], BF16)
            self.epsb = A.alloc([1], F32)
            self.xT = A.alloc([8, TL], F32)
            self.Tx = [T(f"x{g}") for g in range(TL // 128)]
            self.modT = A.alloc([2, 48], F32)
            self.nm = A.alloc([2, 8], F32)
            self.nf = A.alloc([2, 8], F32)
            self.nfin = A.alloc([8], F32)
            self.coefA = A.alloc([2, 2, 8], F32)
            cTt = A.alloc([8], F32)
            adab = A.alloc([2, 48], F32)

            S.dma("sp", self.ident, ident_d, writes=[self.Tc])
            S.op("dve", lambda e: e.memset(self.ones, 1.0), writes=[self.Tc])
            S.op("dve", lambda e: e.memset(self.onesb, 1.0), writes=[self.Tc])
            S.op("dve", lambda e: e.memset(self.epsb, EPS), writes=[self.Tc])
            Tm = T("mod")
            S.dma("sp", cTt, cT_d, writes=[Tm])
            S.dma("sp", self.nm, nm_d, writes=[Tm])
            S.dma("sp", self.nf, nf_d, writes=[Tm])
            S.dma("sp", self.nfin, nfin_d, writes=[Tm])
            S.dma("sp", adab, adab_d, writes=[Tm])
            self.act(cTt, cTt, AF.Silu, [Tm], [Tm])

            m0 = A.mark()
            wbuf = [A.alloc([8, 128], F32) for _ in range(6)]
            Tw = [T(f"adaw{i}") for i in range(6)]
            qs = ("sp", "act")
            for l in range(2):
                for fc in range(48):
                    i = (l * 48 + fc) % 6
                    S.dma(qs[fc % 2], wbuf[i], adaw_d[l, fc], writes=[Tw[i]])
                    for dc in range(8):
                        self.mm(self.ps[0][:, fc:fc + 1], wbuf[i][:, dc, :], cTt[:, dc:dc + 1],
                                dc == 0, dc == 7, [Tw[i], Tm], [self.TP[0]])
                self.tt("dve", self.modT[:, l, :], self.ps[0][:, 0:48], adab[:, l, :], ALU.add,
                        [self.TP[0], Tm], [Tm])
            for l in range(2):
                for sub, gn in ((0, self.nm), (1, self.nf)):
                    sc = self.modT[:, l, (3 * sub + 1) * 8:(3 * sub + 2) * 8]
                    self.stt(self.coefA[:, l, sub, :], sc, 1.0, gn[:, l, :], ALU.add, ALU.mult, [Tm], [Tm])
            self.Tm = Tm
            S.barrier()
            A.reset(m0)

            m0 = A.mark()
            xin = [A.alloc([D], F32) for _ in range(2)]
            Txin = [T("xin0"), T("xin1")]
            for tl in range(TL // 128):
                b = tl % 2
                S.dma(qs[b], xin[b], xloc[tl * 128:(tl + 1) * 128, :], writes=[Txin[b]])
                for half in range(2):
                    pk = 2 * b + half
                    for q4 in range(4):
                        dc = half * 4 + q4
                        self.tr(self.ps[pk][:, q4 * 128:(q4 + 1) * 128], xin[b][:, dc * 128:(dc + 1) * 128],
                                [Txin[b]], [self.TP[pk]])
                    self.cp("act" if half else "dve",
                            self.xT[:, half * 4:(half + 1) * 4, tl * 128:(tl + 1) * 128],
                            self.ps[pk][:, :].rearrange("p (a b) -> p a b", b=128),
                            [self.TP[pk]], [self.Tx[tl]])
            S.barrier()
            A.reset(m0)

            for ph in self.phases:
                m0 = A.mark()
                if ph == "ret":
                    self.phase_ret()
                elif ph == "peer0":
                    self.phase_peer(0)
                elif ph == "sgu":
                    self.phase_sgu()
                elif ph == "peer1":
                    self.phase_peer(1)
                S.barrier()
                A.reset(m0)

            self.phase_final()
            S.emit()
        return nc

    def norm_mod(self, src, Tsrc, n, coef, shift, dst, Tdst, scr):
        S = self.S
        sq, Tsq, rstd, Trs, tmp, Ttmp, pk = scr
        for dc in range(8):
            i = dc % 2
            self.act(sq[i][:, :n], src[:, dc, :], AF.Square, Tsrc, [Tsq[i]])
            self.mm(self.ps[pk][:, :n], self.ones, sq[i][:, :n], dc == 0, dc == 7,
                    [Tsq[i], self.Tc], [self.TP[pk]])
        self.act(rstd[:, :n], self.ps[pk][:, :n], AF.Sqrt, [self.TP[pk], self.Tc], [Trs],
                 bias=self.epsb[:, 0:1], scale=1.0 / D)
        S.op("dve", lambda e: e.reciprocal(rstd[:, :n], rstd[:, :n]), [Trs], [Trs])
        for dc in range(8):
            i = dc % 2
            self.tt("dve", tmp[i][:, :n], src[:, dc, :], rstd[:, :n], ALU.mult, list(Tsrc) + [Trs], [Ttmp[i]])
            if shift is None:
                self.act(dst[:, dc, :], tmp[i][:, :n], AF.Copy, [Ttmp[i], self.Tm], Tdst,
                         scale=coef[:, dc:dc + 1])
            else:
                self.act(dst[:, dc, :], tmp[i][:, :n], AF.Identity, [Ttmp[i], self.Tm], Tdst,
                         bias=shift[:, dc:dc + 1], scale=coef[:, dc:dc + 1])

    def norm_scratch(self, pk, n=512):
        A = self.A
        sq = [A.alloc([n], F32) for _ in range(2)]
        rstd = A.alloc([n], F32)
        tmp = [A.alloc([n], F32) for _ in range(2)]
        return (sq, [T("sq0"), T("sq1")], rstd, T("rstd"), tmp, [T("tmp0"), T("tmp1")], pk)

    def phase_final(self):
        S, A = self.S, self.A
        m0 = A.mark()
        scr = self.norm_scratch(0)
        xn = [A.alloc([8, 512], F32) for _ in range(1)]
        Txn = T("xn")
        ob = [A.alloc([D], F32) for _ in range(2)]
        Tob = [T("ob0"), T("ob1")]
        Tout = T("yout")
        for g in range(TL // 512):
            src = self.xT[:, :, g * 512:(g + 1) * 512]
            Tsrc = self.Tx[g * 4:(g + 1) * 4]
            if self.final_norm:
                self.norm_mod(src, Tsrc, 512, self.nfin, None, xn[0], [Txn], scr)
                s2, Ts2 = xn[0], [Txn]
            else:
                s2, Ts2 = src, Tsrc
            for tt in range(4):
                b = tt % 2
                for half in range(2):
                    pk = 2 + 2 * b + half
                    for q4 in range(4):
                        dc = half * 4 + q4
                        self.tr(self.ps[pk][:, q4 * 128:(q4 + 1) * 128], s2[:, dc, tt * 128:(tt + 1) * 128],
                                Ts2, [self.TP[pk]])
                    self.cp("act" if half else "dve", ob[b][:, half * 512:(half + 1) * 512],
                            self.ps[pk][:, :], [self.TP[pk]], [Tob[b]])
                r0 = g * 512 + tt * 128
                S.dma("sp" if b else "act", self.y[r0:r0 + 128, :], ob[b], reads=[Tob[b]], writes=[Tout])
        waits = S._deps("sp", [Tout], ())
        S.lists["sp"].append((waits, None, None))
        A.reset(m0)


def _consts():
    c = {}
    c["ident"] = np.eye(128, dtype=np.float32)
    half = 128
    c["invf"] = (1.0 / (np.float32(10000.0) ** (np.arange(half, dtype=np.float32) / np.float32(half)))
                 ).astype(np.float32).reshape(128, 1)
    H = 4
    lg = np.log(1.0 - 2.0 ** (-5.0 - np.arange(H, dtype=np.float64)))
    idx = np.arange(128, dtype=np.float64)
    ch = (np.arange(128) // 64)
    dect = np.zeros((128, H, 128), np.float64)
    for h in range(H):
        i = idx[None, :]
        j = idx[:, None]
        same = (ch[None, :] == ch[:, None])
        earlier = (ch[:, None] < ch[None, :])
        w = np.where(same, np.exp(lg[h] * np.abs(i - j)), np.where(earlier, np.exp(lg[h] * (i - j)), 0.0))
        dect[:, h, :] = w / 16.0
    c["dect"] = dect.astype(np.float32)
    xi = np.exp(lg[:, None] * (idx[None, :] + 1.0))
    c["xit"] = np.broadcast_to(xi[None], (128, H, 128)).astype(np.float32).copy()
    ze = np.exp(lg[None, :] * (127.0 - idx[:, None])) / 16.0
    c["zet"] = ze.astype(np.float32)
    c["gam128"] = [float(np.exp(lg[h] * 128.0)) for h in range(H)]
    c["iota"] = np.broadcast_to(np.arange(128, dtype=np.float32)[None], (128, 128)).copy()
    pc = np.arange(128) // 64
    c["sgumask"] = (pc[None, :] <= pc[:, None]).astype(np.float32)
    return c


_CONSTS = _consts()


def _host_layout(inp, names):
    f = np.float32
    x = np.asarray(inp["x"], f)
    pos = np.asarray(inp["positions"], np.int32)
    shared = {}

    def need(n):
        return n in names
    for k in ("ident", "invf", "dect", "xit", "zet", "iota", "sgumask"):
        if need(k):
            shared[k] = _CONSTS[k]
    if need("nmT"):
        shared["nmT"] = np.ascontiguousarray(np.asarray(inp["norm_mix"], f).reshape(2, 8, 128).transpose(2, 0, 1))
        shared["nfT"] = np.ascontiguousarray(np.asarray(inp["norm_ffn"], f).reshape(2, 8, 128).transpose(2, 0, 1))
        shared["nfinT"] = np.ascontiguousarray(np.asarray(inp["norm_final"], f).reshape(8, 128).T)
        aw = np.asarray(inp["ada_w"], f).reshape(2, 8, 128, 48, 128)
        shared["adaw"] = np.ascontiguousarray(aw.transpose(0, 3, 2, 1, 4))
        shared["adab"] = np.ascontiguousarray(np.asarray(inp["ada_b"], f).reshape(2, 48, 128).transpose(2, 0, 1))
    if need("retwin"):
        w = np.asarray(inp["ret_w_in"], f)[0].reshape(8, 128, 6144)
        parts = []
        for h in range(4):
            cols = np.concatenate([np.arange(h * 256, (h + 1) * 256), 1024 + np.arange(h * 256, (h + 1) * 256),
                                   2048 + np.arange(h * 512, (h + 1) * 512), 4096 + np.arange(h * 512, (h + 1) * 512)])
            parts.append(w[:, :, cols].transpose(1, 0, 2))
        shared["retwin"] = np.ascontiguousarray(np.stack(parts))
        wo = np.asarray(inp["ret_w_out"], f)[0].reshape(4, 4, 128, 1024)
        shared["retwo"] = np.ascontiguousarray(wo.transpose(0, 2, 1, 3))
    if need("sguwu"):
        w = np.asarray(inp["sgu_w_in"], f)[0].reshape(8, 128, 6144)
        shared["sguwu"] = np.ascontiguousarray(w[:, :, :3072].reshape(8, 128, 24, 128).transpose(2, 1, 0, 3))
        shared["sguwv"] = np.ascontiguousarray(w[:, :, 3072:].reshape(8, 128, 6, 512).transpose(2, 1, 0, 3))
        b = np.asarray(inp["sgu_b_in"], f)[0]
        shared["sgubu"] = np.ascontiguousarray(b[:3072].reshape(24, 128).T)
        shared["sgubv"] = np.ascontiguousarray(b[3072:].reshape(1, 3072))
        shared["sgulng"] = np.ascontiguousarray(np.asarray(inp["sgu_ln_g"], f)[0].reshape(24, 128).T)
        lb = np.asarray(inp["sgu_ln_b"], f)[0]
        shared["sgulnb2"] = np.ascontiguousarray(np.stack([lb, np.ones_like(lb)]))
        shared["sguws"] = np.ascontiguousarray(np.asarray(inp["sgu_w_s"], f)[0].transpose(1, 0, 2))
        shared["sgubs"] = np.ascontiguousarray(np.asarray(inp["sgu_b_s"], f)[0].reshape(1, 8, 128))
        shared["sguwo"] = np.ascontiguousarray(np.asarray(inp["sgu_w_out"], f)[0].reshape(24, 128, 1024))
    if need("peerq"):
        wq = np.asarray(inp["peer_w_query"], f).reshape(2, 8, 128, 16, 128)
        shared["peerq"] = np.ascontiguousarray(wq.transpose(0, 3, 2, 1, 4))
        sk = np.asarray(inp["peer_sub_keys"], f).reshape(2, 16, 128, 128)
        shared["peerk"] = np.ascontiguousarray(sk.transpose(0, 3, 1, 2))
        u = np.asarray(inp["peer_u"], f).reshape(2, 128, 128, 8, 128)
        shared["peeru"] = np.ascontiguousarray(u.transpose(0, 1, 4, 3, 2))
        shared["peerv"] = np.ascontiguousarray(np.asarray(inp["peer_v"], f).reshape(2, 128, 128, 1024))
    maps = []
    for c in range(8):
        b, s = c // 4, c % 4
        m = dict(shared)
        m["xloc"] = np.ascontiguousarray(x[b, s * TL:(s + 1) * TL])
        m["cT"] = np.ascontiguousarray(np.asarray(inp["c"], f)[b].reshape(8, 128).T)
        if need("wst"):
            lg = np.log(1.0 - 2.0 ** (-5.0 - np.arange(4, dtype=np.float64)))
            w = np.zeros((128, 32), f)
            for c2 in range(8):
                b2, s2 = c2 // 4, c2 % 4
                if b2 == b and s2 < s:
                    for h in range(4):
                        w[:, c2 * 4 + h] = np.exp(lg[h] * TL * (s - s2 - 1))
            m["wst"] = w
            m["posloc"] = np.ascontiguousarray(pos[b, s * TL:(s + 1) * TL].reshape(1, TL))
        if need("xprev"):
            xp = np.zeros((NPREV, D), f)
            pp = np.zeros((1, NPREV), np.int32)
            n = s * TL
            if n:
                xp[NPREV - n:] = x[b, :n]
                pp[0, NPREV - n:] = pos[b, :n]
            m["xprev"] = xp
            m["posprev"] = pp
            m["posloc"] = np.ascontiguousarray(pos[b, s * TL:(s + 1) * TL].reshape(1, TL))
            val = np.zeros((128, NPREV // 128), f)
            val[:, (NPREV - n) // 128:] = 1.0
            m["valid"] = val
        maps.append(m)
    return maps


_PROG_CACHE = {}


def _run(inp, phases, final_norm=True, xoverride=None):
    key = (tuple(phases), final_norm)
    if key not in _PROG_CACHE:
        p = Prog(phases, final_norm)
        p.build()
        _PROG_CACHE[key] = p
    p = _PROG_CACHE[key]
    maps = _host_layout(inp, set(p.din.keys()))
    if xoverride is not None:
        for c in range(8):
            maps[c]["xloc"] = np.ascontiguousarray(xoverride[c])
    maps = [{k: m[k] for k in p.din.keys()} for m in maps]
    res = run_bass_kernel_spmd(p.nc, maps, core_ids=list(range(8)))
    return [np.asarray(r["y"]) for r in res.results]


def kernel(**inputs):
    ys = _run(inputs, ["ret", "peer0", "sgu", "peer1"], True)
    out = np.stack(ys).reshape(2, 4 * TL, D)
    return out.astype(np.float32)


MAGIC = 12582912.0
INV2PI = 0.15915494309189535
C1 = 6.28125
C2 = 0.0019353071795864769


def _phase_ret(self):
    S, A, nc = self.S, self.A, self.nc
    posloc = self.dram("posloc", [1, TL], I32)
    xprev = self.dram("xprev", [NPREV, D])
    posprev = self.dram("posprev", [1, NPREV], I32)
    valid_d = self.dram("valid", [128, NPREV // 128])
    invf_d = self.dram("invf", [128, 1])
    dect_d = self.dram("dect", [128, 4, 128])
    xit_d = self.dram("xit", [128, 4, 128])
    zet_d = self.dram("zet", [128, 4])
    win_d = self.dram("retwin", [4, 128, 8, 1536])
    wo_d = self.dram("retwo", [4, 128, 4, 1024])
    gam = _CONSTS["gam128"]
    ps, TP = self.ps, self.TP
    L = 0
    sh1 = self.modT[:, L, 0:8]
    g1 = self.modT[:, L, 16:24]
    coef = self.coefA[:, L, 0, :]

    Tt = T("rtab")
    invf = A.alloc([1], F32)
    dect = A.alloc([4, 128], F32)
    xit = A.alloc([4, 128], F32)
    zet = A.alloc([4], F32)
    valid = A.alloc([NPREV // 128], F32)
    halfpi = A.alloc([1], F32)
    for dst, src in ((invf, invf_d), (dect, dect_d), (xit, xit_d), (zet, zet_d), (valid, valid_d)):
        S.dma("sp", dst, src, writes=[Tt])
    S.op("dve", lambda e: e.memset(halfpi, math.pi / 2), writes=[Tt])

    S4 = A.alloc([4, 2, 512], F32)
    TS4 = [T(f"S4_{h}") for h in range(4)]
    S.op("dve", lambda e: e.memset(S4, 0.0), writes=TS4)
    m0 = A.mark()
    Wkv = A.alloc([8, 4, 768], BF16)
    TWkv = T("Wkv")
    for h in range(4):
        for dc in range(8):
            S.dma("pool", Wkv[:, dc, h, :], win_d[h][:, dc, 256:1024], writes=[TWkv])
    if "sgu" in self.phases:
        self.issue_sgu_conv()
    for L_ in (0, 1):
        if ("peer%d" % L_) in self.phases:
            self.issue_conv(L_)
    pscr = self.norm_scratch(7, 128)
    xin = [A.alloc([D], F32) for _ in range(2)]
    Txin = [T("xin0"), T("xin1")]
    xpT = [A.alloc([8, 128], F32) for _ in range(2)]
    Txp = [T("xp0"), T("xp1")]
    hp = [A.alloc([8, 128], BF16) for _ in range(2)]
    Thp = [T("hp0"), T("hp1")]
    pi32p = A.alloc([128], I32)
    angp = A.alloc([128], F32)
    nnp = A.alloc([128], F32)
    csp = [A.alloc([2, 128], F32) for _ in range(2)]
    Tpp = T("posp")
    Tcsp = [T("csp0"), T("csp1")]
    prt = [[A.alloc([128], F32) for _ in range(2)] for _ in range(4)]
    Tprt = [[T(f"prt{i}{j}") for j in range(2)] for i in range(4)]
    pkT = [A.alloc([2, 128], F32) for _ in range(2)]
    Tpk = [T("pk0"), T("pk1")]
    pvb = [A.alloc([512], BF16) for _ in range(2)]
    Tpv = [T("pv0"), T("pv1")]
    pkz = [A.alloc([256], BF16) for _ in range(2)]
    Tpkz = [T("pkz0"), T("pkz1")]
    print("ret prepass arena words used", A.off, "of", A.n)
    def pre_work(ti):
        b = ti % 2
        S.dma("sp" if b else "act", xin[b], xprev[ti * 128:(ti + 1) * 128, :], writes=[Txin[b]])
        for half in range(2):
            pk = 5 + half
            for q4 in range(4):
                dc = half * 4 + q4
                self.tr(ps[pk][:, q4 * 128:(q4 + 1) * 128], xin[b][:, dc * 128:(dc + 1) * 128], [Txin[b]], [TP[pk]])
            self.cp("act" if half else "dve", xpT[b][:, half * 4:(half + 1) * 4, :],
                    ps[pk][:, :].rearrange("p (a b) -> p a b", b=128), [TP[pk]], [Txp[b]])
        self.norm_mod(xpT[b], [Txp[b]], 128, coef, sh1, hp[b], [Thp[b]], pscr)
        S.dma("sp", pi32p, posprev[0:1, ti * 128:(ti + 1) * 128].partition_broadcast(128), writes=[Tpp])
        self.cp("dve", angp, pi32p, [Tpp], [Tpp])
        self.ts("dve", angp, angp, invf[:, 0:1], None, ALU.mult, None, [Tpp, Tt], [Tpp])
        self.ts("dve", nnp, angp, INV2PI, MAGIC, ALU.mult, ALU.add, [Tpp], [Tpp])
        self.ts("dve", nnp, nnp, -MAGIC, None, ALU.add, None, [Tpp], [Tpp])
        self.stt(angp, nnp, -C1, angp, ALU.mult, ALU.add, [Tpp], [Tpp])
        self.stt(angp, nnp, -C2, angp, ALU.mult, ALU.add, [Tpp], [Tpp])
        self.ts("dve", angp, angp, 3.14159, -3.14159, ALU.min, ALU.max, [Tpp], [Tpp])
        self.act(csp[b][:, 1, :], angp, AF.Sin, [Tpp], [Tcsp[b]])
        self.act(nnp, angp, AF.Abs, [Tpp], [Tpp])
        self.act(csp[b][:, 0, :], nnp, AF.Sin, [Tpp, Tt], [Tcsp[b]], bias=halfpi[:, 0:1], scale=-1.0)

    def pre_proj(ti, h):
        b = ti % 2
        u = (ti * 4 + h) % 2
        pa = 0 if u == 0 else 2
        pv = 1 if u == 0 else 4
        for c in range(2):
            for dc in range(8):
                self.mm(ps[pa][:, c * 128:(c + 1) * 128], Wkv[:, dc, h, c * 128:(c + 1) * 128], hp[b][:, dc, :],
                        dc == 0, dc == 7, [TWkv, Thp[b]], [TP[pa]])
        for dc in range(8):
            self.mm(ps[pv][:, :], hp[b][:, dc, :], Wkv[:, dc, h, 256:768], dc == 0, dc == 7,
                    [TWkv, Thp[b]], [TP[pv]])

    def pre_rest(ti, h):
        b = ti % 2
        u = (ti * 4 + h) % 2
        pa = 0 if u == 0 else 2
        pv = 1 if u == 0 else 4
        cosP, sinP = csp[b][:, 0, :], csp[b][:, 1, :]
        x1 = ps[pa][:, 0:128]
        x2 = ps[pa][:, 128:256]
        self.tt("dve", prt[0][u], x1, cosP, ALU.mult, [TP[pa], Tcsp[b]], [Tprt[0][u]])
        self.tt("dve", prt[1][u], x2, sinP, ALU.mult, [TP[pa], Tcsp[b]], [Tprt[1][u]])
        self.tt("pool", pkT[u][:, 0, :], prt[0][u], prt[1][u], ALU.subtract, [Tprt[0][u], Tprt[1][u]], [Tpk[u]])
        self.tt("dve", prt[2][u], x1, sinP, ALU.mult, [TP[pa], Tcsp[b]], [Tprt[2][u]])
        self.tt("dve", prt[3][u], x2, cosP, ALU.mult, [TP[pa], Tcsp[b]], [Tprt[3][u]])
        self.tt("pool", pkT[u][:, 1, :], prt[2][u], prt[3][u], ALU.add, [Tprt[2][u], Tprt[3][u]], [Tpk[u]])
        self.cp("act", pvb[u], ps[pv][:, :], [TP[pv]], [Tpv[u]])
        for c in range(2):
            self.tr(ps[3][:, c * 128:(c + 1) * 128], pkT[u][:, c, :], [Tpk[u]], [TP[3]])
        self.ts("dve", pkz[u], ps[3][:, 0:256], zet[:, h:h + 1], valid[:, ti:ti + 1], ALU.mult, ALU.mult,
                [TP[3], Tt], [Tpkz[u]])
        for c in range(2):
            pd = 5 + c
            self.mm(ps[pd][:, :], pkz[u][:, c * 128:(c + 1) * 128], pvb[u], True, True, [Tpkz[u], Tpv[u]], [TP[pd]])
            self.stt(S4[:, h, c, :], S4[:, h, c, :], gam[h], ps[pd][:, :], ALU.mult, ALU.add,
                     [TS4[h], TP[pd]], [TS4[h]])

    bodies = [(ti, h) for ti in range(NPREV // 128) for h in range(4)]
    pre_work(0)
    pre_proj(*bodies[0])
    for n, (ti, h) in enumerate(bodies):
        if n + 1 < len(bodies):
            ti2, h2 = bodies[n + 1]
            if h2 == 0:
                pre_work(ti2)
            pre_proj(ti2, h2)
        pre_rest(ti, h)
    S.barrier()
    A.reset(m0)

    hnT = A.alloc([8, TL], BF16)
    Thn = [T(f"hn{g}") for g in range(4)]
    cosL = A.alloc([TL], F32)
    sinL = A.alloc([TL], F32)
    Tcs = T("cs")
    m1 = A.mark()
    scr = self.norm_scratch(7)
    for g in range(4):
        self.norm_mod(self.xT[:, :, g * 512:(g + 1) * 512], self.Tx[g * 4:(g + 1) * 4], 512, coef, sh1,
                      hnT[:, :, g * 512:(g + 1) * 512], [Thn[g]], scr)

    pi32 = A.alloc([512], I32)
    ang = A.alloc([512], F32)
    nn = A.alloc([512], F32)
    Tpos = T("pos")
    for g in range(4):
        sl = slice(g * 512, (g + 1) * 512)
        S.dma("sp", pi32, posloc[0:1, sl].partition_broadcast(128), writes=[Tpos])
        self.cp("dve", ang, pi32, [Tpos], [Tpos])
        self.ts("dve", ang, ang, invf[:, 0:1], None, ALU.mult, None, [Tpos, Tt], [Tpos])
        self.ts("dve", nn, ang, INV2PI, MAGIC, ALU.mult, ALU.add, [Tpos], [Tpos])
        self.ts("dve", nn, nn, -MAGIC, None, ALU.add, None, [Tpos], [Tpos])
        self.stt(ang, nn, -C1, ang, ALU.mult, ALU.add, [Tpos], [Tpos])
        self.stt(ang, nn, -C2, ang, ALU.mult, ALU.add, [Tpos], [Tpos])
        self.ts("dve", ang, ang, 3.14159, -3.14159, ALU.min, ALU.max, [Tpos], [Tpos])
        self.act(sinL[:, sl], ang, AF.Sin, [Tpos], [Tcs])
        self.act(nn, ang, AF.Abs, [Tpos], [Tpos])
        self.act(cosL[:, sl], nn, AF.Sin, [Tpos, Tt], [Tcs], bias=halfpi[:, 0:1], scale=-1.0)
    S.barrier()
    A.reset(m1)

    Wi = A.alloc([8, 1536], BF16)
    Wo = A.alloc([4, 1024], BF16)
    TWi, TWo = T("Wi"), T("Wo")
    S32 = A.alloc([2, 512], F32)
    Sbf = A.alloc([2, 512], BF16)
    TS32, TSbf = T("S32"), T("Sbf")
    NBF = 2

    def dbl(shape, dt, name):
        return [A.alloc(shape, dt) for _ in range(NBF)], [T(f"{name}{i}") for i in range(NBF)]
    rt, Trt = [], []
    for i in range(4):
        a_, t_ = dbl([128], F32, f"rt{i}_")
        rt.append(a_)
        Trt.append(t_)
    kTr, Tk = dbl([2, 128], F32, "kTr")
    kTb, Tkb = dbl([2, 128], BF16, "kTb")
    qTr, Tq = dbl([2, 128], BF16, "qTr")
    qx, Tqx = dbl([2, 128], BF16, "qx")
    vb, Tv = dbl([512], BF16, "vb")
    gs, Tgs = dbl([512], F32, "gs")
    SD, TSD = dbl([128], BF16, "SD")
    kz, Tkz = dbl([256], BF16, "kz")
    junk = A.alloc([512], F32)
    ss, Tss = dbl([1], F32, "ss")
    gy, Tgy = dbl([512], F32, "gy")
    gyT, TgyT = dbl([4, 128], BF16, "gyT")
    print("ret arena words used", A.off, "of", A.n)

    def tile_body(h, tl, full):
        b = tl % NBF
        hsrc, Th = hnT[:, :, tl * 128:(tl + 1) * 128], [Thn[tl // 4]]
        cosT = cosL[:, tl * 128:(tl + 1) * 128]
        sinT = sinL[:, tl * 128:(tl + 1) * 128]
        pa = 0 if b == 0 else 2
        for c in range(2):
            for dc in range(8):
                self.mm(ps[pa][:, c * 128:(c + 1) * 128], Wi[:, dc, 256 + c * 128:256 + (c + 1) * 128],
                        hsrc[:, dc, :], dc == 0, dc == 7, [TWi] + Th, [TP[pa]])
        if full:
            for c in range(2):
                for dc in range(8):
                    self.mm(ps[pa][:, 256 + c * 128:256 + (c + 1) * 128], Wi[:, dc, c * 128:(c + 1) * 128],
                            hsrc[:, dc, :], dc == 0, dc == 7, [TWi] + Th, [TP[pa]])
        for dc in range(8):
            self.mm(ps[1][:, :], hsrc[:, dc, :], Wi[:, dc, 512:1024], dc == 0, dc == 7, [TWi] + Th, [TP[1]])

        def rotary(base, o1, o2, To):
            x1 = ps[pa][:, base:base + 128]
            x2 = ps[pa][:, base + 128:base + 256]
            self.tt("dve", rt[0][b], x1, cosT, ALU.mult, [TP[pa], Tcs], [Trt[0][b]])
            self.tt("dve", rt[1][b], x2, sinT, ALU.mult, [TP[pa], Tcs], [Trt[1][b]])
            self.tt("dve", o1, rt[0][b], rt[1][b], ALU.subtract, [Trt[0][b], Trt[1][b]], [To])
            self.tt("dve", rt[2][b], x1, sinT, ALU.mult, [TP[pa], Tcs], [Trt[2][b]])
            self.tt("dve", rt[3][b], x2, cosT, ALU.mult, [TP[pa], Tcs], [Trt[3][b]])
            self.tt("dve", o2, rt[2][b], rt[3][b], ALU.add, [Trt[2][b], Trt[3][b]], [To])
        rotary(0, kTr[b][:, 0, :], kTr[b][:, 1, :], Tk[b])
        self.cp("act", vb[b], ps[1][:, :], [TP[1]], [Tv[b]])
        if full:
            for dc in range(8):
                self.mm(ps[1][:, :], hsrc[:, dc, :], Wi[:, dc, 1024:1536], dc == 0, dc == 7,
                        [TWi] + Th, [TP[1]])
            self.cp("act", kTb[b], kTr[b], [Tk[b]], [Tkb[b]])
            rotary(256, qTr[b][:, 0, :], qTr[b][:, 1, :], Tq[b])
            self.act(gs[b], ps[1][:, :], AF.Silu, [TP[1]], [Tgs[b]])
            for c in range(2):
                self.mm(ps[3][:, 0:128], kTb[b][:, c, :], qTr[b][:, c, :], c == 0, c == 1, [Tkb[b], Tq[b]], [TP[3]])
            self.tt("dve", SD[b], ps[3][:, 0:128], dect[:, h, :], ALU.mult, [TP[3], Tt], [TSD[b]])
            for c in range(2):
                self.tt("dve", qx[b][:, c, :], qTr[b][:, c, :], xit[:, h, :], ALU.mult, [Tq[b], Tt], [Tqx[b]])
            self.mm(ps[4][:, :], SD[b], vb[b], True, False, [TSD[b], Tv[b]], [TP[4]])
            for c in range(2):
                self.mm(ps[4][:, :], qx[b][:, c, :], Sbf[:, c, :], False, c == 1, [Tqx[b], TSbf], [TP[4]])
            self.act(junk, ps[4][:, :], AF.Square, [TP[4]], [Tss[b]], accum_out=ss[b][:, 0:1])
            self.act(ss[b], ss[b], AF.Sqrt, [Tss[b], self.Tc], [Tss[b]], bias=self.epsb[:, 0:1], scale=1.0 / 512)
            S.op("dve", lambda e: e.reciprocal(ss[b], ss[b]), [Tss[b]], [Tss[b]])
            self.stt(gy[b], ps[4][:, :], ss[b][:, 0:1], gs[b], ALU.mult, ALU.mult, [TP[4], Tss[b], Tgs[b]], [Tgy[b]])
            for fc in range(4):
                self.tr(ps[7][:, fc * 128:(fc + 1) * 128], gy[b][:, fc * 128:(fc + 1) * 128], [Tgy[b]], [TP[7]])
            self.cp("act", gyT[b], ps[7][:, :].rearrange("p (a b) -> p a b", b=128), [TP[7]], [TgyT[b]])
        for c in range(2):
            self.tr(ps[3][:, 128 + c * 128:128 + (c + 1) * 128], kTr[b][:, c, :], [Tk[b]], [TP[3]])
        self.ts("dve", kz[b], ps[3][:, 128:384], zet[:, h:h + 1], None, ALU.mult, None, [TP[3], Tt], [Tkz[b]])
        for c in range(2):
            self.mm(ps[5 + c][:, :], kz[b][:, c * 128:(c + 1) * 128], vb[b], True, True, [Tkz[b], Tv[b]], [TP[5 + c]])
            self.stt(S32[:, c, :], S32[:, c, :], gam[h], ps[5 + c][:, :], ALU.mult, ALU.add,
                     [TS32, TP[5 + c]], [TS32])
        if full:
            self.cp("act", Sbf, S32, [TS32], [TSbf])
            for dc in range(8):
                pk = 5 + dc // 4
                for fc in range(4):
                    self.mm(ps[pk][:, (dc % 4) * 128:(dc % 4 + 1) * 128], Wo[:, fc, dc * 128:(dc + 1) * 128],
                            gyT[b][:, fc, :], fc == 0, fc == 3, [TWo, TgyT[b]], [TP[pk]])
            for dc in range(8):
                pk = 5 + dc // 4
                xs = self.xT[:, dc, tl * 128:(tl + 1) * 128]
                self.stt(xs, ps[pk][:, (dc % 4) * 128:(dc % 4 + 1) * 128], g1[:, dc:dc + 1], xs,
                         ALU.mult, ALU.add, [TP[pk], self.Tm, self.Tx[tl]], [self.Tx[tl]])

    for h in range(4):
        S.dma("pool", Wi, win_d[h], writes=[TWi], max_dma_last_dim=2048)
        S.dma("pool", Wo, wo_d[h], writes=[TWo], max_dma_last_dim=2048)
        self.cp("dve", S32, S4[:, h, :, :], [TS4[h]], [TS32])
        self.cp("act", Sbf, S32, [TS32], [TSbf])
        for tl in range(TL // 128):
            tile_body(h, tl, True)


Prog.phase_ret = _phase_ret


def _phase_peer(self, L):
    S, A, nc = self.S, self.A, self.nc
    ps, TP = self.ps, self.TP
    wq_d = self.dram("peerq", [2, 16, 128, 8, 128])
    kk_d = self.dram("peerk", [2, 128, 16, 128])
    u_d = self.dram("peeru", [2, 128, 128, 8, 128])
    v_d = self.dram("peerv", [2, 128, 128, 1024])
    iota_d = self.dram("iota", [128, 128])
    TB = 256
    NB = TL // TB
    self.issue_conv(L)
    ubf, vbf, Tcv = self.conv[L]
    sh2 = self.modT[:, L, 24:32]
    g2 = self.modT[:, L, 40:48]
    coef = self.coefA[:, L, 1, :]

    Tt = T("ptab")
    keysT = A.alloc([16, 128], BF16)
    iota = A.alloc([128], F32)
    S.dma("pool", keysT, kk_d[L], writes=[Tt])
    S.dma("sp", iota, iota_d, writes=[Tt])
    Gt = A.alloc([TB, 128], BF16)
    TG = T("Gt")
    hn = A.alloc([8, TB], BF16)
    Thn = T("hn")
    m_scr = A.mark()
    qTP = A.alloc([16, TB], BF16)
    NS = 16
    Pb = [A.alloc([NS, 128], BF16) for _ in range(2)]
    TqP = T("qTP")
    TPh = [T("P0"), T("P1")]
    scr = self.norm_scratch(7, TB)
    wqb = [A.alloc([8, 128], BF16) for _ in range(4)]
    Twq = [T(f"wq{i}") for i in range(4)]
    sc = A.alloc([16, 128], F32)
    Tsc = T("sc")
    eq2 = sc.rearrange("p a b -> p (a b)").rearrange("p (h r k) -> p h r k", r=16, k=16)
    mr = A.alloc([256], F32)
    Tmr = T("mr")
    mr2 = [mr, A.alloc([256], F32)]
    Tmr2 = [Tmr, T("mrB")]
    Tsth = [T(f"st{i}") for i in range(16)]
    Tbh = [T(f"bh{i}") for i in range(8)]
    stop = A.alloc([16, 16], F32)
    itop = A.alloc([16, 16], U32)
    itopf = A.alloc([16, 16], F32)
    Tst = T("stop")
    cand = A.alloc([8, 256], F32)
    Tcd = T("cand")
    eq = cand.rearrange("p h (r k) -> p h r k", k=16)
    best = A.alloc([8, 16], F32)
    pos = A.alloc([8, 16], U32)
    k1u = A.alloc([8, 16], U32)
    k2u = A.alloc([8, 16], U32)
    k1f = A.alloc([8, 16], F32)
    k2f = A.alloc([8, 16], F32)
    ee = A.alloc([8, 16], F32)
    zz = A.alloc([8], F32)
    Tb = T("best")
    ijg = A.alloc([3, 128], F32)
    Tijg = T("ijg")
    ijgT = A.alloc([3, 128], F32)
    TijgT = T("ijgT")
    Qgb = [A.alloc([NS, 128], BF16) for _ in range(2)]
    TQb = [T("Qg0"), T("Qg1")]
    m_scr_end = A.mark()
    A.reset(m_scr)
    NBUF = 8
    ub = [A.alloc([2, 8, 128], BF16) for _ in range(NBUF)]
    vb = [A.alloc([2, 1024], BF16) for _ in range(NBUF)]
    Tub = [T(f"ub{i}") for i in range(NBUF)]
    Tvb = [T(f"vb{i}") for i in range(NBUF)]
    gel = [A.alloc([TB], BF16) for _ in range(3)]
    Ab = [A.alloc([TB], BF16) for _ in range(3)]
    print("peer arena words used", A.off, m_scr_end, "of", A.n)
    A.off = max(A.off, m_scr_end)
    Tgel = [T("gel0"), T("gel1"), T("gel2")]
    TAb = [T("Ab0"), T("Ab1"), T("Ab2")]
    stop4 = stop.rearrange("p (h two) k -> p h two k", two=2)
    itop4 = itopf.rearrange("p (h two) k -> p h two k", two=2)
    iota16 = iota[:, 0:16].unsqueeze(1).unsqueeze(1).to_broadcast([128, 8, 16, 16])

    for blk in range(NB):
        t0 = blk * TB
        Txb = self.Tx[2 * blk:2 * blk + 2]
        self.norm_mod(self.xT[:, :, t0:t0 + TB], Txb, TB, coef, sh2, hn, [Thn], scr)
        for fc in range(16):
            i = fc % 2
            w = fc % 4
            S.dma("pool", wqb[w], wq_d[L, fc], writes=[Twq[w]])
            for dc in range(8):
                self.mm(ps[i][:, 0:TB], wqb[w][:, dc, :], hn[:, dc, :], dc == 0, dc == 7, [Twq[w], Thn], [TP[i]])
            self.cp("act" if i else "dve", qTP[:, fc, :], ps[i][:, 0:TB], [TP[i]], [TqP])
        for tt in range(2):
            for hp in range(16):
                pk = 2 + hp // 4
                self.mm(ps[pk][:, (hp % 4) * 128:(hp % 4 + 1) * 128], qTP[:, hp, tt * 128:(tt + 1) * 128],
                        keysT[:, hp, :], True, True, [TqP, Tt], [TP[pk]])
            for q4 in range(4):
                self.cp("act" if q4 % 2 else "dve", sc[:, 4 * q4:4 * q4 + 4, :],
                        ps[2 + q4][:, :].rearrange("p (a b) -> p a b", b=128), [TP[2 + q4]], [Tsc])
            V = S
            for hp0 in range(0, 16, 2):
                for step in range(5):
                    for m in range(2):
                        hp = hp0 + m
                        row = sc[:, hp, :]
                        mrm, Tm_, Th_ = mr2[m], Tmr2[m], Tsth[hp]
                        if step == 0:
                            V.op("dve", lambda e, hp=hp, row=row: e.max(stop[:, hp, 0:8], row), [Tsc], [Th_])
                        elif step == 1:
                            V.op("dve", lambda e, hp=hp, row=row: e.max_index(itop[:, hp, 0:8], stop[:, hp, 0:8], row),
                                 [Tsc, Th_], [Th_])
                        elif step == 2:
                            V.op("dve", lambda e, hp=hp, row=row, mrm=mrm: e.match_replace(mrm[:, 0:128], stop[:, hp, 0:8], row, NEG),
                                 [Tsc, Th_], [Tm_])
                        elif step == 3:
                            V.op("dve", lambda e, hp=hp, mrm=mrm: e.max(stop[:, hp, 8:16], mrm[:, 0:128]), [Tm_], [Th_])
                        else:
                            V.op("dve", lambda e, hp=hp, mrm=mrm: e.max_index(itop[:, hp, 8:16], stop[:, hp, 8:16], mrm[:, 0:128]),
                                 [Tm_, Th_], [Th_])
            self.cp("dve", itopf, itop, Tsth, [Tst])
            self.tt("dve", eq, stop4[:, :, 0, :].unsqueeze(3).to_broadcast([128, 8, 16, 16]),
                    stop4[:, :, 1, :].unsqueeze(2).to_broadcast([128, 8, 16, 16]), ALU.add, Tsth + [Tst], [Tcd])
            for h0 in range(0, 8, 2):
                for step in range(5):
                    for m in range(2):
                        h = h0 + m
                        row = cand[:, h, :]
                        mrm, Tm_, Th_ = mr2[m], Tmr2[m], Tbh[h]
                        if step == 0:
                            V.op("dve", lambda e, h=h, row=row: e.max(best[:, h, 0:8], row), [Tcd, Tb], [Th_])
                        elif step == 1:
                            V.op("dve", lambda e, h=h, row=row: e.max_index(pos[:, h, 0:8], best[:, h, 0:8], row),
                                 [Tcd, Th_], [Th_])
                        elif step == 2:
                            V.op("dve", lambda e, h=h, row=row, mrm=mrm: e.match_replace(mrm[:, 0:256], best[:, h, 0:8], row, NEG),
                                 [Tcd, Th_], [Tm_])
                        elif step == 3:
                            V.op("dve", lambda e, h=h, mrm=mrm: e.max(best[:, h, 8:16], mrm[:, 0:256]), [Tm_], [Th_])
                        else:
                            V.op("dve", lambda e, h=h, mrm=mrm: e.max_index(pos[:, h, 8:16], best[:, h, 8:16], mrm[:, 0:256]),
                                 [Tm_, Th_], [Th_])
            self.cp("dve", k2f, pos, Tbh, [Tb])
            gcf = ijg[:, 2, :].rearrange("p (h r) -> p h r", r=16)
            self.tt("dve", ee, best, best[:, :, 0:1].to_broadcast([128, 8, 16]), ALU.subtract, Tbh + [Tb], [Tb])
            self.act(ee, ee, AF.Exp, [Tb], [Tb])
            V.op("dve", lambda e: e.tensor_reduce(zz, ee, AX.X, ALU.add), [Tb], [Tb])
            V.op("dve", lambda e: e.reciprocal(zz, zz), [Tb], [Tb])
            self.tt("dve", gcf, ee, zz.unsqueeze(2).to_broadcast([128, 8, 16]), ALU.mult, [Tb], [Tijg])
            self.ts("dve", k1f, k2f, 0.0625, -0.46875, ALU.mult, ALU.add, [Tb], [Tb])
            self.ts("dve", k1f, k1f, MAGIC, None, ALU.add, None, [Tb], [Tb])
            self.ts("dve", k1f, k1f, -MAGIC, None, ALU.add, None, [Tb], [Tb])
            self.stt(k2f, k1f, -16.0, k2f, ALU.mult, ALU.add, [Tb], [Tb])
            for which, kf, eqb, Teq in ((0, k1f, eq, Tcd), (1, k2f, eq2, Tsc)):
                self.tt("dve", eqb, kf.unsqueeze(3).to_broadcast([128, 8, 16, 16]), iota16, ALU.is_equal,
                        [Tb, Tt], [Teq])
                self.tt("dve", eqb, eqb, itop4[:, :, which, :].unsqueeze(2).to_broadcast([128, 8, 16, 16]),
                        ALU.mult, [Teq, Tst], [Teq])
                dst = ijg[:, which, :].rearrange("p (h r) -> p h r", r=16)
                V.op("dve", lambda e, dst=dst, eqb=eqb: e.tensor_reduce(dst, eqb, AX.X, ALU.add), [Teq], [Tijg])
            for w in range(3):
                self.tr(ps[6][:, w * 128:(w + 1) * 128], ijg[:, w, :], [Tijg], [TP[6]])
            self.cp("act", ijgT, ps[6][:, 0:384].rearrange("p (a b) -> p a b", b=128), [TP[6]], [TijgT])
            for sub in range(128 // NS):
                tsl = slice(sub * NS, (sub + 1) * NS)
                iob = iota.unsqueeze(1).to_broadcast([128, NS, 128])
                P, Qg, TPs, TQ = Pb[sub % 2], Qgb[sub % 2], TPh[sub % 2], TQb[sub % 2]
                self.tt("dve", P, iob, ijgT[:, 0, tsl].unsqueeze(2).to_broadcast([128, NS, 128]), ALU.is_equal,
                        [Tt, TijgT], [TPs])
                self.tt("dve", Qg, iob, ijgT[:, 1, tsl].unsqueeze(2).to_broadcast([128, NS, 128]), ALU.is_equal,
                        [Tt, TijgT], [TQ])
                self.tt("pool", Qg, Qg, ijgT[:, 2, tsl].unsqueeze(2).to_broadcast([128, NS, 128]), ALU.mult,
                        [TQ, TijgT], [TQ])
                for t4 in range(NS // 4):
                    pk = t4 % 2
                    for q in range(4):
                        t = t4 * 4 + q
                        self.mm(ps[pk][:, q * 128:(q + 1) * 128], Qg[:, t, :], P[:, t, :], True, True,
                                [TQ, TPs], [TP[pk]])
                    tok = tt * 128 + sub * NS + t4 * 4
                    self.cp("act", Gt[:, tok:tok + 4, :],
                            ps[pk][:, :].rearrange("p (t i) -> p t i", i=128), [TP[pk]], [TG])
        S.barrier()
        dq = ("sp", "act", "pool")
        for i in range(128):
            k = (i // 2) % NBUF
            if i % 2 == 0:
                S.dma(dq[(i // 2) % 3], ub[k], ubf[i:i + 2].rearrange("i p f -> p i f"),
                      reads=[Tcv[i // 8]], writes=[Tub[k]])
                S.dma(dq[(i // 2 + 1) % 3], vb[k], vbf[i:i + 2].rearrange("i p f -> p i f"),
                      reads=[Tcv[i // 8]], writes=[Tvb[k]])
            sp = 4 + i % 3
            for dc in range(8):
                self.mm(ps[sp][:, 0:TB], ub[k][:, i % 2, dc, :], hn[:, dc, :], dc == 0, dc == 7,
                        [Tub[k], Thn], [TP[sp]])
            g = i % 3
            self.act(gel[g], ps[sp][:, 0:TB], AF.Gelu_apprx_tanh, [TP[sp]], [Tgel[g]])
            self.tt("dve", Ab[g], gel[g], Gt[:, :, i], ALU.mult, [Tgel[g], TG], [TAb[g]])

            def vmm(i):
                k = (i // 2) % NBUF
                g = i % 3
                for dc in range(8):
                    pk = dc // 2
                    self.mm(ps[pk][:, (dc % 2) * TB:(dc % 2 + 1) * TB], vb[k][:, i % 2, dc * 128:(dc + 1) * 128],
                            Ab[g], i == 0 and dc % 2 == 0, i == 127, [Tvb[k], TAb[g]], [TP[pk]])
            if i >= 2:
                vmm(i - 2)
            if i == 127:
                vmm(126)
                vmm(127)
        for dc in range(8):
            pk = dc // 2
            for hh in range(2):
                xs = self.xT[:, dc, t0 + hh * 128:t0 + (hh + 1) * 128]
                self.stt(xs, ps[pk][:, (dc % 2) * TB + hh * 128:(dc % 2) * TB + (hh + 1) * 128], g2[:, dc:dc + 1],
                         xs, ALU.mult, ALU.add, [TP[pk], self.Tm, Txb[hh]], [Txb[hh]])
        S.barrier()


def _issue_conv(self, L):
    if not hasattr(self, "conv"):
        self.conv = {}
    if L in self.conv:
        return
    nc, S = self.nc, self.S
    u_d = self.dram("peeru", [2, 128, 128, 8, 128])
    v_d = self.dram("peerv", [2, 128, 128, 1024])
    ubf = nc.dram_tensor(f"ubf{L}", [128, 128, 1024], BF16, kind="Internal").ap()
    vbf = nc.dram_tensor(f"vbf{L}", [128, 128, 1024], BF16, kind="Internal").ap()
    Tcv = [T(f"cv{L}_{i}") for i in range(16)]
    for k in range(16):
        S.dma("pool", ubf[8 * k:8 * k + 8].rearrange("i p f -> (i p) f"),
              u_d[L, 8 * k:8 * k + 8].rearrange("i p c j -> (i p) (c j)"), writes=[Tcv[k]],
              max_dma_last_dim=2048)
        S.dma("pool", vbf[8 * k:8 * k + 8].rearrange("i p f -> (i p) f"),
              v_d[L, 8 * k:8 * k + 8].rearrange("i p f -> (i p) f"), writes=[Tcv[k]],
              max_dma_last_dim=2048)
    self.conv[L] = (ubf, vbf, Tcv)


def _issue_sgu_conv(self):
    if hasattr(self, "sconv"):
        return
    nc, S = self.nc, self.S
    wu_d = self.dram("sguwu", [24, 128, 8, 128])
    wv_d = self.dram("sguwv", [6, 128, 8, 512])
    wo_d = self.dram("sguwo", [24, 128, 1024])
    wu_b = nc.dram_tensor("sguwu_bf", [24, 128, 1024], BF16, kind="Internal").ap()
    wv_b = nc.dram_tensor("sguwv_bf", [6, 128, 4096], BF16, kind="Internal").ap()
    wo_b = nc.dram_tensor("sguwo_bf", [24, 128, 1024], BF16, kind="Internal").ap()
    Tu, Tv, To = T("cvu"), T("cvv"), T("cvo")
    for k in range(3):
        S.dma("pool", wu_b[8 * k:8 * k + 8].rearrange("f p x -> (f p) x"),
              wu_d[8 * k:8 * k + 8].rearrange("f p c m -> (f p) (c m)"), writes=[Tu], max_dma_last_dim=2048)
        S.dma("pool", wo_b[8 * k:8 * k + 8].rearrange("f p x -> (f p) x"),
              wo_d[8 * k:8 * k + 8].rearrange("f p x -> (f p) x"), writes=[To], max_dma_last_dim=2048)
    for g in range(6):
        S.dma("pool", wv_b[g].rearrange("p (a x) -> (p a) x", a=2),
              wv_d[g].rearrange("p (a c) n -> (p a) (c n)", a=2), writes=[Tv], max_dma_last_dim=2048)
    self.sconv = (wu_b, wv_b, wo_b, Tu, Tv, To)


Prog.issue_sgu_conv = _issue_sgu_conv
Prog.issue_conv = _issue_conv
Prog.phase_peer = _phase_peer


def _phase_sgu(self):
    S, A = self.S, self.A
    ps, TP = self.ps, self.TP
    wu_d = self.dram("sguwu", [24, 128, 8, 128])
    wv_d = self.dram("sguwv", [6, 128, 8, 512])
    bu_d = self.dram("sgubu", [128, 24])
    bv_d = self.dram("sgubv", [1, 3072])
    lng_d = self.dram("sgulng", [128, 24])
    lnb2_d = self.dram("sgulnb2", [2, 3072])
    ws_d = self.dram("sguws", [128, 8, 128])
    bs_d = self.dram("sgubs", [1, 8, 128])
    wo_d = self.dram("sguwo", [24, 128, 1024])
    mask_d = self.dram("sgumask", [128, 128])
    L = 1
    TB = 256
    NB = TL // TB
    sh1 = self.modT[:, L, 0:8]
    g1 = self.modT[:, L, 16:24]
    coef = self.coefA[:, L, 0, :]
    self.issue_sgu_conv()
    wu_b, wv_b, wo_b, Tcu, Tcv_, Tco = self.sconv
    dq = ("sp", "act", "pool")

    Tt = T("stab")
    bu = A.alloc([24], F32)
    lng = A.alloc([24], F32)
    bv = A.alloc([3072], F32)
    lnb2 = A.alloc([3072], F32)
    rhs2 = A.alloc([8, 128], F32)
    WmT = A.alloc([8, 128], BF16)
    Btab = A.alloc([24, 128], F32)
    S.dma("sp", bu, bu_d, writes=[Tt])
    S.dma("sp", lng, lng_d, writes=[Tt])
    S.dma("sp", bv[0:1, :], bv_d, writes=[Tt])
    S.dma("sp", lnb2[0:2, :], lnb2_d, writes=[Tt])
    S.dma("sp", rhs2[1:2, :, :], bs_d, writes=[Tt])
    m1 = A.mark()
    ws = A.alloc([8, 128], F32)
    msk = A.alloc([128], F32)
    wm32 = A.alloc([8, 128], F32)
    Tws = T("ws")
    S.dma("act", ws, ws_d, writes=[Tws])
    S.dma("act", msk, mask_d, writes=[Tws])
    self.tt("dve", ws, ws, msk.unsqueeze(1).to_broadcast([128, 8, 128]), ALU.mult, [Tws], [Tws])
    for half in range(2):
        for q in range(4):
            g = half * 4 + q
            self.tr(ps[6 + half][:, q * 128:(q + 1) * 128], ws[:, g, :], [Tws], [TP[6 + half]])
        self.cp("dve", wm32[:, half * 4:(half + 1) * 4, :], ps[6 + half][:, :].rearrange("p (a b) -> p a b", b=128),
                [TP[6 + half]], [Tws])
    self.cp("act", WmT, wm32, [Tws], [Tt])
    for half in range(2):
        self.mm(ps[6 + half][0:1, :], self.ones[:, 0:1], wm32[:, half * 4:(half + 1) * 4, :].rearrange("p a b -> p (a b)"),
                True, True, [Tws, self.Tc], [TP[6 + half]])
        self.cp("dve", rhs2[0:1, half * 4:(half + 1) * 4, :], ps[6 + half][0:1, :].rearrange("p (a b) -> p a b", b=128),
                [TP[6 + half]], [Tt])
    for fc in range(24):
        pk = 6 + (fc // 4) % 2
        self.mm(ps[pk][:, (fc % 4) * 128:(fc % 4 + 1) * 128], lnb2[0:2, fc * 128:(fc + 1) * 128],
                rhs2[0:2, fc // 3, :], True, True, [Tt], [TP[pk]])
        if fc % 4 == 3:
            self.cp("dve", Btab[:, fc - 3:fc + 1, :], ps[pk][:, :].rearrange("p (a b) -> p a b", b=128),
                    [TP[pk]], [Tt])
    S.barrier()
    A.reset(m1)

    hn = A.alloc([8, TB], BF16)
    Thn = T("hn")
    scr = self.norm_scratch(7, TB)
    uT = A.alloc([24, TB], BF16)
    TuT = T("uT")
    vf = [A.alloc([3072], F32) for _ in range(2)]
    vn = [A.alloc([3072], BF16) for _ in range(2)]
    Tvf = [T("vf0"), T("vf1")]
    Tvn = [T("vn0"), T("vn1")]
    NWU = 4
    wub = [A.alloc([8, 128], BF16) for _ in range(NWU)]
    Twu = [T(f"wu{i}") for i in range(NWU)]
    wvb = [A.alloc([8, 512], BF16) for _ in range(2)]
    Twv = [T("wv0"), T("wv1")]
    NWO = 4
    wob = [A.alloc([1024], BF16) for _ in range(NWO)]
    Two = [T(f"wo{i}") for i in range(NWO)]
    s1 = A.alloc([8], F32)
    s2 = A.alloc([1], F32)
    mu = A.alloc([1], F32)
    var = A.alloc([1], F32)
    nb = A.alloc([1], F32)
    Tst = T("lnstat")
    tmp = [A.alloc([128], F32) for _ in range(2)]
    Ttmp = [T("t0"), T("t1")]
    print("sgu arena words used", A.off, "of", A.n)

    for blk in range(NB):
        t0 = blk * TB
        Txb = self.Tx[2 * blk:2 * blk + 2]
        self.norm_mod(self.xT[:, :, t0:t0 + TB], Txb, TB, coef, sh1, hn, [Thn], scr)
        for fc in range(24):
            i = fc % 2
            w = fc % NWU
            S.dma(dq[fc % 3], wub[w], wu_b[fc], reads=[Tcu], writes=[Twu[w]])
            for dc in range(8):
                self.mm(ps[4 + i][:, 0:TB], wub[w][:, dc, :], hn[:, dc, :], dc == 0, dc == 7, [Twu[w], Thn], [TP[4 + i]])
            self.act(uT[:, fc, :], ps[4 + i][:, 0:TB], AF.Gelu_apprx_tanh, [TP[4 + i], Tt], [TuT], bias=bu[:, fc:fc + 1])
        for cg in range(6):
            i = cg % 2
            S.dma(dq[cg % 3], wvb[i], wv_b[cg], reads=[Tcv_], writes=[Twv[i]])
            for tt in range(2):
                pk = 4 + tt
                self.mm(ps[pk][:, :], self.ones[0:1, :], bv[0:1, cg * 512:(cg + 1) * 512], True, False,
                        [self.Tc, Tt], [TP[pk]])
                for dc in range(8):
                    self.mm(ps[pk][:, :], hn[:, dc, tt * 128:(tt + 1) * 128], wvb[i][:, dc, :], False, dc == 7,
                            [Twv[i], Thn], [TP[pk]])
                self.act(vf[tt][:, cg * 512:(cg + 1) * 512], ps[pk][:, :], AF.Gelu_apprx_tanh, [TP[pk]], [Tvf[tt], Tst],
                         accum_out=s1[:, tt * 4 + cg // 2 * 0 + 0:tt * 4 + 1] if False else None)
        for tt in range(2):
            S.op("dve", lambda e, tt=tt: e.tensor_reduce(mu, vf[tt], AX.X, ALU.add), [Tvf[tt]], [Tst])
            self.act(vn[tt], vf[tt], AF.Square, [Tvf[tt]], [Tvn[tt], Tst], accum_out=s2[:, 0:1])
            self.ts("dve", mu, mu, 1.0 / 3072, None, ALU.mult, None, [Tst], [Tst])
            self.tt("dve", var, mu, mu, ALU.mult, [Tst], [Tst])
            self.stt(var, s2, 1.0 / 3072, var, ALU.mult, ALU.subtract, [Tst], [Tst])
            self.act(var, var, AF.Sqrt, [Tst, self.Tc], [Tst], bias=self.epsb[:, 0:1], scale=1.0)
            S.op("dve", lambda e: e.reciprocal(var, var), [Tst], [Tst])
            self.stt(nb, mu, -1.0, var, ALU.mult, ALU.mult, [Tst], [Tst])
            self.act(vn[tt], vf[tt], AF.Identity, [Tvf[tt], Tst], [Tvn[tt]], bias=nb[:, 0:1], scale=var[:, 0:1])
        for tt in range(2):
            for fc in range(24):
                pk = 6 + fc % 2
                q = (fc // 2) % 4
                self.mm(ps[pk][:, q * 128:(q + 1) * 128], vn[tt][:, fc * 128:(fc + 1) * 128], WmT[:, fc // 3, :],
                        True, True, [Tvn[tt], Tt], [TP[pk]])
                i = fc % 2
                self.stt(tmp[i], ps[pk][:, q * 128:(q + 1) * 128], lng[:, fc:fc + 1], Btab[:, fc, :],
                         ALU.mult, ALU.add, [TP[pk], Tt], [Ttmp[i]])
                usl = uT[:, fc, tt * 128:(tt + 1) * 128]
                self.tt("pool", usl, usl, tmp[i], ALU.mult, [Ttmp[i], TuT], [TuT])
        for fc in range(24):
            k = fc % NWO
            S.dma(dq[fc % 3], wob[k], wo_b[fc], reads=[Tco], writes=[Two[k]])
            for dc in range(8):
                pk = dc // 2
                self.mm(ps[pk][:, (dc % 2) * TB:(dc % 2 + 1) * TB], wob[k][:, dc * 128:(dc + 1) * 128], uT[:, fc, :],
                        fc == 0 and dc % 2 == 0, fc == 23, [Two[k], TuT], [TP[pk]])
        for dc in range(8):
            pk = dc // 2
            for hh in range(2):
                xs = self.xT[:, dc, t0 + hh * 128:t0 + (hh + 1) * 128]
                self.stt(xs, ps[pk][:, (dc % 2) * TB + hh * 128:(dc % 2) * TB + (hh + 1) * 128], g1[:, dc:dc + 1],
                         xs, ALU.mult, ALU.add, [TP[pk], self.Tm, Txb[hh]], [Txb[hh]])


Prog.phase_sgu = _phase_sgu
```
